# Optimizing a Trainium2 kernel written in Bass

```python
import jax, jax.numpy as jnp
from jax import lax
import numpy as np

D_MODEL = 1024
BATCH = 8
SEQ = 2048
DEPTH = 2

CTX_LEN = 256
GRID_W = 64
N_Q_HEADS = 8
N_KV_HEADS = 2
HEAD_DIM = 64
ATTN_WIDTH = N_Q_HEADS * HEAD_DIM
KV_WIDTH = N_KV_HEADS * HEAD_DIM
Q_BLOCK = 128
ROPE_THETA = 10000.0
LRU_WIDTH = 512
LRU_BLOCKS = 8
LRU_BLOCK = LRU_WIDTH // LRU_BLOCKS
CONV_WIDTH = 4
LRU_C = 8.0
N_DIRS = 2
MIX_WIDTH = ATTN_WIDTH + LRU_WIDTH
IN_WIDTH = ATTN_WIDTH + 2 * KV_WIDTH + 2 * LRU_WIDTH
N_EXPERTS = 16
N_EXPERT_GROUPS = 4
EXPERTS_PER_GROUP = N_EXPERTS // N_EXPERT_GROUPS
TOP_K = 2
D_EXPERT = 512
N_MOD = 6
NORM_EPS = 1e-6

kernel_name = "hybrid_gqa_rglru_grouped_moe_dit"


def rms_norm(x, g):
    xf = x.astype(jnp.float32)
    y = xf * lax.rsqrt(jnp.mean(xf * xf, axis=-1, keepdims=True) + NORM_EPS)
    return (y * g.astype(jnp.float32)).astype(x.dtype)


def modulate(h, shift, scale):
    return h * (1 + scale) + shift


def axial_rope_tables(n_tokens):
    rows = n_tokens // GRID_W
    r, col = jnp.meshgrid(jnp.arange(rows), jnp.arange(GRID_W), indexing="ij")
    r = r.reshape(-1).astype(jnp.float32)
    col = col.reshape(-1).astype(jnp.float32)
    half = HEAD_DIM // 2
    inv = ROPE_THETA ** (-jnp.arange(0, half, 2, dtype=jnp.float32) / half)
    ang = jnp.concatenate([r[:, None] * inv, col[:, None] * inv], axis=-1)
    return jnp.cos(ang), jnp.sin(ang)


def apply_axial_rope(x, cos, sin):
    S = x.shape[1]
    quarter = HEAD_DIM // 4
    xr = x.reshape(x.shape[:-1] + (2, 2, quarter))
    x1, x2 = xr[..., 0, :], xr[..., 1, :]
    cs = cos.reshape(S, 1, 2, quarter).astype(x.dtype)
    sn = sin.reshape(S, 1, 2, quarter).astype(x.dtype)
    o1 = x1 * cs - x2 * sn
    o2 = x2 * cs + x1 * sn
    return jnp.stack([o1, o2], axis=-2).reshape(x.shape)


def blocked_attention(q, k, v):
    B, Tq = q.shape[:2]
    n_blk = Tq // Q_BLOCK
    grp = N_Q_HEADS // N_KV_HEADS
    scale = HEAD_DIM ** -0.5
    qb = q.reshape(B, n_blk, Q_BLOCK, N_KV_HEADS, grp, HEAD_DIM).transpose(1, 0, 2, 3, 4, 5)

    def one_block(q_blk):
        s = jnp.einsum("bqhgd,bkhd->bhgqk", q_blk, k).astype(jnp.float32) * scale
        p = jax.nn.softmax(s, axis=-1).astype(v.dtype)
        return jnp.einsum("bhgqk,bkhd->bqhgd", p, v)

    out = lax.map(one_block, qb)
    return out.transpose(1, 0, 2, 3, 4, 5).reshape(B, Tq, ATTN_WIDTH)


def centred_depthwise_conv(u, w, b):
    left = CONV_WIDTH // 2
    out = lax.conv_general_dilated(
        u, w[:, None, :].astype(u.dtype), window_strides=(1,),
        padding=[(left, CONV_WIDTH - 1 - left)],
        dimension_numbers=("NWC", "WIO", "NWC"), feature_group_count=u.shape[-1])
    return out + b


def linear_scan(a, b):
    def combine(left, right):
        a_l, b_l = left
        a_r, b_r = right
        return a_l * a_r, a_r * b_l + b_r
    _, h = lax.associative_scan(combine, (a, b), axis=1)
    return h


def rglru_direction(u, h0, w_a, b_a, w_x, b_x, lam, reverse):
    B, T, W = u.shape
    ub = u.reshape(B, T, LRU_BLOCKS, LRU_BLOCK)
    r = jax.nn.sigmoid(jnp.einsum("btnd,nde->btne", ub, w_a).reshape(B, T, W) + b_a)
    i = jax.nn.sigmoid(jnp.einsum("btnd,nde->btne", ub, w_x).reshape(B, T, W) + b_x)
    log_a = (-LRU_C * r.astype(jnp.float32)) * jax.nn.softplus(-lam.astype(jnp.float32))
    a = jnp.exp(log_a)
    b = jnp.sqrt(-jnp.expm1(2.0 * log_a)) * (i * u).astype(jnp.float32)
    if reverse:
        a, b = a[:, ::-1], b[:, ::-1]
    b = b.at[:, 0].add(a[:, 0] * h0)
    h = linear_scan(a, b)
    if reverse:
        h = h[:, ::-1]
    return h


def parallel_mixers(h, hc, p, cos, sin, last):
    B, S, _ = h.shape
    C = hc.shape[1]
    cuts = [ATTN_WIDTH, ATTN_WIDTH + KV_WIDTH, ATTN_WIDTH + 2 * KV_WIDTH,
            ATTN_WIDTH + 2 * KV_WIDTH + LRU_WIDTH]
    q, k, v, u, gb = jnp.split(h @ p["w_in"], cuts, axis=-1)
    qc, kc, vc, uc, gbc = jnp.split(hc @ p["w_in"], cuts, axis=-1)

    q = apply_axial_rope(rms_norm(q.reshape(B, S, N_Q_HEADS, HEAD_DIM), p["q_norm_g"]), cos, sin)
    k = apply_axial_rope(rms_norm(k.reshape(B, S, N_KV_HEADS, HEAD_DIM), p["k_norm_g"]), cos, sin)
    v = v.reshape(B, S, N_KV_HEADS, HEAD_DIM)
    kc = rms_norm(kc.reshape(B, C, N_KV_HEADS, HEAD_DIM), p["k_norm_g"])
    vc = vc.reshape(B, C, N_KV_HEADS, HEAD_DIM)
    k_all = jnp.concatenate([kc, k], axis=1)
    v_all = jnp.concatenate([vc, v], axis=1)
    attn = blocked_attention(q, k_all, v_all)

    u = centred_depthwise_conv(u, p["conv_w"], p["conv_b"])
    uc = centred_depthwise_conv(uc, p["conv_w"], p["conv_b"])
    zeros = jnp.zeros((B, LRU_WIDTH), jnp.float32)
    h_lat = 0.0
    h_ctx = 0.0
    for d, reverse in enumerate((False, True)):
        args = (p["lru_wa"][d], p["lru_ba"][d], p["lru_wx"][d], p["lru_bx"][d], p["lru_lambda"][d], reverse)
        hc_d = rglru_direction(uc, zeros, *args)
        h0 = hc_d[:, 0] if reverse else hc_d[:, -1]
        h_lat = h_lat + rglru_direction(u, h0, *args)
        h_ctx = h_ctx + hc_d
    rec = h_lat.astype(h.dtype) * jax.nn.gelu(gb, approximate=True)
    mix = jnp.concatenate([rms_norm(attn, p["attn_out_g"]), rms_norm(rec, p["lru_out_g"])], axis=-1)
    if last:
        return mix, None

    qc = rms_norm(qc.reshape(B, C, N_Q_HEADS, HEAD_DIM), p["q_norm_g"])
    attn_c = blocked_attention(qc, kc, vc)
    rec_c = h_ctx.astype(hc.dtype) * jax.nn.gelu(gbc, approximate=True)
    mix_c = jnp.concatenate([rms_norm(attn_c, p["attn_out_g"]), rms_norm(rec_c, p["lru_out_g"])], axis=-1)
    return mix, mix_c


def grouped_moe(h, router_w, router_b, w_gate, w_up, w_down):
    N = h.shape[0]
    scores = jax.nn.sigmoid(h.astype(jnp.float32) @ router_w.astype(jnp.float32))
    biased = scores + router_b.astype(jnp.float32)
    grouped = biased.reshape(N, N_EXPERT_GROUPS, EXPERTS_PER_GROUP)
    group_score = lax.top_k(grouped, TOP_K)[0].sum(-1)
    best_group = jnp.argmax(group_score, axis=-1)
    in_group = (jnp.arange(N_EXPERTS) // EXPERTS_PER_GROUP)[None, :] == best_group[:, None]
    masked = jnp.where(in_group, biased, -jnp.inf)
    _, top_idx = lax.top_k(masked, TOP_K)
    top_w = jnp.take_along_axis(scores, top_idx, axis=-1)
    top_w = top_w / jnp.sum(top_w, axis=-1, keepdims=True)
    combine = jnp.einsum("nk,nke->ne", top_w,
                         jax.nn.one_hot(top_idx, N_EXPERTS, dtype=jnp.float32)).astype(h.dtype)
    out = jnp.zeros_like(h)
    for e in range(N_EXPERTS):
        hid = jax.nn.silu(h @ w_gate[e]) * (h @ w_up[e])
        out = out + combine[:, e:e + 1] * (hid @ w_down[e])
    return out


def hybrid_layer(x, xc, mod, mod_c, p, router_w, router_b, cos, sin, last):
    B, S, D = x.shape
    C = xc.shape[1]
    sh1, sc1, g1, sh2, sc2, g2 = jnp.split(mod[:, None, :], N_MOD, axis=-1)
    csh1, csc1, cg1, csh2, csc2, cg2 = jnp.split(mod_c, N_MOD, axis=-1)

    h = modulate(rms_norm(x, p["norm1_g"]), sh1, sc1)
    hc = modulate(rms_norm(xc, p["norm1_g"]), csh1, csc1)
    mix, mix_c = parallel_mixers(h, hc, p, cos, sin, last)
    x = x + g1 * (mix @ p["w_out"])
    h2 = modulate(rms_norm(x, p["norm2_g"]), sh2, sc2)
    moe_w = (router_w, router_b, p["w_gate"], p["w_up"], p["w_down"])
    if last:
        y = grouped_moe(h2.reshape(B * S, D), *moe_w).reshape(B, S, D)
        return x + g2 * y, xc

    xc = xc + cg1 * (mix_c @ p["w_out"])
    hc2 = modulate(rms_norm(xc, p["norm2_g"]), csh2, csc2)
    tokens = jnp.concatenate([h2.reshape(B * S, D), hc2.reshape(B * C, D)], axis=0)
    y = grouped_moe(tokens, *moe_w)
    x = x + g2 * y[:B * S].reshape(B, S, D)
    xc = xc + cg2 * y[B * S:].reshape(B, C, D)
    return x, xc


def setup_inputs(seed: int = 0) -> dict:
    key = jax.random.key(seed)
    ks = jax.random.split(key, 32)
    f32 = jnp.float32
    D = D_MODEL

    def nrm(k, shape, scale):
        return jax.random.normal(k, shape, f32) * scale

    a_pow = jax.random.uniform(ks[16], (DEPTH, N_DIRS, LRU_WIDTH), f32, minval=0.9, maxval=0.999)
    a_base = a_pow ** (1.0 / LRU_C)
    lru_lambda = jnp.log(a_base) - jnp.log1p(-a_base)
    return {
        "x": nrm(ks[0], (BATCH, SEQ, D), 1.0),
        "c": nrm(ks[1], (BATCH, D), 1.0),
        "ctx": nrm(ks[2], (BATCH, CTX_LEN, D), 1.0),
        "c_ctx": nrm(ks[3], (D,), 1.0),
        "ada_w": nrm(ks[4], (DEPTH, D, N_MOD * D), 0.5 * D ** -0.5),
        "ada_b": nrm(ks[5], (DEPTH, N_MOD * D), 0.02),
        "norm1_g": 1.0 + nrm(ks[6], (DEPTH, D), 0.02),
        "w_in": nrm(ks[7], (DEPTH, D, IN_WIDTH), D ** -0.5),
        "q_norm_g": 1.0 + nrm(ks[8], (DEPTH, HEAD_DIM), 0.02),
        "k_norm_g": 1.0 + nrm(ks[9], (DEPTH, HEAD_DIM), 0.02),
        "conv_w": nrm(ks[10], (DEPTH, CONV_WIDTH, LRU_WIDTH), CONV_WIDTH ** -0.5),
        "conv_b": nrm(ks[11], (DEPTH, LRU_WIDTH), 0.02),
        "lru_wa": nrm(ks[12], (DEPTH, N_DIRS, LRU_BLOCKS, LRU_BLOCK, LRU_BLOCK), LRU_BLOCK ** -0.5),
        "lru_ba": nrm(ks[13], (DEPTH, N_DIRS, LRU_WIDTH), 0.1),
        "lru_wx": nrm(ks[14], (DEPTH, N_DIRS, LRU_BLOCKS, LRU_BLOCK, LRU_BLOCK), LRU_BLOCK ** -0.5),
        "lru_bx": nrm(ks[15], (DEPTH, N_DIRS, LRU_WIDTH), 0.1),
        "lru_lambda": lru_lambda,
        "attn_out_g": 1.0 + nrm(ks[17], (DEPTH, ATTN_WIDTH), 0.02),
        "lru_out_g": 1.0 + nrm(ks[18], (DEPTH, LRU_WIDTH), 0.02),
        "w_out": nrm(ks[19], (DEPTH, MIX_WIDTH, D), MIX_WIDTH ** -0.5),
        "norm2_g": 1.0 + nrm(ks[20], (DEPTH, D), 0.02),
        "router_w": nrm(ks[21], (D, N_EXPERTS), D ** -0.5),
        "router_b": nrm(ks[22], (N_EXPERTS,), 0.01),
        "exp_w_gate": nrm(ks[23], (DEPTH, N_EXPERTS, D, D_EXPERT), D ** -0.5),
        "exp_w_up": nrm(ks[24], (DEPTH, N_EXPERTS, D, D_EXPERT), D ** -0.5),
        "exp_w_down": nrm(ks[25], (DEPTH, N_EXPERTS, D_EXPERT, D), D_EXPERT ** -0.5),
        "final_g": 1.0 + nrm(ks[26], (D,), 0.02),
    }


def reference(x, c, ctx, c_ctx, ada_w, ada_b, norm1_g, w_in, q_norm_g, k_norm_g,
              conv_w, conv_b, lru_wa, lru_ba, lru_wx, lru_bx, lru_lambda,
              attn_out_g, lru_out_g, w_out, norm2_g, router_w, router_b,
              exp_w_gate, exp_w_up, exp_w_down, final_g):
    cos, sin = axial_rope_tables(x.shape[1])
    silu_c = jax.nn.silu(c)
    silu_cc = jax.nn.silu(c_ctx)
    xc = ctx
    for l in range(DEPTH):
        mod = silu_c @ ada_w[l] + ada_b[l]
        mod_c = silu_cc @ ada_w[l] + ada_b[l]
        p = {
            "norm1_g": norm1_g[l], "w_in": w_in[l], "q_norm_g": q_norm_g[l], "k_norm_g": k_norm_g[l],
            "conv_w": conv_w[l], "conv_b": conv_b[l], "lru_wa": lru_wa[l], "lru_ba": lru_ba[l],
            "lru_wx": lru_wx[l], "lru_bx": lru_bx[l], "lru_lambda": lru_lambda[l],
            "attn_out_g": attn_out_g[l], "lru_out_g": lru_out_g[l], "w_out": w_out[l],
            "norm2_g": norm2_g[l], "w_gate": exp_w_gate[l], "w_up": exp_w_up[l], "w_down": exp_w_down[l],
        }
        x, xc = hybrid_layer(x, xc, mod, mod_c, p, router_w, router_b, cos, sin, l == DEPTH - 1)
    return rms_norm(x, final_g)
```

```python
import contextlib
import os
import numpy as np
import concourse.bass as bass
import concourse.mybir as mybir
from concourse.bass_utils import run_bass_kernel_spmd

F32 = mybir.dt.float32
BF16 = mybir.dt.bfloat16
AF = mybir.ActivationFunctionType
ALU = mybir.AluOpType
AX = mybir.AxisListType

D = 1024
S = 2048
C = 256
T = S + C
NL = 2
NE = 16
DE = 512
INW = 1792
TT = [(0, 256), (256, 768), (768, 1280), (1280, 1792), (1792, 2304)]
NCH = T // 128
EPS = 1e-6

PV_ITEMS = [
    ("cc", (8, 2)), ("adab", (NL, 48)), ("n1g", (NL, 8)), ("n2g", (NL, 8)), ("fg", (8,)),
    ("qg", (NL,)), ("kg", (NL,)), ("cw", (NL, 4, 4)), ("cb", (NL, 4)),
    ("lba", (NL, 2, 4)), ("lbx", (NL, 2, 4)), ("lam", (NL, 2, 4)),
    ("aog", (NL, 4)), ("log", (NL, 4)), ("rb", (NCH, 16)),
]
PV_OFF = {}
_o = 0
for _n, _s in PV_ITEMS:
    PV_OFF[_n] = (_o, _s)
    _o += int(np.prod(_s))
NPV = _o


class Prog:
    def __init__(self, nc, es):
        self.nc = nc
        self.es = es
        self.E = dict(pe=nc.tensor, act=nc.scalar, dve=nc.vector, pool=nc.gpsimd, sp=nc.sync)
        self.sem = {}
        self.cnt = {}
        for k in self.E:
            self.sem[k] = es.enter_context(nc.semaphore("sem_" + k))
            self.cnt[k] = 0
        self.seen = {k: {} for k in self.E}
        self.res = {}
        self.q = {k: [] for k in self.E}
        self._pw = {k: [] for k in self.E}
        self._bank = 0
        self.reserved = set()

    def _deps(self, eng, r, w, skip=None):
        need = {}
        for key in r:
            st = self.res.get(key)
            if st and st[0] is not None:
                k, v = st[0]
                if need.get(k, 0) < v:
                    need[k] = v
        for key in w:
            st = self.res.get(key)
            if st:
                if st[0] is not None:
                    k, v = st[0]
                    if need.get(k, 0) < v:
                        need[k] = v
                for k, v in st[1].items():
                    if need.get(k, 0) < v:
                        need[k] = v
        for k, v in need.items():
            if k == skip:
                continue
            if k == eng:
                if eng == "pe" or v > self.cnt[eng]:
                    continue
            if self.seen[eng].get(k, 0) < v:
                self.E[eng].wait_ge(self.sem[k], v)
                self.seen[eng][k] = v
                self._pw[eng].append((k, v))

    def _mark(self, tag, r, w):
        k, v = tag
        for key in r:
            st = self.res.setdefault(key, [None, {}])
            if st[1].get(k, 0) < v:
                st[1][k] = v
        for key in w:
            self.res[key] = [tag, {}]

    def op(self, eng, fn, r=(), w=(), inc=True):
        pr = [k for k in r if isinstance(k, tuple) and k[0] == "ps"]
        if pr:
            w = list(w) + pr
        self._deps(eng, r, w)
        inst = fn(self.E[eng])
        self.q[eng].append((self._pw[eng], (eng, 1) if inc else None))
        self._pw[eng] = []
        if inc:
            self.cnt[eng] += 1
            inst.then_inc(self.sem[eng], 1)
            tag = (eng, self.cnt[eng])
        else:
            tag = (eng, self.cnt[eng] + 1)
        self._mark(tag, r, w)
        return inst

    def dma(self, q, out, in_, semkey, r=(), w=(), **kw):
        if semkey not in self.sem:
            self.sem[semkey] = self.es.enter_context(self.nc.semaphore("d_" + semkey))
            self.cnt[semkey] = 0
        self._deps(q, r, w, skip=semkey)
        inst = self.E[q].dma_start(out=out, in_=in_, **kw)
        self.q[q].append((self._pw[q], (semkey, 16)))
        self._pw[q] = []
        self.cnt[semkey] += 16
        inst.then_inc(self.sem[semkey], 16)
        self._mark((semkey, self.cnt[semkey]), r, w)
        return inst

    def barrier(self):
        for e in self.E:
            for k, v in self.cnt.items():
                if k != e and v > 0 and self.seen[e].get(k, 0) < v:
                    self.E[e].wait_ge(self.sem[k], v)
                    self.seen[e][k] = v
                    self._pw[e].append((k, v))

    def check_deadlock(self):
        val = {k: 0 for k in self.cnt}
        pos = {k: 0 for k in self.E}
        for e in self.E:
            if self._pw[e]:
                self.q[e].append((self._pw[e], None))
                self._pw[e] = []
        progress = True
        while progress:
            progress = False
            for e in self.E:
                ql = self.q[e]
                while pos[e] < len(ql):
                    waits, inc = ql[pos[e]]
                    if all(val[k] >= v for k, v in waits):
                        if inc is not None:
                            val[inc[0]] += inc[1]
                        pos[e] += 1
                        progress = True
                    else:
                        break
        stuck = {e: (pos[e], len(self.q[e]), self.q[e][pos[e]][0]) for e in self.E if pos[e] < len(self.q[e])}
        if stuck:
            raise RuntimeError("DEADLOCK in semaphore plan: %r ; vals=%r" % (stuck, {k: val[k] for e in stuck for k, _ in stuck[e][2]}))

    def nb(self):
        while True:
            b = self._bank
            self._bank = (self._bank + 1) % 8
            if b not in self.reserved:
                return b


def build(stage=99, dbg=False):
    nc = bass.Bass("TRN2", target_bir_lowering=False)

    def dten(name, shape, dty=F32, kind="ExternalInput"):
        return nc.dram_tensor(name, shape, dty, kind=kind).ap()

    x_d = dten("x", [S, D])
    ctx_d = dten("ctx", [C, D])
    pv_d = dten("pvec", [128, NPV])
    cm_d = dten("cmat", [128, 256])
    cmb_d = dten("cmatb", [128, 256 + 2048])
    rope_d = dten("rope", [2, 128, S])
    adaw_d = dten("ada_w", [NL, D, 6 * D])
    win_d = dten("w_in", [NL, D, INW])
    lru_d = dten("lrubd", [NL, 128, 2048])
    wout_d = dten("w_out", [NL, D, D])
    rw_d = dten("rw", [128, 128])
    wg_d = dten("wg", [NL, NE, D, DE])
    wu_d = dten("wu", [NL, NE, D, DE])
    wd_d = dten("wd", [NL, NE, DE, D])
    out_d = dten("out", [S, D], kind="ExternalOutput")
    if dbg:
        dbgf_d = dten("dbgf", [128, 8 * T], F32, kind="ExternalOutput")
        dbgb_d = dten("dbgb", [128, 8 * T], BF16, kind="ExternalOutput")

    with contextlib.ExitStack() as es:
        P = Prog(nc, es)

        def sb(stack, name, shape, dty):
            return stack.enter_context(nc.sbuf_tensor("s_" + name, shape, dty))

        ps = [es.enter_context(nc.psum_tensor("ps%d" % i, [128, 512], F32)) for i in range(8)]

        def psk(b):
            return ("ps", b)

        xT = sb(es, "xT", [128, 8, T], F32)
        hT = sb(es, "hT", [128, 8, T], BF16)
        pv = sb(es, "pv", [128, NPV], F32)
        cm = sb(es, "cm", [128, 256], F32)
        cmb = sb(es, "cmb", [128, 256], BF16)
        scb = sb(es, "scb", [128, 8, 2], BF16)
        modv = sb(es, "modv", [128, NL, 48, 2], F32)
        gsv = sb(es, "gsv", [128, NL, 2, 8, 2], F32)
        hsp = sb(es, "hsp", [128, NL * 2 * 4], F32)
        hba = sb(es, "hba", [128, NL * 2 * 4], F32)
        hbx = sb(es, "hbx", [128, NL * 2 * 4], F32)
        rw = sb(es, "rw", [128, 128], F32)
        smalltmp = sb(es, "smalltmp", [128, 64], F32)

        ident = cm[:, 0:128]
        perm = cm[:, 128:256]
        ones_b = cmb[:, 0:128]
        bones_b = cmb[:, 128:256]

        def pvv(name):
            o, s = PV_OFF[name]
            n = int(np.prod(s))
            return pv[:, o:o + n]

        def pvi(name, *idx):
            o, s = PV_OFF[name]
            flat = 0
            for i, d in zip(idx, s):
                flat = flat * d + i
            return pv[:, o + flat:o + flat + 1]

        P.dma("sp", pv[:], pv_d, "c_pv", w=["pv"])
        P.dma("sp", cm[:], cm_d, "c_cm", w=["cm"])
        P.dma("sp", rw[:], rw_d, "c_rw", w=["rw"])
        P.dma("pool", cmb[:], cmb_d[:, 0:256], "c_cmb", w=["cmb"])

        o_cc = PV_OFF["cc"][0]
        P.op("act", lambda e: e.activation(out=scb[:].rearrange("p a b -> p (a b)"), in_=pv[:, o_cc:o_cc + 16],
                                           func=AF.Silu), r=["pv"], w=["scb"])
        P.op("act", lambda e: e.activation(out=smalltmp[:, 0:16], in_=pvv("lam"), func=AF.Exp, scale=-1.0),
             r=["pv"], w=["smalltmp"])
        P.op("act", lambda e: e.activation(out=smalltmp[:, 0:16], in_=smalltmp[:, 0:16], func=AF.Ln, bias=1.0),
             r=["smalltmp"], w=["smalltmp"])
        P.op("dve", lambda e: e.tensor_scalar(out=hsp[:], in0=smalltmp[:, 0:16], scalar1=-4.0, scalar2=None,
                                              op0=ALU.mult), r=["smalltmp"], w=["hsp"])
        P.op("dve", lambda e: e.tensor_scalar(out=hba[:], in0=pvv("lba"), scalar1=0.5, scalar2=None,
                                              op0=ALU.mult), r=["pv"], w=["hba"])
        P.op("dve", lambda e: e.tensor_scalar(out=hbx[:], in0=pvv("lbx"), scalar1=0.5, scalar2=None,
                                              op0=ALU.mult), r=["pv"], w=["hbx"])

        with contextlib.ExitStack() as s0:
            adaw = [sb(s0, "adaw%d" % i, [128, 8, 256], BF16) for i in range(2)]
            xin = [sb(s0, "xin%d" % i, [128, D], F32) for i in range(2)]
            def load_x(tc):
                k = tc % 2
                src = ctx_d[tc * 128:(tc + 1) * 128, :] if tc < 2 else x_d[(tc - 2) * 128:(tc - 1) * 128, :]
                P.dma("sp", xin[k][:], src, "xin%d" % k, w=[("xin", k)])
                tt = 0 if tc < 2 else 1 + (tc - 2) // 4
                for half in range(2):
                    b = P.nb()
                    for q in range(4):
                        c = half * 4 + q
                        P.op("pe", lambda e: e.transpose(out=ps[b][:, q * 128:(q + 1) * 128],
                                                         in_=xin[k][:, c * 128:(c + 1) * 128], identity=ident),
                             r=[("xin", k), "cm"], w=[psk(b)], inc=(q == 3))
                    eng = "act" if half == 0 else "dve"
                    if eng == "act":
                        P.op("act", lambda e: e.copy(out=xT[:, half * 4:half * 4 + 4, tc * 128:(tc + 1) * 128],
                                                     in_=ps[b][:].rearrange("p (q t) -> p q t", q=4)),
                             r=[psk(b)], w=[("xT", c_, tt) for c_ in range(half * 4, half * 4 + 4)])
                    else:
                        P.op("dve", lambda e: e.tensor_copy(out=xT[:, half * 4:half * 4 + 4, tc * 128:(tc + 1) * 128],
                                                            in_=ps[b][:].rearrange("p (q t) -> p q t", q=4)),
                             r=[psk(b)], w=[("xT", c_, tt) for c_ in range(half * 4, half * 4 + 4)])

            xi = 0
            NPIECE = 24
            mod_state = {}

            def mods_begin(l):
                pm = P.nb()
                P.reserved.add(pm)
                mod_state[l] = pm

            def mods_dma(l, j, bufs):
                k = j % 2
                P.dma("pool", bufs[k][:], adaw_d[l][:, j * 256:(j + 1) * 256].rearrange("(kc p) n -> p kc n", p=128),
                      "adaw%d" % k, w=[("adaw", k)])

            def mods_piece(l, j, bufs, dma=True):
                pm = mod_state[l]
                k = j % 2
                if dma:
                    mods_dma(l, j, bufs)
                for fc in range(2):
                    oc = j * 2 + fc
                    for kc in range(8):
                        P.op("pe", lambda e: e.matmul(ps[pm][:, oc * 2:oc * 2 + 2], lhsT=bufs[k][:, kc, fc * 128:(fc + 1) * 128],
                                                      rhs=scb[:, kc, :], start=(kc == 0), stop=(kc == 7)),
                             r=[("adaw", k), "scb"], w=[psk(pm)], inc=(kc == 7))

            def mods_finish(l, m0=0, m1=6, release=True):
                pm = mod_state[l]
                o_ab = PV_OFF["adab"][0] + l * 48
                for jj in range(2):
                    P.op("dve", lambda e: e.tensor_tensor(out=modv[:, l, m0 * 8:m1 * 8, jj],
                                                          in0=ps[pm][:, 0:96].rearrange("p (a b) -> p a b", b=2)[:, m0 * 8:m1 * 8, jj],
                                                          in1=pv[:, o_ab + m0 * 8:o_ab + m1 * 8], op=ALU.add),
                         r=[psk(pm), "pv"], w=[("modv", m_) for m_ in range(m0, m1)])
                if release:
                    P.reserved.discard(pm)
                for n_, (gname, mi) in enumerate((("n1g", 1), ("n2g", 4))):
                    if not (m0 <= mi < m1):
                        continue
                    og = PV_OFF[gname][0] + l * 8
                    for jj in range(2):
                        P.op("dve", lambda e: e.scalar_tensor_tensor(out=gsv[:, l, n_, :, jj], in0=modv[:, l, mi * 8:(mi + 1) * 8, jj],
                                                                     scalar=1.0, in1=pv[:, og:og + 8], op0=ALU.add, op1=ALU.mult),
                             r=[("modv", mi), "pv"], w=[("gsv", n_)])

            mods_begin(0)
            for j in range(8):
                mods_piece(0, j, adaw)
                for _ in range(2):
                    if xi < NCH:
                        load_x(xi); xi += 1
            mods_finish(0, 0, 2, release=False)
            while xi < NCH:
                load_x(xi); xi += 1
            P.barrier()

        if dbg and stage == 0:
            P.dma("sp", dbgf_d[:, 0:8 * T], xT[:].rearrange("p a b -> p (a b)"), "dbg", r=[("xT", c_, t_) for c_ in range(8) for t_ in range(5)])
            P.dma("sp", out_d[0:128, 0:192], modv[:].rearrange("p a b c -> p (a b c)"), "dbg", r=[("modv", m_) for m_ in range(6)])
            P.dma("sp", out_d[128:256, 0:64], gsv[:].rearrange("p a b c d -> p (a b c d)"), "dbg", r=[("gsv", 0), ("gsv", 1)])

        def xk(cs, tts):
            return [("xT", c_, t_) for c_ in cs for t_ in tts]

        def hk(cs, tts):
            return [("hT", c_, t_) for c_ in cs for t_ in tts]

        def norm_mod(l, nidx, tiles, stk, h2f=None, after_tile=None, post_rstd=None):
            shi = 0 if nidx == 0 else 3
            sq = [sb(stk, "nsq%d_%d_%d" % (l, nidx, i), [128, 512], BF16) for i in range(2)]
            rs = [sb(stk, "nrs%d_%d_%d" % (l, nidx, i), [128, 512], F32) for i in range(2)]
            tmp = [sb(stk, "ntmp%d_%d_%d" % (l, nidx, i), [128, 512], F32) for i in range(2)]
            def stageA(ti, tt):
                s, e_ = TT[tt]
                n = e_ - s
                b = P.nb()
                rk = ("nrs", ti % 2)
                for c in range(8):
                    k = c % 2
                    P.op("act", lambda e: e.activation(out=sq[k][:, :n], in_=xT[:, c, s:e_], func=AF.Square),
                         r=xk([c], [tt]), w=[("nsq", k)])
                    P.op("pe", lambda e: e.matmul(ps[b][:, :n], lhsT=ones_b, rhs=sq[k][:, :n], start=(c == 0), stop=(c == 7)),
                         r=[("nsq", k), "cmb"], w=[psk(b)])
                r_ = rs[ti % 2]
                P.op("act", lambda e: e.activation(out=r_[:, :n], in_=ps[b][:, :n], func=AF.Ln, scale=1.0 / D, bias=EPS),
                     r=[psk(b)], w=[rk])
                P.op("act", lambda e: e.activation(out=r_[:, :n], in_=r_[:, :n], func=AF.Exp, scale=-0.5), r=[rk], w=[rk])
                if post_rstd is not None:
                    post_rstd(ti, tt, r_, rk)

            def stageB(ti, tt):
                s, e_ = TT[tt]
                n = e_ - s
                jj = 1 if tt == 0 else 0
                rk = ("nrs", ti % 2)
                r_ = rs[ti % 2]
                for c in range(8):
                    k = c % 2
                    P.op("dve", lambda e: e.tensor_tensor(out=tmp[k][:, :n], in0=xT[:, c, s:e_], in1=r_[:, :n], op=ALU.mult),
                         r=xk([c], [tt]) + [rk], w=[("ntmp", k)])
                    if h2f is None:
                        P.op("act", lambda e: e.activation(out=hT[:, c, s:e_], in_=tmp[k][:, :n], func=AF.Identity,
                                                           scale=gsv[:, l, nidx, c, jj:jj + 1], bias=modv[:, l, shi * 8 + c, jj:jj + 1]),
                             r=[("ntmp", k), ("gsv", nidx), ("modv", shi)], w=hk([c], [tt]))
                    else:
                        P.op("act", lambda e: e.activation(out=h2f[:, c, :n], in_=tmp[k][:, :n], func=AF.Identity,
                                                           scale=gsv[:, l, nidx, c, jj:jj + 1], bias=modv[:, l, shi * 8 + c, jj:jj + 1]),
                             r=[("ntmp", k), ("gsv", nidx), ("modv", shi)], w=[("h2f", c)])
                        P.op("dve", lambda e: e.tensor_copy(out=hT[:, c, s:e_], in_=h2f[:, c, :n]),
                             r=[("h2f", c)], w=hk([c], [tt]))
                if after_tile is not None:
                    after_tile(tt)

            nt = len(tiles)
            for i in range(nt + 1):
                if i < nt:
                    stageA(i, tiles[i])
                if i - 1 >= 0:
                    stageB(i - 1, tiles[i - 1])

        def dump_and_finish():
            pass

        for l in range(NL):
            if dbg and stage == 0:
                break
            last = (l == NL - 1)
            all_tiles = [0, 1, 2, 3, 4]
            lat_tiles = [1, 2, 3, 4]
            q_tiles = lat_tiles if last else all_tiles
            with contextlib.ExitStack() as sm:
                with contextlib.ExitStack() as sn:
                    norm_mod(l, 0, all_tiles, sn)
                    P.barrier()
                if dbg and stage == 1 and l == 0:
                    P.dma("sp", dbgb_d[:, 0:8 * T], hT[:].rearrange("p a b -> p (a b)"), "dbg", r=hk(range(8), range(5)))
                    break
                NWP = 3
                wp = [sb(sm, "wp%d_%d" % (l, i), [128, 8, 128], BF16) for i in range(NWP)]
                wpc = [0]

                def wp_load(src_list):
                    k = wpc[0] % NWP
                    wpc[0] += 1
                    for (c0, c1, src) in src_list:
                        P.dma("pool", wp[k][:, :, c0:c1], src.rearrange("(kc p) n -> p kc n", p=128), "wp%d" % k, w=[("wp", k)])
                    return k

                def proj(k, tt, b):
                    s, e_ = TT[tt]
                    n = e_ - s
                    for kc in range(8):
                        P.op("pe", lambda e: e.matmul(ps[b][:, :n], lhsT=wp[k][:, kc, :], rhs=hT[:, kc, s:e_], start=(kc == 0), stop=(kc == 7)),
                             r=[("wp", k)] + hk([kc], [tt]), w=[psk(b)], inc=(kc == 7))

                mixR = sb(sm, "mixR%d" % l, [128, 4, T], BF16)
                mixA_box = [None]

                def MX(c_, p0=0, p1=128, c0=None, c1=None):
                    t_ = mixA_box[0] if c_ < 4 else mixR
                    return t_[p0:p1, c_ % 4, c0:c1]

                def mk(cs, tts):
                    return [("mixT", c_, t_) for c_ in cs for t_ in tts]

                with contextlib.ExitStack() as sl:
                    lw = sb(sl, "lw%d" % l, [128, 16, 128], BF16)
                    P.dma("pool", lw[:], lru_d[l].rearrange("p (a m) -> p a m", m=128), "c_lw", w=["lw"])
                    uh = sb(sl, "uh%d" % l, [128, T], F32)
                    cu = sb(sl, "cu%d" % l, [128, T], F32)
                    cub = sb(sl, "cub%d" % l, [128, T], BF16)
                    TA = sb(sl, "TA%d" % l, [128, T], F32)
                    TX = sb(sl, "TX%d" % l, [128, T], F32)
                    NM = sb(sl, "NM%d" % l, [128, T], F32)
                    gl = [sb(sl, "gl%d_%d" % (l, i), [128, 512], F32) for i in range(2)]
                    if l == 0:
                        adawL = [sb(sl, "adawS%d" % i, [128, 8, 256], BF16) for i in range(2)]
                        lp = [8]
                        lq = [8]
                        for _ in range(2):
                            mods_dma(0, lp[0], adawL)
                            lp[0] += 1
                    for c in range(4):
                        if l == 0:
                            for _ in range(4):
                                if lq[0] < NPIECE:
                                    mods_piece(0, lq[0], adawL, dma=False)
                                    lq[0] += 1
                                    if lp[0] < NPIECE:
                                        mods_dma(0, lp[0], adawL)
                                        lp[0] += 1
                        ku = wp_load([(0, 128, win_d[l][:, 768 + c * 128:768 + (c + 1) * 128])])
                        kg_ = wp_load([(0, 128, win_d[l][:, 1280 + c * 128:1280 + (c + 1) * 128])])
                        for tt in all_tiles:
                            s, e_ = TT[tt]
                            b = P.nb()
                            proj(ku, tt, b)
                            P.op("act", lambda e: e.copy(out=uh[:, s:e_], in_=ps[b][:, :e_ - s]), r=[psk(b)], w=["uh"])
                        P.op("dve", lambda e: e.tensor_scalar(out=cu[:, 0:T], in0=uh[:, 0:T], scalar1=pvi("cw", l, 2, c), scalar2=pvi("cb", l, c),
                                                              op0=ALU.mult, op1=ALU.add), r=["uh", "pv"], w=["cu"])
                        for (ga, gz) in ((0, C), (C, T)):
                            P.op("dve", lambda e: e.scalar_tensor_tensor(out=cu[:, ga + 2:gz], in0=uh[:, ga:gz - 2], scalar=pvi("cw", l, 0, c),
                                                                         in1=cu[:, ga + 2:gz], op0=ALU.mult, op1=ALU.add), r=["uh", "cu", "pv"], w=["cu"])
                            P.op("dve", lambda e: e.scalar_tensor_tensor(out=cu[:, ga + 1:gz], in0=uh[:, ga:gz - 1], scalar=pvi("cw", l, 1, c),
                                                                         in1=cu[:, ga + 1:gz], op0=ALU.mult, op1=ALU.add), r=["uh", "cu", "pv"], w=["cu"])
                            P.op("dve", lambda e: e.scalar_tensor_tensor(out=cu[:, ga:gz - 1], in0=uh[:, ga + 1:gz], scalar=pvi("cw", l, 3, c),
                                                                         in1=cu[:, ga:gz - 1], op0=ALU.mult, op1=ALU.add), r=["uh", "cu", "pv"], w=["cu"])
                        P.op("act", lambda e: e.copy(out=cub[:, :], in_=cu[:, :]), r=["cu"], w=["cub"])
                        for d in range(2):
                            li = (l * 2 + d) * 4 + c
                            for tt in all_tiles:
                                s, e_ = TT[tt]
                                n = e_ - s
                                ba_ = P.nb()
                                P.op("pe", lambda e: e.matmul(ps[ba_][:, :n], lhsT=lw[:, (d * 2 + 0) * 4 + c, :], rhs=cub[:, s:e_], start=True, stop=True),
                                     r=["lw", "cub"], w=[psk(ba_)])
                                P.op("act", lambda e: e.activation(out=TA[:, s:e_], in_=ps[ba_][:, :n], func=AF.Tanh, scale=0.5, bias=hba[:, li:li + 1]),
                                     r=[psk(ba_), "hba"], w=["TA"])
                            P.op("act", lambda e: e.activation(out=TA[:, :], in_=TA[:, :], func=AF.Exp, scale=hsp[:, li:li + 1], bias=hsp[:, li:li + 1]),
                                 r=["TA", "hsp"], w=["TA"])
                            P.op("dve", lambda e: e.scalar_tensor_tensor(out=NM[:, :], in0=TA[:, :], scalar=-1.0, in1=TA[:, :], op0=ALU.mult, op1=ALU.mult),
                                 r=["TA"], w=["NM"])
                            for tt in all_tiles:
                                s, e_ = TT[tt]
                                n = e_ - s
                                bx_ = P.nb()
                                P.op("pe", lambda e: e.matmul(ps[bx_][:, :n], lhsT=lw[:, (d * 2 + 1) * 4 + c, :], rhs=cub[:, s:e_], start=True, stop=True),
                                     r=["lw", "cub"], w=[psk(bx_)])
                                P.op("act", lambda e: e.activation(out=TX[:, s:e_], in_=ps[bx_][:, :n], func=AF.Tanh, scale=0.5, bias=hbx[:, li:li + 1]),
                                     r=[psk(bx_), "hbx"], w=["TX"])
                            P.op("act", lambda e: e.activation(out=NM[:, :], in_=NM[:, :], func=AF.Sqrt, scale=0.25, bias=0.25), r=["NM"], w=["NM"])
                            P.op("dve", lambda e: e.scalar_tensor_tensor(out=TX[:, :], in0=TX[:, :], scalar=1.0, in1=cu[:, :], op0=ALU.add, op1=ALU.mult),
                                 r=["TX", "cu"], w=["TX"])
                            P.op("dve", lambda e: e.tensor_tensor(out=TX[:, :], in0=TX[:, :], in1=NM[:, :], op=ALU.mult), r=["TX", "NM"], w=["TX"])
                            if d == 0:
                                P.op("dve", lambda e: e.tensor_tensor_scan(out=uh[:, 0:T], data0=TA[:, 0:T], data1=TX[:, 0:T], initial=0.0,
                                                                           op0=ALU.mult, op1=ALU.add), r=["TA", "TX", "cu"], w=["uh"])
                            else:
                                P.op("dve", lambda e: e.tensor_tensor_scan(out=NM[:, 0:C][:, ::-1], data0=TA[:, 0:C][:, ::-1], data1=TX[:, 0:C][:, ::-1],
                                                                           initial=0.0, op0=ALU.mult, op1=ALU.add), r=["TA", "TX"], w=["NM"])
                                P.op("dve", lambda e: e.tensor_tensor_scan(out=NM[:, C:T][:, ::-1], data0=TA[:, C:T][:, ::-1], data1=TX[:, C:T][:, ::-1],
                                                                           initial=NM[:, 0:1], op0=ALU.mult, op1=ALU.add), r=["TA", "TX", "NM"], w=["NM"])
                                P.op("dve", lambda e: e.tensor_tensor(out=uh[:, :], in0=uh[:, :], in1=NM[:, :], op=ALU.add), r=["uh", "NM"], w=["uh"])
                        for ti, tt in enumerate(q_tiles):
                            s, e_ = TT[tt]
                            n = e_ - s
                            b = P.nb()
                            proj(kg_, tt, b)
                            g_ = gl[ti % 2]
                            P.op("act", lambda e: e.activation(out=g_[:, :n], in_=ps[b][:, :n], func=AF.Gelu_apprx_tanh), r=[psk(b)], w=[("gl", ti % 2)])
                            P.op("dve", lambda e: e.tensor_tensor(out=MX(4 + c, c0=s, c1=e_), in0=uh[:, s:e_], in1=g_[:, :n], op=ALU.mult),
                                 r=["uh", ("gl", ti % 2)], w=mk([4 + c], [tt]))
                    if l == 0:
                        assert lq[0] == NPIECE
                        mods_finish(0, 2, 6)
                    P.barrier()

                if dbg and stage == 2 and l == 0:
                    (mixA_box[0] is not None and P.dma("sp", dbgb_d[:, 0:4 * T], mixA_box[0][:].rearrange("p a b -> p (a b)"), "dbg", r=mk(range(4), range(5)))); P.dma("sp", dbgb_d[:, 4 * T:8 * T], mixR[:].rearrange("p a b -> p (a b)"), "dbg", r=mk(range(4, 8), range(5)))
                    break

                mixA_box[0] = sb(sm, "mixA%d" % l, [128, 4, T], BF16)
                with contextlib.ExitStack() as sa:
                    ropeT = sb(sa, "rope%d" % l, [128, 2, S], BF16)
                    for a_ in range(2):
                        P.dma("pool", ropeT[:, a_, :], rope_d[a_], "c_rope", w=["rope"], max_dma_last_dim=4096)
                    ATT_STOP = int(os.environ.get("ATT_STOP", "9"))
                    kT2 = sb(sa, "kT2_%d" % l, [128, 2, T], BF16)
                    vaug = sb(sa, "vaug%d" % l, [128, NCH, 2, 128], BF16)
                    qZ = [sb(sa, "qZ%d_%d" % (l, i), [128, T], BF16) for i in range(2)]
                    NPT = 3
                    pt = [sb(sa, "pt%d_%d" % (l, i), [128, 512], BF16) for i in range(NPT)]
                    rc = [sb(sa, "rc%d_%d" % (l, i), [64, 512], F32) for i in range(2)]
                    sqh = sb(sa, "sqh%d" % l, [128, 512], BF16)
                    rsh = sb(sa, "rsh%d" % l, [128, 512], F32)
                    qn = sb(sa, "qn%d" % l, [128, 512], F32)
                    P.op("dve", lambda e: e.memset(vaug[:, :, :, 64:128], 1.0), w=["vones"])
                    P.op("dve", lambda e: e.memset(qZ[0][64:128, :], 0.0), w=["qz0"])
                    P.op("dve", lambda e: e.memset(qZ[1][0:64, :], 0.0), w=["qz1"])

                    sqh2 = [sqh, sb(sa, "sqhB%d" % l, [128, 512], BF16)]
                    rsh2 = [rsh, sb(sa, "rshB%d" % l, [128, 512], F32)]
                    qn2 = [qn, sb(sa, "qnB%d" % l, [128, 512], F32)]
                    hnc = [0]

                    def pnr_pipeline(items):
                        st = {}
                        n_it = len(items)
                        for i in range(n_it + 2):
                            if i < n_it:
                                k_, tt, gname, dst, dkeys = items[i]
                                b = P.nb()
                                proj(k_, tt, b)
                                st[i] = dict(b=b)
                            if 0 <= i - 1 < n_it:
                                k_, tt, gname, dst, dkeys = items[i - 1]
                                s, e_ = TT[tt]
                                n = e_ - s
                                b = st[i - 1]["b"]
                                u_ = hnc[0] % 2
                                hnc[0] += 1
                                st[i - 1]["u"] = u_
                                sq_, rs_, q_ = sqh2[u_], rsh2[u_], qn2[u_]
                                P.op("act", lambda e: e.activation(out=sq_[:, :n], in_=ps[b][:, :n], func=AF.Square), r=[psk(b)], w=[("sqh", u_)])
                                b2 = P.nb()
                                P.op("pe", lambda e: e.matmul(ps[b2][:, :n], lhsT=bones_b, rhs=sq_[:, :n], start=True, stop=True), r=[("sqh", u_), "cmb"], w=[psk(b2)])
                                P.op("act", lambda e: e.activation(out=rs_[:, :n], in_=ps[b2][:, :n], func=AF.Ln, scale=1.0 / 64, bias=EPS), r=[psk(b2)], w=[("rsh", u_)])
                                P.op("act", lambda e: e.activation(out=rs_[:, :n], in_=rs_[:, :n], func=AF.Exp, scale=-0.5), r=[("rsh", u_)], w=[("rsh", u_)])
                                P.op("dve", lambda e: e.scalar_tensor_tensor(out=q_[:, :n], in0=ps[b][:, :n], scalar=pvi(gname, l), in1=rs_[:, :n],
                                                                             op0=ALU.mult, op1=ALU.mult), r=[psk(b), ("rsh", u_), "pv"], w=[("qn", u_)])
                                if tt > 0:
                                    b3 = P.nb()
                                    P.op("pe", lambda e: e.matmul(ps[b3][:, :n], lhsT=perm, rhs=q_[:, :n], start=True, stop=True), r=[("qn", u_), "cm"], w=[psk(b3)])
                                    st[i - 1]["b3"] = b3
                            if 0 <= i - 2 < n_it:
                                k_, tt, gname, dst, dkeys = items[i - 2]
                                s, e_ = TT[tt]
                                n = e_ - s
                                u_ = st[i - 2]["u"]
                                sq_, rs_, q_ = sqh2[u_], rsh2[u_], qn2[u_]
                                if tt > 0:
                                    b3 = st[i - 2]["b3"]
                                    P.op("dve", lambda e: e.tensor_tensor(out=q_[:, :n], in0=q_[:, :n], in1=ropeT[:, 0, s - C:e_ - C], op=ALU.mult),
                                         r=[("qn", u_), "rope"], w=[("qn", u_)])
                                    P.op("dve", lambda e: e.tensor_tensor(out=rs_[:, :n], in0=ps[b3][:, :n], in1=ropeT[:, 1, s - C:e_ - C], op=ALU.mult),
                                         r=[psk(b3), "rope"], w=[("rsh", u_)])
                                    for (p0, p1, d_) in dst:
                                        P.op("dve", lambda e: e.tensor_tensor(out=d_, in0=q_[p0:p1, :n], in1=rs_[p0:p1, :n], op=ALU.add),
                                             r=[("qn", u_), ("rsh", u_)], w=dkeys)
                                else:
                                    for (p0, p1, d_) in dst:
                                        P.op("act", lambda e: e.copy(out=d_, in_=q_[p0:p1, :n]), r=[("qn", u_)], w=dkeys)

                    k_items = []
                    for kvh in range(2):
                        c0 = 512 + kvh * 64
                        kk = wp_load([(0, 64, win_d[l][:, c0:c0 + 64]), (64, 128, win_d[l][:, c0:c0 + 64])])
                        for tt in all_tiles:
                            s, e_ = TT[tt]
                            k_items.append((kk, tt, "kg", [(0, 128, kT2[:, kvh, s:e_])], [("kT2", kvh, tt)]))
                    pnr_pipeline(k_items)
                    kv_ = wp_load([(0, 128, win_d[l][:, 640:768])])
                    for g4 in range(5):
                        chunks = list(range(g4 * 4, min(g4 * 4 + 4, NCH)))
                        b = P.nb()
                        for i, tc in enumerate(chunks):
                            tt_ = 0 if tc < 2 else 1 + (tc - 2) // 4
                            for kc in range(8):
                                P.op("pe", lambda e: e.matmul(ps[b][:, i * 128:(i + 1) * 128], lhsT=hT[:, kc, tc * 128:(tc + 1) * 128], rhs=wp[kv_][:, kc, :],
                                                              start=(kc == 0), stop=(kc == 7)),
                                     r=[("wp", kv_)] + hk([kc], [tt_]), w=[psk(b)], inc=(kc == 7 and i == len(chunks) - 1))
                        nch = len(chunks)
                        src = ps[b][:, 0:nch * 128].rearrange("p (i k d) -> p i k d", i=nch, k=2)
                        tc0 = chunks[0]
                        P.op("act", lambda e: e.copy(out=vaug[:, tc0:tc0 + nch, :, 0:64], in_=src), r=[psk(b)], w=[("vaug", g4, 0)])
                    vkeys = [("vaug", g4, 0) for g4 in range(5)] + ["vones"]

                    if ATT_STOP <= 2:
                        P.barrier(); break
                    SB = [0, 1, 2, 3]
                    sbc = [0]
                    ptc = [0]
                    qtc = [0]
                    for c in range(4):
                        kvh = c // 2
                        kq = wp_load([(0, 128, win_d[l][:, c * 128:(c + 1) * 128])])
                        pnr_pipeline([(kq, tt, "qg", [(0, 64, qZ[0][0:64, TT[tt][0]:TT[tt][1]]), (64, 128, qZ[1][64:128, TT[tt][0]:TT[tt][1]])], [("qT", tt)])
                                      for tt in q_tiles])
                        if ATT_STOP <= 3:
                            P.barrier(); break
                        for tt in q_tiles:
                            s, e_ = TT[tt]
                            n = e_ - s
                            kchunks = [0, 1] if tt == 0 else list(range(NCH))
                            po = (4, 5) if qtc[0] % 2 == 0 else (6, 7)
                            qtc[0] += 1
                            steps = [(j, kc) for kc in kchunks for j in range(2)]
                            pend = []

                            def emit_qk(j, kc):
                                sbk = SB[sbc[0] % 4]
                                sbc[0] += 1
                                ktt = 0 if kc < 2 else 1 + (kc - 2) // 4
                                P.op("pe", lambda e: e.matmul(ps[sbk][:, :n], lhsT=kT2[:, kvh, kc * 128:(kc + 1) * 128],
                                                              rhs=qZ[j][:, s:e_], start=True, stop=True),
                                     r=[("kT2", kvh, ktt), ("qT", tt), "qz0", "qz1"], w=[psk(sbk)])
                                return sbk

                            def emit_pv(j, kc, sbk, first, lastk):
                                pk = ptc[0] % NPT
                                ptc[0] += 1
                                P.op("act", lambda e: e.activation(out=pt[pk][:, :n], in_=ps[sbk][:, :n], func=AF.Exp, scale=0.125),
                                     r=[psk(sbk)], w=[("pt", pk)])
                                P.op("pe", lambda e: e.matmul(ps[po[j]][:, :n], lhsT=vaug[:, kc, kvh, :], rhs=pt[pk][:, :n],
                                                              start=first, stop=lastk),
                                     r=[("pt", pk)] + vkeys, w=[psk(po[j])])

                            LOOK = 2
                            for i, (j, kc) in enumerate(steps):
                                pend.append((j, kc, emit_qk(j, kc)))
                                if len(pend) > LOOK:
                                    pj, pkc, psb = pend.pop(0)
                                    emit_pv(pj, pkc, psb, pkc == kchunks[0], pkc == kchunks[-1])
                            while pend:
                                pj, pkc, psb = pend.pop(0)
                                emit_pv(pj, pkc, psb, pkc == kchunks[0], pkc == kchunks[-1])
                            for j in range(2):
                                P.op("dve", lambda e: e.reciprocal(out=rc[j][0:64, :n], in_=ps[po[j]][64:128, :n]), r=[psk(po[j])], w=[("rc", j)])
                                P.op("dve", lambda e: e.tensor_tensor(out=MX(c, j * 64, j * 64 + 64, s, e_), in0=ps[po[j]][0:64, :n], in1=rc[j][0:64, :n], op=ALU.mult),
                                     r=[psk(po[j]), ("rc", j)], w=mk([c], [tt]))
                    P.barrier()

                if dbg and stage == 3 and l == 0:
                    (mixA_box[0] is not None and P.dma("sp", dbgb_d[:, 0:4 * T], mixA_box[0][:].rearrange("p a b -> p (a b)"), "dbg", r=mk(range(4), range(5)))); P.dma("sp", dbgb_d[:, 4 * T:8 * T], mixR[:].rearrange("p a b -> p (a b)"), "dbg", r=mk(range(4, 8), range(5)))
                    break

                with contextlib.ExitStack() as so:
                    osq = [sb(so, "osq%d_%d" % (l, i), [128, 512], BF16) for i in range(2)]
                    ors = [sb(so, "ors%d_%d" % (l, i), [128, 512], F32) for i in range(2)]
                    oi = 0
                    for half, gname in ((0, "aog"), (1, "log")):
                        for tt in q_tiles:
                            s, e_ = TT[tt]
                            n = e_ - s
                            b = P.nb()
                            for c4 in range(4):
                                cc_ = half * 4 + c4
                                k = c4 % 2
                                P.op("act", lambda e: e.activation(out=osq[k][:, :n], in_=MX(cc_, c0=s, c1=e_), func=AF.Square), r=mk([cc_], [tt]), w=[("osq", k)])
                                P.op("pe", lambda e: e.matmul(ps[b][:, :n], lhsT=ones_b, rhs=osq[k][:, :n], start=(c4 == 0), stop=(c4 == 3)),
                                     r=[("osq", k), "cmb"], w=[psk(b)])
                            r_ = ors[oi % 2]
                            rk = ("ors", oi % 2)
                            oi += 1
                            P.op("act", lambda e: e.activation(out=r_[:, :n], in_=ps[b][:, :n], func=AF.Ln, scale=1.0 / 512, bias=EPS), r=[psk(b)], w=[rk])
                            P.op("act", lambda e: e.activation(out=r_[:, :n], in_=r_[:, :n], func=AF.Exp, scale=-0.5), r=[rk], w=[rk])
                            for c4 in range(4):
                                cc_ = half * 4 + c4
                                P.op("dve", lambda e: e.scalar_tensor_tensor(out=MX(cc_, c0=s, c1=e_), in0=MX(cc_, c0=s, c1=e_), scalar=pvi(gname, l, c4),
                                                                             in1=r_[:, :n], op0=ALU.mult, op1=ALU.mult), r=mk([cc_], [tt]) + [rk, "pv"], w=mk([cc_], [tt]))
                    if dbg and stage == 4 and l == 0:
                        (mixA_box[0] is not None and P.dma("sp", dbgb_d[:, 0:4 * T], mixA_box[0][:].rearrange("p a b -> p (a b)"), "dbg", r=mk(range(4), range(5)))); P.dma("sp", dbgb_d[:, 4 * T:8 * T], mixR[:].rearrange("p a b -> p (a b)"), "dbg", r=mk(range(4, 8), range(5)))
                    for o in range(8):
                        ko = wp_load([(0, 128, wout_d[l][:, o * 128:(o + 1) * 128])])
                        for tt in q_tiles:
                            s, e_ = TT[tt]
                            n = e_ - s
                            jj = 1 if tt == 0 else 0
                            b = P.nb()
                            for kc in range(8):
                                P.op("pe", lambda e: e.matmul(ps[b][:, :n], lhsT=wp[ko][:, kc, :], rhs=MX(kc, c0=s, c1=e_), start=(kc == 0), stop=(kc == 7)),
                                     r=[("wp", ko)] + mk([kc], [tt]), w=[psk(b)], inc=(kc == 7))
                            P.op("dve", lambda e: e.scalar_tensor_tensor(out=xT[:, o, s:e_], in0=ps[b][:, :n], scalar=modv[:, l, 2 * 8 + o, jj:jj + 1],
                                                                         in1=xT[:, o, s:e_], op0=ALU.mult, op1=ALU.add),
                                 r=[psk(b), ("modv", 2)] + xk([o], [tt]), w=xk([o], [tt]))
                    P.barrier()
            if dbg and stage in (1, 2, 3) and l == 0:
                break
            if dbg and stage == 4 and l == 0:
                P.dma("sp", dbgf_d[:, 0:8 * T], xT[:].rearrange("p a b -> p (a b)"), "dbg", r=xk(range(8), range(5)))
                break

            with contextlib.ExitStack() as se:
                NEP = 6
                ep = [sb(se, "ep%d_%d" % (l, i), [128, 4096], BF16) for i in range(NEP)]
                epc = [0]
                sel = sb(se, "sel%d" % l, [128, 16, 128], BF16)
                P.dma("pool", sel[:], cmb_d[:, 256:256 + 2048].rearrange("p (a m) -> p a m", m=128), "c_sel", w=["sel"])
                combT = sb(se, "combT%d" % l, [128, T], BF16)
                cbt = sb(se, "cbt%d" % l, [128, NCH, 16], F32)

                def ep_load(e_i):
                    ks = []
                    for wi, (wd_, pat) in enumerate(((wg_d, "g"), (wu_d, "u"), (wd_d, "d"))):
                        k = epc[0] % NEP
                        epc[0] += 1
                        if pat == "d":
                            P.dma("pool", ep[k][:].rearrange("p (j n) -> p j n", j=4), wd_[l][e_i].rearrange("(j p) n -> p j n", p=128), "ep%d" % k, w=[("ep", k)])
                        else:
                            P.dma("pool", ep[k][:].rearrange("p (j n) -> p j n", j=8), wd_[l][e_i].rearrange("(j p) n -> p j n", p=128), "ep%d" % k, w=[("ep", k)])
                        ks.append(k)
                    return ks

                m_tiles = lat_tiles if last else all_tiles
                ch0 = 2 if last else 0
                nchs = NCH - ch0
                eks = {0: ep_load(0)}
                with contextlib.ExitStack() as sg:
                    sg2 = contextlib.ExitStack()
                    rl = P.nb()
                    P.reserved.add(rl)
                    rwg = sb(sg2, "rwg%d" % l, [128, 2, 8, 16], F32)
                    cst = sb(sg2, "cst%d" % l, [16, 2], F32)
                    lgt = [sb(sg2, "lgt%d_%d" % (l, i), [16, 512], F32) for i in range(2)]
                    eT = sb(sg2, "eT%d" % l, [16, T], F32)
                    for jj in range(2):
                        for kc in range(8):
                            P.op("dve", lambda e: e.tensor_scalar(out=rwg[:, jj, kc, :], in0=rw[:, kc * 16:(kc + 1) * 16], scalar1=gsv[:, l, 1, kc, jj:jj + 1],
                                                                  scalar2=None, op0=ALU.mult), r=["rw", ("gsv", 1)], w=["rwg"])
                    bc = P.nb()
                    for kc in range(8):
                        P.op("pe", lambda e: e.matmul(ps[bc][0:16, 0:2], lhsT=rw[:, kc * 16:(kc + 1) * 16], rhs=modv[:, l, 3 * 8 + kc, :], start=(kc == 0), stop=(kc == 7)),
                             r=["rw", ("modv", 3)], w=[psk(bc)], inc=(kc == 7))
                    P.op("dve", lambda e: e.tensor_scalar(out=cst[:, :], in0=ps[bc][0:16, 0:2], scalar1=-1.0, scalar2=None, op0=ALU.mult), r=[psk(bc)], w=["cst"])

                    def router_tile(ti, tt, r_, rk):
                        s, e_ = TT[tt]
                        n = e_ - s
                        jj = 1 if tt == 0 else 0
                        b = P.nb()
                        for kc in range(8):
                            P.op("pe", lambda e: e.matmul(ps[b][0:16, :n], lhsT=rwg[:, jj, kc, :], rhs=xT[:, kc, s:e_], start=(kc == 0), stop=(kc == 7)),
                                 r=["rwg"] + xk([kc], [tt]), w=[psk(b)], inc=(kc == 7))
                        lg_ = lgt[ti % 2]
                        def fin():
                            P.op("dve", lambda e: e.tensor_tensor(out=lg_[:, :n], in0=ps[b][0:16, :n], in1=r_[0:16, :n], op=ALU.mult), r=[psk(b), rk], w=[("lgt", ti % 2)])
                            P.op("act", lambda e: e.activation(out=eT[:, s:e_], in_=lg_[:, :n], func=AF.Exp, scale=-1.0, bias=cst[:, jj:jj + 1]),
                                 r=[("lgt", ti % 2), "cst"], w=[("eT", tt)])
                        rdefer.append(fin)

                    rdefer = []

                    def flush_router(tt):
                        while rdefer:
                            rdefer.pop(0)()

                    norm_mod(l, 1, m_tiles, sg2, post_rstd=router_tile, after_tile=flush_router)
                    flush_router(None)
                    for ch in range(ch0, NCH):
                        tt_ = 0 if ch < 2 else 1 + (ch - 2) // 4
                        P.op("pe", lambda e: e.transpose(out=ps[rl][:, ch * 16:(ch + 1) * 16], in_=eT[0:16, ch * 128:(ch + 1) * 128], identity=ident[0:16, 0:16]),
                             r=[("eT", tt_), "cm"], w=[psk(rl)], inc=(ch == NCH - 1))
                    P.barrier()
                    sg2.close()

                    def rt(name):
                        return sb(sg, "%s_%d" % (name, l), [128, NCH * 16], F32)

                    sc = rt("r_sc"); bi = rt("r_bi"); selm = rt("r_sel")
                    m1 = rt("r_m1"); m2 = rt("r_m2"); mn = rt("r_mn"); gsum = rt("r_gs"); gmax = rt("r_gm"); ing = rt("r_in"); den = rt("r_den")
                    lo, hi = ch0 * 16, NCH * 16
                    nq = nchs * 4
                    P.op("dve", lambda e: e.tensor_scalar(out=sc[:, lo:hi], in0=ps[rl][:, lo:hi], scalar1=1.0, scalar2=None, op0=ALU.add), r=[psk(rl)], w=["r_sc"])
                    P.op("dve", lambda e: e.reciprocal(out=sc[:, lo:hi], in_=sc[:, lo:hi]), r=["r_sc"], w=["r_sc"])
                    orb = PV_OFF["rb"][0]
                    P.op("dve", lambda e: e.tensor_tensor(out=bi[:, lo:hi], in0=sc[:, lo:hi], in1=pv[:, orb + lo:orb + hi], op=ALU.add), r=["r_sc", "pv"], w=["r_bi"])
                    g4v = bi[:, lo:hi].rearrange("p (q i) -> p q i", i=4)
                    P.op("dve", lambda e: e.tensor_reduce(out=m1[:, 0:nq], in_=g4v, axis=AX.X, op=ALU.max), r=["r_bi"], w=["r_m1"])
                    first = True
                    for i in range(4):
                        for j in range(i + 1, 4):
                            if first:
                                P.op("dve", lambda e: e.tensor_tensor(out=m2[:, 0:nq], in0=g4v[:, :, i], in1=g4v[:, :, j], op=ALU.min), r=["r_bi"], w=["r_m2"])
                                first = False
                            else:
                                P.op("dve", lambda e: e.tensor_tensor(out=mn[:, 0:nq], in0=g4v[:, :, i], in1=g4v[:, :, j], op=ALU.min), r=["r_bi"], w=["r_mn"])
                                P.op("dve", lambda e: e.tensor_tensor(out=m2[:, 0:nq], in0=m2[:, 0:nq], in1=mn[:, 0:nq], op=ALU.max), r=["r_m2", "r_mn"], w=["r_m2"])
                    P.op("dve", lambda e: e.tensor_tensor(out=gsum[:, 0:nq], in0=m1[:, 0:nq], in1=m2[:, 0:nq], op=ALU.add), r=["r_m1", "r_m2"], w=["r_gs"])
                    gsv3 = gsum[:, 0:nq].rearrange("p (c g) -> p c g", g=4)
                    P.op("dve", lambda e: e.tensor_reduce(out=gmax[:, 0:nchs], in_=gsv3, axis=AX.X, op=ALU.max), r=["r_gs"], w=["r_gm"])
                    ing3 = ing[:, 0:nq].rearrange("p (c g) -> p c g", g=4)
                    for g in range(4):
                        P.op("dve", lambda e: e.tensor_tensor(out=ing3[:, :, g], in0=gsv3[:, :, g], in1=gmax[:, 0:nchs], op=ALU.is_ge), r=["r_gs", "r_gm"], w=["r_in"])
                    sel3 = selm[:, lo:hi].rearrange("p (q i) -> p q i", i=4)
                    for i in range(4):
                        P.op("dve", lambda e: e.tensor_tensor(out=sel3[:, :, i], in0=g4v[:, :, i], in1=m2[:, 0:nq], op=ALU.is_ge), r=["r_bi", "r_m2"], w=["r_sel"])
                        P.op("dve", lambda e: e.tensor_tensor(out=sel3[:, :, i], in0=sel3[:, :, i], in1=ing[:, 0:nq], op=ALU.mult), r=["r_sel", "r_in"], w=["r_sel"])
                    P.op("dve", lambda e: e.tensor_tensor(out=selm[:, lo:hi], in0=selm[:, lo:hi], in1=sc[:, lo:hi], op=ALU.mult), r=["r_sel", "r_sc"], w=["r_sel"])
                    P.op("dve", lambda e: e.tensor_reduce(out=den[:, 0:nchs], in_=selm[:, lo:hi].rearrange("p (c x) -> p c x", x=16), axis=AX.X, op=ALU.add),
                         r=["r_sel"], w=["r_den"])
                    P.op("dve", lambda e: e.reciprocal(out=den[:, 0:nchs], in_=den[:, 0:nchs]), r=["r_den"], w=["r_den"])
                    for ci in range(nchs):
                        ch = ch0 + ci
                        P.op("dve", lambda e: e.tensor_scalar(out=cbt[:, ch, :], in0=selm[:, ch * 16:(ch + 1) * 16], scalar1=den[:, ci:ci + 1], scalar2=None, op0=ALU.mult),
                             r=["r_sel", "r_den"], w=[("cbt", ch)])
                    for g4 in range((nchs + 3) // 4):
                        chs = list(range(ch0 + g4 * 4, min(ch0 + g4 * 4 + 4, NCH)))
                        b = P.nb()
                        for i, ch in enumerate(chs):
                            P.op("pe", lambda e: e.transpose(out=ps[b][0:16, i * 128:(i + 1) * 128], in_=cbt[:, ch, :], identity=ident),
                                 r=[("cbt", ch), "cm"], w=[psk(b)], inc=(i == len(chs) - 1))
                        P.op("act", lambda e: e.copy(out=combT[0:16, chs[0] * 128:(chs[-1] + 1) * 128], in_=ps[b][0:16, 0:len(chs) * 128]), r=[psk(b)], w=["combT"])
                    P.reserved.discard(rl)
                    P.barrier()

                if dbg and stage == 5 and l == 0:
                    P.dma("sp", dbgb_d[:, 0:8 * T], hT[:].rearrange("p a b -> p (a b)"), "dbg", r=hk(range(8), range(5)))
                    P.dma("sp", dbgf_d[:, 0:NCH * 16], cbt[:].rearrange("p a b -> p (a b)"), "dbg", r=[("cbt", ch) for ch in range(NCH)])
                    break

                with contextlib.ExitStack() as sh:
                    cbe = [sb(sh, "cbe%d_%d" % (l, i), [128, T], BF16) for i in range(2)]
                    sgt = [sb(sh, "sgt%d_%d" % (l, i), [128, 512], BF16) for i in range(2)]
                    hid = [sb(sh, "hid%d_%d" % (l, i), [128, 4, 512], BF16) for i in range(2)]
                    sgc = [0]
                    hdc = [0]
                    if l + 1 < NL:
                        adaw2 = [sb(sh, "adawL%d_%d" % (l, i), [128, 8, 256], BF16) for i in range(2)]
                        mods_begin(l + 1)
                        mp = [0]
                        mq = [0]
                    for ei in range(NE):
                        if ei + 1 < NE:
                            eks[ei + 1] = ep_load(ei + 1)
                        if l + 1 < NL:
                            while mq[0] < mp[0]:
                                mods_piece(l + 1, mq[0], adaw2, dma=False)
                                mq[0] += 1
                            for _ in range(2):
                                if mp[0] < NPIECE:
                                    mods_dma(l + 1, mp[0], adaw2)
                                    mp[0] += 1
                        kg_, ku_, kd_ = eks[ei]
                        wgv = ep[kg_][:].rearrange("p (j n) -> p j n", j=8)
                        wuv = ep[ku_][:].rearrange("p (j n) -> p j n", j=8)
                        wdv = ep[kd_][:].rearrange("p (j n) -> p j n", j=4)
                        cb_ = cbe[ei % 2]
                        ck = ("cbe", ei % 2)
                        for tt in m_tiles:
                            s, e_ = TT[tt]
                            n = e_ - s
                            b = P.nb()
                            P.op("pe", lambda e: e.matmul(ps[b][:, :n], lhsT=sel[0:16, ei, :], rhs=combT[0:16, s:e_], start=True, stop=True),
                                 r=["sel", "combT"], w=[psk(b)])
                            P.op("act", lambda e: e.copy(out=cb_[:, s:e_], in_=ps[b][:, :n]), r=[psk(b)], w=[ck])
                        for tt in m_tiles:
                            s, e_ = TT[tt]
                            n = e_ - s
                            jj = 1 if tt == 0 else 0
                            hd = hid[hdc[0] % 2]
                            hkey = ("hid", hdc[0] % 2)
                            hdc[0] += 1
                            for f in range(4):
                                bg = P.nb()
                                for kc in range(8):
                                    P.op("pe", lambda e: e.matmul(ps[bg][:, :n], lhsT=wgv[:, kc, f * 128:(f + 1) * 128], rhs=hT[:, kc, s:e_], start=(kc == 0), stop=(kc == 7)),
                                         r=[("ep", kg_)] + hk([kc], [tt]), w=[psk(bg)], inc=(kc == 7))
                                bu = P.nb()
                                for kc in range(8):
                                    P.op("pe", lambda e: e.matmul(ps[bu][:, :n], lhsT=wuv[:, kc, f * 128:(f + 1) * 128], rhs=hT[:, kc, s:e_], start=(kc == 0), stop=(kc == 7)),
                                         r=[("ep", ku_)] + hk([kc], [tt]), w=[psk(bu)], inc=(kc == 7))
                                sk = sgc[0] % 2
                                sgc[0] += 1
                                P.op("act", lambda e: e.activation(out=sgt[sk][:, :n], in_=ps[bg][:, :n], func=AF.Silu), r=[psk(bg)], w=[("sgt", sk)])
                                P.op("dve", lambda e: e.tensor_tensor(out=sgt[sk][:, :n], in0=sgt[sk][:, :n], in1=cb_[:, s:e_], op=ALU.mult), r=[("sgt", sk), ck], w=[("sgt", sk)])
                                P.op("dve", lambda e: e.tensor_tensor(out=hd[:, f, :n], in0=ps[bu][:, :n], in1=sgt[sk][:, :n], op=ALU.mult), r=[psk(bu), ("sgt", sk)], w=[hkey])
                            for o in range(8):
                                bo = P.nb()
                                for jc in range(4):
                                    P.op("pe", lambda e: e.matmul(ps[bo][:, :n], lhsT=wdv[:, jc, o * 128:(o + 1) * 128], rhs=hd[:, jc, :n], start=(jc == 0), stop=(jc == 3)),
                                         r=[("ep", kd_), hkey], w=[psk(bo)], inc=(jc == 3))
                                P.op("dve", lambda e: e.scalar_tensor_tensor(out=xT[:, o, s:e_], in0=ps[bo][:, :n], scalar=modv[:, l, 5 * 8 + o, jj:jj + 1],
                                                                             in1=xT[:, o, s:e_], op0=ALU.mult, op1=ALU.add),
                                     r=[psk(bo), ("modv", 5)] + xk([o], [tt]), w=xk([o], [tt]))
                    if l + 1 < NL:
                        assert mp[0] == NPIECE
                        while mq[0] < mp[0]:
                            mods_piece(l + 1, mq[0], adaw2, dma=False)
                            mq[0] += 1
                        mods_finish(l + 1)
                    P.barrier()
            if dbg and stage == 5 and l == 0:
                break
            if dbg and stage == 6 and l == 0:
                P.dma("sp", dbgf_d[:, 0:8 * T], xT[:].rearrange("p a b -> p (a b)"), "dbg", r=xk(range(8), range(5)))
                break

        if not dbg or stage >= 99:
            with contextlib.ExitStack() as sf:
                fsq = [sb(sf, "fsq%d" % i, [128, 512], BF16) for i in range(2)]
                frs = [sb(sf, "frs%d" % i, [128, 512], F32) for i in range(2)]
                yT = [sb(sf, "yT%d" % i, [128, 8, 512], F32) for i in range(2)]
                ob = [sb(sf, "ob%d" % i, [128, D], F32) for i in range(2)]
                obc = 0
                ofg = PV_OFF["fg"][0]
                for ti, tt in enumerate([1, 2, 3, 4]):
                    s, e_ = TT[tt]
                    n = e_ - s
                    b = P.nb()
                    for c in range(8):
                        k = c % 2
                        P.op("act", lambda e: e.activation(out=fsq[k][:, :n], in_=xT[:, c, s:e_], func=AF.Square), r=xk([c], [tt]), w=[("fsq", k)])
                        P.op("pe", lambda e: e.matmul(ps[b][:, :n], lhsT=ones_b, rhs=fsq[k][:, :n], start=(c == 0), stop=(c == 7)),
                             r=[("fsq", k), "cmb"], w=[psk(b)])
                    r_ = frs[ti % 2]
                    rk = ("frs", ti % 2)
                    P.op("act", lambda e: e.activation(out=r_[:, :n], in_=ps[b][:, :n], func=AF.Ln, scale=1.0 / D, bias=EPS), r=[psk(b)], w=[rk])
                    P.op("act", lambda e: e.activation(out=r_[:, :n], in_=r_[:, :n], func=AF.Exp, scale=-0.5), r=[rk], w=[rk])
                    y_ = yT[ti % 2]
                    for c in range(8):
                        P.op("dve", lambda e: e.scalar_tensor_tensor(out=y_[:, c, :n], in0=xT[:, c, s:e_], scalar=pv[:, ofg + c:ofg + c + 1], in1=r_[:, :n],
                                                                     op0=ALU.mult, op1=ALU.mult), r=xk([c], [tt]) + [rk, "pv"], w=[("yT", ti % 2, c)])
                    for i in range(4):
                        k = obc % 2
                        obc += 1
                        for half in range(2):
                            b2 = P.nb()
                            for q in range(4):
                                c = half * 4 + q
                                P.op("pe", lambda e: e.transpose(out=ps[b2][:, q * 128:(q + 1) * 128], in_=y_[:, c, i * 128:(i + 1) * 128], identity=ident),
                                     r=[("yT", ti % 2, c), "cm"], w=[psk(b2)], inc=(q == 3))
                            if half == 0:
                                P.op("act", lambda e: e.copy(out=ob[k][:, 0:512], in_=ps[b2][:, :]), r=[psk(b2)], w=[("ob", k, 0)])
                            else:
                                P.op("dve", lambda e: e.tensor_copy(out=ob[k][:, 512:1024], in_=ps[b2][:, :]), r=[psk(b2)], w=[("ob", k, 1)])
                        tok = (s - C) + i * 128
                        P.dma("sp", out_d[tok:tok + 128, :], ob[k][:], "o%d" % k, r=[("ob", k, 0), ("ob", k, 1)])
                P.barrier()
        for k, v in P.cnt.items():
            if k not in P.E and v > 0:
                P.E["sp"].wait_ge(P.sem[k], v)
                P._pw["sp"].append((k, v))
        P.check_deadlock()
    return nc


def _chunked(v):
    v = np.asarray(v, np.float32)
    k = v.shape[-1] // 128
    v = v.reshape(v.shape[:-1] + (k, 128))
    return np.moveaxis(v, -1, 0)


def _rope_tables():
    t = np.arange(S)
    r = (t // 64).astype(np.float32)
    col = (t % 64).astype(np.float32)
    inv = (np.float32(10000.0) ** (-np.arange(0, 32, 2, dtype=np.float32) / np.float32(32))).astype(np.float32)
    cosT = np.zeros((128, S), np.float32)
    sinT = np.zeros((128, S), np.float32)
    for p in range(128):
        d = p % 64
        axis, half, f = d // 32, (d % 32) // 16, d % 16
        pos = r if axis == 0 else col
        ang = (pos * inv[f]).astype(np.float32)
        cosT[p] = np.cos(ang)
        sinT[p] = np.sin(ang) * (-1.0 if half == 0 else 1.0)
    return np.stack([cosT, sinT]).astype(np.float32)


def _consts():
    ident = np.eye(128, dtype=np.float32)
    perm = np.zeros((128, 128), np.float32)
    for m in range(128):
        half = (m % 32) // 16
        k = m + 16 if half == 0 else m - 16
        perm[k, m] = 1.0
    cmat = np.concatenate([ident, perm], axis=1)
    ones = np.ones((128, 128), np.float32)
    bones = np.zeros((128, 128), np.float32)
    bones[0:64, 0:64] = 1.0
    bones[64:128, 64:128] = 1.0
    sel = np.zeros((128, 16, 128), np.float32)
    for e in range(16):
        sel[e, e, :] = 1.0
    cmatb = np.concatenate([ones, bones, sel.reshape(128, 2048)], axis=1)
    return cmat, cmatb


def _pvec(inp, b):
    pvv = np.zeros((128, NPV), np.float32)

    def put(name, arr):
        o, s = PV_OFF[name]
        n = int(np.prod(s))
        pvv[:, o:o + n] = np.asarray(arr, np.float32).reshape(128, n)

    cc = np.stack([inp["c"][b], inp["c_ctx"]], axis=0)
    put("cc", np.moveaxis(_chunked(cc), 1, 2))
    put("adab", _chunked(inp["ada_b"]))
    put("n1g", _chunked(inp["norm1_g"]))
    put("n2g", _chunked(inp["norm2_g"]))
    put("fg", _chunked(inp["final_g"]))
    put("qg", np.tile(inp["q_norm_g"].T, (2, 1)))
    put("kg", np.tile(inp["k_norm_g"].T, (2, 1)))
    put("cw", _chunked(inp["conv_w"]))
    put("cb", _chunked(inp["conv_b"]))
    put("lba", _chunked(inp["lru_ba"]))
    put("lbx", _chunked(inp["lru_bx"]))
    put("lam", _chunked(inp["lru_lambda"]))
    put("aog", _chunked(inp["attn_out_g"]))
    put("log", _chunked(inp["lru_out_g"]))
    put("rb", np.broadcast_to(inp["router_b"].reshape(1, 1, 16), (128, NCH, 16)))
    return pvv


def _lru_blockdiag(inp):
    out = np.zeros((NL, 128, 2, 2, 4, 128), np.float32)
    for gi, name in enumerate(("lru_wa", "lru_wx")):
        w = np.asarray(inp[name], np.float32)
        for c in range(4):
            out[:, 0:64, :, gi, c, 0:64] = np.transpose(w[:, :, 2 * c], (0, 2, 1, 3))
            out[:, 64:128, :, gi, c, 64:128] = np.transpose(w[:, :, 2 * c + 1], (0, 2, 1, 3))
    return out.reshape(NL, 128, 2048)


_CACHE = {}


def _get_nc():
    if "nc" not in _CACHE:
        _CACHE["nc"] = build()
    return _CACHE["nc"]


def make_in_maps(inp):
    inp = {k: np.asarray(v) for k, v in inp.items()}
    cmat, cmatb = _consts()
    rope = _rope_tables()
    lrubd = _lru_blockdiag(inp)
    rwl = np.ascontiguousarray(_chunked(inp["router_w"].T).transpose(0, 2, 1)).reshape(128, 128)
    shared = dict(cmat=cmat, cmatb=cmatb, rope=rope, ada_w=np.ascontiguousarray(inp["ada_w"], np.float32),
                  w_in=np.ascontiguousarray(inp["w_in"], np.float32), lrubd=lrubd,
                  w_out=np.ascontiguousarray(inp["w_out"], np.float32), rw=rwl,
                  wg=np.ascontiguousarray(inp["exp_w_gate"], np.float32), wu=np.ascontiguousarray(inp["exp_w_up"], np.float32),
                  wd=np.ascontiguousarray(inp["exp_w_down"], np.float32))
    maps = []
    for b in range(8):
        m = dict(shared)
        m["x"] = np.ascontiguousarray(inp["x"][b], np.float32)
        m["ctx"] = np.ascontiguousarray(inp["ctx"][b], np.float32)
        m["pvec"] = _pvec(inp, b)
        maps.append(m)
    return maps


def kernel(**inputs):
    nc = _get_nc()
    maps = make_in_maps(inputs)
    res = run_bass_kernel_spmd(nc, maps, core_ids=list(range(8)))
    return np.stack([np.asarray(r["out"], np.float32) for r in res.results], axis=0)
```

```python
import contextlib
import os
import numpy as np
import concourse.bass as bass
import concourse.mybir as mybir
from concourse.bass_utils import run_bass_kernel_spmd

F32 = mybir.dt.float32
BF16 = mybir.dt.bfloat16
AF = mybir.ActivationFunctionType
ALU = mybir.AluOpType
AX = mybir.AxisListType

D = 1024
S = 2048
C = 256
T = S + C
NL = 2
NE = 16
DE = 512
INW = 1792
TT = [(0, 256), (256, 768), (768, 1280), (1280, 1792), (1792, 2304)]
NCH = T // 128
EPS = 1e-6

PV_ITEMS = [
    ("cc", (8, 2)), ("adab", (NL, 48)), ("n1g", (NL, 8)), ("n2g", (NL, 8)), ("fg", (8,)),
    ("qg", (NL,)), ("kg", (NL,)), ("cw", (NL, 4, 4)), ("cb", (NL, 4)),
    ("lba", (NL, 2, 4)), ("lbx", (NL, 2, 4)), ("lam", (NL, 2, 4)),
    ("aog", (NL, 4)), ("log", (NL, 4)), ("rb", (NCH, 16)),
]
PV_OFF = {}
_o = 0
for _n, _s in PV_ITEMS:
    PV_OFF[_n] = (_o, _s)
    _o += int(np.prod(_s))
NPV = _o


class Prog:
    def __init__(self, nc, es):
        self.nc = nc
        self.es = es
        self.E = dict(pe=nc.tensor, act=nc.scalar, dve=nc.vector, pool=nc.gpsimd, sp=nc.sync)
        self.sem = {}
        self.cnt = {}
        for k in self.E:
            self.sem[k] = es.enter_context(nc.semaphore("sem_" + k))
            self.cnt[k] = 0
        self.seen = {k: {} for k in self.E}
        self.res = {}
        self.q = {k: [] for k in self.E}
        self._pw = {k: [] for k in self.E}
        self._bank = 0
        self.reserved = set()

    def _deps(self, eng, r, w, skip=None):
        need = {}
        for key in r:
            st = self.res.get(key)
            if st and st[0] is not None:
                k, v = st[0]
                if need.get(k, 0) < v:
                    need[k] = v
        for key in w:
            st = self.res.get(key)
            if st:
                if st[0] is not None:
                    k, v = st[0]
                    if need.get(k, 0) < v:
                        need[k] = v
                for k, v in st[1].items():
                    if need.get(k, 0) < v:
                        need[k] = v
        for k, v in need.items():
            if k == skip:
                continue
            if k == eng:
                if eng == "pe" or v > self.cnt[eng]:
                    continue
            if self.seen[eng].get(k, 0) < v:
                self.E[eng].wait_ge(self.sem[k], v)
                self.seen[eng][k] = v
                self._pw[eng].append((k, v))

    def _mark(self, tag, r, w):
        k, v = tag
        for key in r:
            st = self.res.setdefault(key, [None, {}])
            if st[1].get(k, 0) < v:
                st[1][k] = v
        for key in w:
            self.res[key] = [tag, {}]

    def op(self, eng, fn, r=(), w=(), inc=True):
        pr = [k for k in r if isinstance(k, tuple) and k[0] == "ps"]
        if pr:
            w = list(w) + pr
        self._deps(eng, r, w)
        inst = fn(self.E[eng])
        self.q[eng].append((self._pw[eng], (eng, 1) if inc else None))
        self._pw[eng] = []
        if inc:
            self.cnt[eng] += 1
            inst.then_inc(self.sem[eng], 1)
            tag = (eng, self.cnt[eng])
        else:
            tag = (eng, self.cnt[eng] + 1)
        self._mark(tag, r, w)
        return inst

    def dma(self, q, out, in_, semkey, r=(), w=(), **kw):
        if semkey not in self.sem:
            self.sem[semkey] = self.es.enter_context(self.nc.semaphore("d_" + semkey))
            self.cnt[semkey] = 0
        self._deps(q, r, w, skip=semkey)
        inst = self.E[q].dma_start(out=out, in_=in_, **kw)
        self.q[q].append((self._pw[q], (semkey, 16)))
        self._pw[q] = []
        self.cnt[semkey] += 16
        inst.then_inc(self.sem[semkey], 16)
        self._mark((semkey, self.cnt[semkey]), r, w)
        return inst

    def barrier(self):
        for e in self.E:
            for k, v in self.cnt.items():
                if k != e and v > 0 and self.seen[e].get(k, 0) < v:
                    self.E[e].wait_ge(self.sem[k], v)
                    self.seen[e][k] = v
                    self._pw[e].append((k, v))

    def check_deadlock(self):
        val = {k: 0 for k in self.cnt}
        pos = {k: 0 for k in self.E}
        for e in self.E:
            if self._pw[e]:
                self.q[e].append((self._pw[e], None))
                self._pw[e] = []
        progress = True
        while progress:
            progress = False
            for e in self.E:
                ql = self.q[e]
                while pos[e] < len(ql):
                    waits, inc = ql[pos[e]]
                    if all(val[k] >= v for k, v in waits):
                        if inc is not None:
                            val[inc[0]] += inc[1]
                        pos[e] += 1
                        progress = True
                    else:
                        break
        stuck = {e: (pos[e], len(self.q[e]), self.q[e][pos[e]][0]) for e in self.E if pos[e] < len(self.q[e])}
        if stuck:
            raise RuntimeError("DEADLOCK in semaphore plan: %r ; vals=%r" % (stuck, {k: val[k] for e in stuck for k, _ in stuck[e][2]}))

    def nb(self):
        while True:
            b = self._bank
            self._bank = (self._bank + 1) % 8
            if b not in self.reserved:
                return b


def build(stage=99, dbg=False):
    nc = bass.Bass("TRN2", target_bir_lowering=False)

    def dten(name, shape, dty=F32, kind="ExternalInput"):
        return nc.dram_tensor(name, shape, dty, kind=kind).ap()

    x_d = dten("x", [S, D])
    ctx_d = dten("ctx", [C, D])
    pv_d = dten("pvec", [128, NPV])
    cm_d = dten("cmat", [128, 256])
    cmb_d = dten("cmatb", [128, 256 + 2048])
    rope_d = dten("rope", [2, 128, S])
    adaw_d = dten("ada_w", [NL, D, 6 * D])
    win_d = dten("w_in", [NL, D, INW])
    lru_d = dten("lrubd", [NL, 128, 2048])
    wout_d = dten("w_out", [NL, D, D])
    rw_d = dten("rw", [128, 128])
    wg_d = dten("wg", [NL, NE, D, DE])
    wu_d = dten("wu", [NL, NE, D, DE])
    wd_d = dten("wd", [NL, NE, DE, D])
    out_d = dten("out", [S, D], kind="ExternalOutput")
    if dbg:
        dbgf_d = dten("dbgf", [128, 8 * T], F32, kind="ExternalOutput")
        dbgb_d = dten("dbgb", [128, 8 * T], BF16, kind="ExternalOutput")

    with contextlib.ExitStack() as es:
        P = Prog(nc, es)

        def sb(stack, name, shape, dty):
            return stack.enter_context(nc.sbuf_tensor("s_" + name, shape, dty))

        ps = [es.enter_context(nc.psum_tensor("ps%d" % i, [128, 512], F32)) for i in range(8)]

        def psk(b):
            return ("ps", b)

        xT = sb(es, "xT", [128, 8, T], F32)
        hT = sb(es, "hT", [128, 8, T], BF16)
        pv = sb(es, "pv", [128, NPV], F32)
        cm = sb(es, "cm", [128, 256], F32)
        cmb = sb(es, "cmb", [128, 256], BF16)
        scb = sb(es, "scb", [128, 8, 2], BF16)
        modv = sb(es, "modv", [128, NL, 48, 2], F32)
        gsv = sb(es, "gsv", [128, NL, 2, 8, 2], F32)
        hsp = sb(es, "hsp", [128, NL * 2 * 4], F32)
        hba = sb(es, "hba", [128, NL * 2 * 4], F32)
        hbx = sb(es, "hbx", [128, NL * 2 * 4], F32)
        rw = sb(es, "rw", [128, 128], F32)
        smalltmp = sb(es, "smalltmp", [128, 64], F32)

        ident = cm[:, 0:128]
        perm = cm[:, 128:256]
        ones_b = cmb[:, 0:128]
        bones_b = cmb[:, 128:256]

        def pvv(name):
            o, s = PV_OFF[name]
            n = int(np.prod(s))
            return pv[:, o:o + n]

        def pvi(name, *idx):
            o, s = PV_OFF[name]
            flat = 0
            for i, d in zip(idx, s):
                flat = flat * d + i
            return pv[:, o + flat:o + flat + 1]

        P.dma("sp", pv[:], pv_d, "c_pv", w=["pv"])
        P.dma("sp", cm[:], cm_d, "c_cm", w=["cm"])
        P.dma("sp", rw[:], rw_d, "c_rw", w=["rw"])
        P.dma("pool", cmb[:], cmb_d[:, 0:256], "c_cmb", w=["cmb"])

        o_cc = PV_OFF["cc"][0]
        P.op("act", lambda e: e.activation(out=scb[:].rearrange("p a b -> p (a b)"), in_=pv[:, o_cc:o_cc + 16],
                                           func=AF.Silu), r=["pv"], w=["scb"])
        P.op("act", lambda e: e.activation(out=smalltmp[:, 0:16], in_=pvv("lam"), func=AF.Exp, scale=-1.0),
             r=["pv"], w=["smalltmp"])
        P.op("act", lambda e: e.activation(out=smalltmp[:, 0:16], in_=smalltmp[:, 0:16], func=AF.Ln, bias=1.0),
             r=["smalltmp"], w=["smalltmp"])
        P.op("dve", lambda e: e.tensor_scalar(out=hsp[:], in0=smalltmp[:, 0:16], scalar1=-4.0, scalar2=None,
                                              op0=ALU.mult), r=["smalltmp"], w=["hsp"])
        P.op("dve", lambda e: e.tensor_scalar(out=hba[:], in0=pvv("lba"), scalar1=0.5, scalar2=None,
                                              op0=ALU.mult), r=["pv"], w=["hba"])
        P.op("dve", lambda e: e.tensor_scalar(out=hbx[:], in0=pvv("lbx"), scalar1=0.5, scalar2=None,
                                              op0=ALU.mult), r=["pv"], w=["hbx"])

        with contextlib.ExitStack() as s0:
            adaw = [sb(s0, "adaw%d" % i, [128, 8, 256], BF16) for i in range(2)]
            xin = [sb(s0, "xin%d" % i, [128, D], F32) for i in range(2)]
            def load_x(tc):
                k = tc % 2
                src = ctx_d[tc * 128:(tc + 1) * 128, :] if tc < 2 else x_d[(tc - 2) * 128:(tc - 1) * 128, :]
                P.dma("sp", xin[k][:], src, "xin%d" % k, w=[("xin", k)])
                tt = 0 if tc < 2 else 1 + (tc - 2) // 4
                for half in range(2):
                    b = P.nb()
                    for q in range(4):
                        c = half * 4 + q
                        P.op("pe", lambda e: e.transpose(out=ps[b][:, q * 128:(q + 1) * 128],
                                                         in_=xin[k][:, c * 128:(c + 1) * 128], identity=ident),
                             r=[("xin", k), "cm"], w=[psk(b)], inc=(q == 3))
                    eng = "act" if half == 0 else "dve"
                    if eng == "act":
                        P.op("act", lambda e: e.copy(out=xT[:, half * 4:half * 4 + 4, tc * 128:(tc + 1) * 128],
                                                     in_=ps[b][:].rearrange("p (q t) -> p q t", q=4)),
                             r=[psk(b)], w=[("xT", c_, tt) for c_ in range(half * 4, half * 4 + 4)])
                    else:
                        P.op("dve", lambda e: e.tensor_copy(out=xT[:, half * 4:half * 4 + 4, tc * 128:(tc + 1) * 128],
                                                            in_=ps[b][:].rearrange("p (q t) -> p q t", q=4)),
                             r=[psk(b)], w=[("xT", c_, tt) for c_ in range(half * 4, half * 4 + 4)])

            xi = 0
            NPIECE = 24
            mod_state = {}

            def mods_begin(l):
                pm = P.nb()
                P.reserved.add(pm)
                mod_state[l] = pm

            def mods_dma(l, j, bufs):
                k = j % 2
                P.dma("pool", bufs[k][:], adaw_d[l][:, j * 256:(j + 1) * 256].rearrange("(kc p) n -> p kc n", p=128),
                      "adaw%d" % k, w=[("adaw", k)])

            def mods_piece(l, j, bufs, dma=True):
                pm = mod_state[l]
                k = j % 2
                if dma:
                    mods_dma(l, j, bufs)
                for fc in range(2):
                    oc = j * 2 + fc
                    for kc in range(8):
                        P.op("pe", lambda e: e.matmul(ps[pm][:, oc * 2:oc * 2 + 2], lhsT=bufs[k][:, kc, fc * 128:(fc + 1) * 128],
                                                      rhs=scb[:, kc, :], start=(kc == 0), stop=(kc == 7)),
                             r=[("adaw", k), "scb"], w=[psk(pm)], inc=(kc == 7))

            def mods_finish(l, m0=0, m1=6, release=True):
                pm = mod_state[l]
                o_ab = PV_OFF["adab"][0] + l * 48
                for jj in range(2):
                    P.op("dve", lambda e: e.tensor_tensor(out=modv[:, l, m0 * 8:m1 * 8, jj],
                                                          in0=ps[pm][:, 0:96].rearrange("p (a b) -> p a b", b=2)[:, m0 * 8:m1 * 8, jj],
                                                          in1=pv[:, o_ab + m0 * 8:o_ab + m1 * 8], op=ALU.add),
                         r=[psk(pm), "pv"], w=[("modv", m_) for m_ in range(m0, m1)])
                if release:
                    P.reserved.discard(pm)
                for n_, (gname, mi) in enumerate((("n1g", 1), ("n2g", 4))):
                    if not (m0 <= mi < m1):
                        continue
                    og = PV_OFF[gname][0] + l * 8
                    for jj in range(2):
                        P.op("dve", lambda e: e.scalar_tensor_tensor(out=gsv[:, l, n_, :, jj], in0=modv[:, l, mi * 8:(mi + 1) * 8, jj],
                                                                     scalar=1.0, in1=pv[:, og:og + 8], op0=ALU.add, op1=ALU.mult),
                             r=[("modv", mi), "pv"], w=[("gsv", n_)])

            mods_begin(0)
            for j in range(8):
                mods_piece(0, j, adaw)
                for _ in range(2):
                    if xi < NCH:
                        load_x(xi); xi += 1
            mods_finish(0, 0, 2, release=False)
            while xi < NCH:
                load_x(xi); xi += 1
            P.barrier()

        if dbg and stage == 0:
            P.dma("sp", dbgf_d[:, 0:8 * T], xT[:].rearrange("p a b -> p (a b)"), "dbg", r=[("xT", c_, t_) for c_ in range(8) for t_ in range(5)])
            P.dma("sp", out_d[0:128, 0:192], modv[:].rearrange("p a b c -> p (a b c)"), "dbg", r=[("modv", m_) for m_ in range(6)])
            P.dma("sp", out_d[128:256, 0:64], gsv[:].rearrange("p a b c d -> p (a b c d)"), "dbg", r=[("gsv", 0), ("gsv", 1)])

        def xk(cs, tts):
            return [("xT", c_, t_) for c_ in cs for t_ in tts]

        def hk(cs, tts):
            return [("hT", c_, t_) for c_ in cs for t_ in tts]

        def norm_mod(l, nidx, tiles, stk, h2f=None, after_tile=None, post_rstd=None):
            shi = 0 if nidx == 0 else 3
            sq = [sb(stk, "nsq%d_%d_%d" % (l, nidx, i), [128, 512], BF16) for i in range(2)]
            rs = [sb(stk, "nrs%d_%d_%d" % (l, nidx, i), [128, 512], F32) for i in range(2)]
            tmp = [sb(stk, "ntmp%d_%d_%d" % (l, nidx, i), [128, 512], F32) for i in range(2)]
            def stageA(ti, tt):
                s, e_ = TT[tt]
                n = e_ - s
                b = P.nb()
                rk = ("nrs", ti % 2)
                for c in range(8):
                    k = c % 2
                    P.op("act", lambda e: e.activation(out=sq[k][:, :n], in_=xT[:, c, s:e_], func=AF.Square),
                         r=xk([c], [tt]), w=[("nsq", k)])
                    P.op("pe", lambda e: e.matmul(ps[b][:, :n], lhsT=ones_b, rhs=sq[k][:, :n], start=(c == 0), stop=(c == 7)),
                         r=[("nsq", k), "cmb"], w=[psk(b)])
                r_ = rs[ti % 2]
                P.op("act", lambda e: e.activation(out=r_[:, :n], in_=ps[b][:, :n], func=AF.Ln, scale=1.0 / D, bias=EPS),
                     r=[psk(b)], w=[rk])
                P.op("act", lambda e: e.activation(out=r_[:, :n], in_=r_[:, :n], func=AF.Exp, scale=-0.5), r=[rk], w=[rk])
                if post_rstd is not None:
                    post_rstd(ti, tt, r_, rk)

            def stageB(ti, tt):
                s, e_ = TT[tt]
                n = e_ - s
                jj = 1 if tt == 0 else 0
                rk = ("nrs", ti % 2)
                r_ = rs[ti % 2]
                for c in range(8):
                    k = c % 2
                    P.op("dve", lambda e: e.tensor_tensor(out=tmp[k][:, :n], in0=xT[:, c, s:e_], in1=r_[:, :n], op=ALU.mult),
                         r=xk([c], [tt]) + [rk], w=[("ntmp", k)])
                    if h2f is None:
                        P.op("act", lambda e: e.activation(out=hT[:, c, s:e_], in_=tmp[k][:, :n], func=AF.Identity,
                                                           scale=gsv[:, l, nidx, c, jj:jj + 1], bias=modv[:, l, shi * 8 + c, jj:jj + 1]),
                             r=[("ntmp", k), ("gsv", nidx), ("modv", shi)], w=hk([c], [tt]))
                    else:
                        P.op("act", lambda e: e.activation(out=h2f[:, c, :n], in_=tmp[k][:, :n], func=AF.Identity,
                                                           scale=gsv[:, l, nidx, c, jj:jj + 1], bias=modv[:, l, shi * 8 + c, jj:jj + 1]),
                             r=[("ntmp", k), ("gsv", nidx), ("modv", shi)], w=[("h2f", c)])
                        P.op("dve", lambda e: e.tensor_copy(out=hT[:, c, s:e_], in_=h2f[:, c, :n]),
                             r=[("h2f", c)], w=hk([c], [tt]))
                if after_tile is not None:
                    after_tile(tt)

            nt = len(tiles)
            for i in range(nt + 1):
                if i < nt:
                    stageA(i, tiles[i])
                if i - 1 >= 0:
                    stageB(i - 1, tiles[i - 1])

        def dump_and_finish():
            pass

        for l in range(NL):
            if dbg and stage == 0:
                break
            last = (l == NL - 1)
            all_tiles = [0, 1, 2, 3, 4]
            lat_tiles = [1, 2, 3, 4]
            q_tiles = lat_tiles if last else all_tiles
            with contextlib.ExitStack() as sm:
                with contextlib.ExitStack() as sn:
                    norm_mod(l, 0, all_tiles, sn)
                    P.barrier()
                if dbg and stage == 1 and l == 0:
                    P.dma("sp", dbgb_d[:, 0:8 * T], hT[:].rearrange("p a b -> p (a b)"), "dbg", r=hk(range(8), range(5)))
                    break
                NWP = 3
                wp = [sb(sm, "wp%d_%d" % (l, i), [128, 8, 128], BF16) for i in range(NWP)]
                wpc = [0]

                def wp_load(src_list):
                    k = wpc[0] % NWP
                    wpc[0] += 1
                    for (c0, c1, src) in src_list:
                        P.dma("pool", wp[k][:, :, c0:c1], src.rearrange("(kc p) n -> p kc n", p=128), "wp%d" % k, w=[("wp", k)])
                    return k

                def proj(k, tt, b):
                    s, e_ = TT[tt]
                    n = e_ - s
                    for kc in range(8):
                        P.op("pe", lambda e: e.matmul(ps[b][:, :n], lhsT=wp[k][:, kc, :], rhs=hT[:, kc, s:e_], start=(kc == 0), stop=(kc == 7)),
                             r=[("wp", k)] + hk([kc], [tt]), w=[psk(b)], inc=(kc == 7))

                mixR = sb(sm, "mixR%d" % l, [128, 4, T], BF16)
                mixA_box = [None]

                def MX(c_, p0=0, p1=128, c0=None, c1=None):
                    t_ = mixA_box[0] if c_ < 4 else mixR
                    return t_[p0:p1, c_ % 4, c0:c1]

                def mk(cs, tts):
                    return [("mixT", c_, t_) for c_ in cs for t_ in tts]

                with contextlib.ExitStack() as sl:
                    lw = sb(sl, "lw%d" % l, [128, 16, 128], BF16)
                    P.dma("pool", lw[:], lru_d[l].rearrange("p (a m) -> p a m", m=128), "c_lw", w=["lw"])
                    uh = sb(sl, "uh%d" % l, [128, T], F32)
                    cu = sb(sl, "cu%d" % l, [128, T], F32)
                    cub = sb(sl, "cub%d" % l, [128, T], BF16)
                    TA = sb(sl, "TA%d" % l, [128, T], F32)
                    TX = sb(sl, "TX%d" % l, [128, T], F32)
                    NM = sb(sl, "NM%d" % l, [128, T], F32)
                    gl = [sb(sl, "gl%d_%d" % (l, i), [128, 512], F32) for i in range(2)]
                    if l == 0:
                        adawL = [sb(sl, "adawS%d" % i, [128, 8, 256], BF16) for i in range(2)]
                        lp = [8]
                        lq = [8]
                        for _ in range(2):
                            mods_dma(0, lp[0], adawL)
                            lp[0] += 1
                    for c in range(4):
                        if l == 0:
                            for _ in range(4):
                                if lq[0] < NPIECE:
                                    mods_piece(0, lq[0], adawL, dma=False)
                                    lq[0] += 1
                                    if lp[0] < NPIECE:
                                        mods_dma(0, lp[0], adawL)
                                        lp[0] += 1
                        ku = wp_load([(0, 128, win_d[l][:, 768 + c * 128:768 + (c + 1) * 128])])
                        kg_ = wp_load([(0, 128, win_d[l][:, 1280 + c * 128:1280 + (c + 1) * 128])])
                        for tt in all_tiles:
                            s, e_ = TT[tt]
                            b = P.nb()
                            proj(ku, tt, b)
                            P.op("act", lambda e: e.copy(out=uh[:, s:e_], in_=ps[b][:, :e_ - s]), r=[psk(b)], w=["uh"])
                        P.op("dve", lambda e: e.tensor_scalar(out=cu[:, 0:T], in0=uh[:, 0:T], scalar1=pvi("cw", l, 2, c), scalar2=pvi("cb", l, c),
                                                              op0=ALU.mult, op1=ALU.add), r=["uh", "pv"], w=["cu"])
                        for (ga, gz) in ((0, C), (C, T)):
                            P.op("dve", lambda e: e.scalar_tensor_tensor(out=cu[:, ga + 2:gz], in0=uh[:, ga:gz - 2], scalar=pvi("cw", l, 0, c),
                                                                         in1=cu[:, ga + 2:gz], op0=ALU.mult, op1=ALU.add), r=["uh", "cu", "pv"], w=["cu"])
                            P.op("dve", lambda e: e.scalar_tensor_tensor(out=cu[:, ga + 1:gz], in0=uh[:, ga:gz - 1], scalar=pvi("cw", l, 1, c),
                                                                         in1=cu[:, ga + 1:gz], op0=ALU.mult, op1=ALU.add), r=["uh", "cu", "pv"], w=["cu"])
                            P.op("dve", lambda e: e.scalar_tensor_tensor(out=cu[:, ga:gz - 1], in0=uh[:, ga + 1:gz], scalar=pvi("cw", l, 3, c),
                                                                         in1=cu[:, ga:gz - 1], op0=ALU.mult, op1=ALU.add), r=["uh", "cu", "pv"], w=["cu"])
                        P.op("act", lambda e: e.copy(out=cub[:, :], in_=cu[:, :]), r=["cu"], w=["cub"])
                        for d in range(2):
                            li = (l * 2 + d) * 4 + c
                            for tt in all_tiles:
                                s, e_ = TT[tt]
                                n = e_ - s
                                ba_ = P.nb()
                                P.op("pe", lambda e: e.matmul(ps[ba_][:, :n], lhsT=lw[:, (d * 2 + 0) * 4 + c, :], rhs=cub[:, s:e_], start=True, stop=True),
                                     r=["lw", "cub"], w=[psk(ba_)])
                                P.op("act", lambda e: e.activation(out=TA[:, s:e_], in_=ps[ba_][:, :n], func=AF.Tanh, scale=0.5, bias=hba[:, li:li + 1]),
                                     r=[psk(ba_), "hba"], w=["TA"])
                            P.op("act", lambda e: e.activation(out=TA[:, :], in_=TA[:, :], func=AF.Exp, scale=hsp[:, li:li + 1], bias=hsp[:, li:li + 1]),
                                 r=["TA", "hsp"], w=["TA"])
                            P.op("dve", lambda e: e.scalar_tensor_tensor(out=NM[:, :], in0=TA[:, :], scalar=-1.0, in1=TA[:, :], op0=ALU.mult, op1=ALU.mult),
                                 r=["TA"], w=["NM"])
                            for tt in all_tiles:
                                s, e_ = TT[tt]
                                n = e_ - s
                                bx_ = P.nb()
                                P.op("pe", lambda e: e.matmul(ps[bx_][:, :n], lhsT=lw[:, (d * 2 + 1) * 4 + c, :], rhs=cub[:, s:e_], start=True, stop=True),
                                     r=["lw", "cub"], w=[psk(bx_)])
                                P.op("act", lambda e: e.activation(out=TX[:, s:e_], in_=ps[bx_][:, :n], func=AF.Tanh, scale=0.5, bias=hbx[:, li:li + 1]),
                                     r=[psk(bx_), "hbx"], w=["TX"])
                            P.op("act", lambda e: e.activation(out=NM[:, :], in_=NM[:, :], func=AF.Sqrt, scale=0.25, bias=0.25), r=["NM"], w=["NM"])
                            P.op("dve", lambda e: e.scalar_tensor_tensor(out=TX[:, :], in0=TX[:, :], scalar=1.0, in1=cu[:, :], op0=ALU.add, op1=ALU.mult),
                                 r=["TX", "cu"], w=["TX"])
                            P.op("dve", lambda e: e.tensor_tensor(out=TX[:, :], in0=TX[:, :], in1=NM[:, :], op=ALU.mult), r=["TX", "NM"], w=["TX"])
                            if d == 0:
                                P.op("dve", lambda e: e.tensor_tensor_scan(out=uh[:, 0:T], data0=TA[:, 0:T], data1=TX[:, 0:T], initial=0.0,
                                                                           op0=ALU.mult, op1=ALU.add), r=["TA", "TX", "cu"], w=["uh"])
                            else:
                                P.op("dve", lambda e: e.tensor_tensor_scan(out=NM[:, 0:C][:, ::-1], data0=TA[:, 0:C][:, ::-1], data1=TX[:, 0:C][:, ::-1],
                                                                           initial=0.0, op0=ALU.mult, op1=ALU.add), r=["TA", "TX"], w=["NM"])
                                P.op("dve", lambda e: e.tensor_tensor_scan(out=NM[:, C:T][:, ::-1], data0=TA[:, C:T][:, ::-1], data1=TX[:, C:T][:, ::-1],
                                                                           initial=NM[:, 0:1], op0=ALU.mult, op1=ALU.add), r=["TA", "TX", "NM"], w=["NM"])
                                P.op("dve", lambda e: e.tensor_tensor(out=uh[:, :], in0=uh[:, :], in1=NM[:, :], op=ALU.add), r=["uh", "NM"], w=["uh"])
                        for ti, tt in enumerate(q_tiles):
                            s, e_ = TT[tt]
                            n = e_ - s
                            b = P.nb()
                            proj(kg_, tt, b)
                            g_ = gl[ti % 2]
                            P.op("act", lambda e: e.activation(out=g_[:, :n], in_=ps[b][:, :n], func=AF.Gelu_apprx_tanh), r=[psk(b)], w=[("gl", ti % 2)])
                            P.op("dve", lambda e: e.tensor_tensor(out=MX(4 + c, c0=s, c1=e_), in0=uh[:, s:e_], in1=g_[:, :n], op=ALU.mult),
                                 r=["uh", ("gl", ti % 2)], w=mk([4 + c], [tt]))
                    if l == 0:
                        assert lq[0] == NPIECE
                        mods_finish(0, 2, 6)
                    P.barrier()

                if dbg and stage == 2 and l == 0:
                    (mixA_box[0] is not None and P.dma("sp", dbgb_d[:, 0:4 * T], mixA_box[0][:].rearrange("p a b -> p (a b)"), "dbg", r=mk(range(4), range(5)))); P.dma("sp", dbgb_d[:, 4 * T:8 * T], mixR[:].rearrange("p a b -> p (a b)"), "dbg", r=mk(range(4, 8), range(5)))
                    break

                mixA_box[0] = sb(sm, "mixA%d" % l, [128, 4, T], BF16)
                with contextlib.ExitStack() as sa:
                    ropeT = sb(sa, "rope%d" % l, [128, 2, S], BF16)
                    for a_ in range(2):
                        P.dma("pool", ropeT[:, a_, :], rope_d[a_], "c_rope", w=["rope"], max_dma_last_dim=4096)
                    ATT_STOP = int(os.environ.get("ATT_STOP", "9"))
                    kT2 = sb(sa, "kT2_%d" % l, [128, 2, T], BF16)
                    vaug = sb(sa, "vaug%d" % l, [128, NCH, 2, 128], BF16)
                    qZ = [sb(sa, "qZ%d_%d" % (l, i), [128, T], BF16) for i in range(2)]
                    NPT = 3
                    pt = [sb(sa, "pt%d_%d" % (l, i), [128, 512], BF16) for i in range(NPT)]
                    rc = [sb(sa, "rc%d_%d" % (l, i), [64, 512], F32) for i in range(2)]
                    sqh = sb(sa, "sqh%d" % l, [128, 512], BF16)
                    rsh = sb(sa, "rsh%d" % l, [128, 512], F32)
                    qn = sb(sa, "qn%d" % l, [128, 512], F32)
                    P.op("dve", lambda e: e.memset(vaug[:, :, :, 64:128], 1.0), w=["vones"])
                    P.op("dve", lambda e: e.memset(qZ[0][64:128, :], 0.0), w=["qz0"])
                    P.op("dve", lambda e: e.memset(qZ[1][0:64, :], 0.0), w=["qz1"])

                    sqh2 = [sqh, sb(sa, "sqhB%d" % l, [128, 512], BF16)]
                    rsh2 = [rsh, sb(sa, "rshB%d" % l, [128, 512], F32)]
                    qn2 = [qn, sb(sa, "qnB%d" % l, [128, 512], F32)]
                    hnc = [0]

                    def pnr_pipeline(items):
                        st = {}
                        n_it = len(items)
                        for i in range(n_it + 2):
                            if i < n_it:
                                k_, tt, gname, dst, dkeys = items[i]
                                b = P.nb()
                                proj(k_, tt, b)
                                st[i] = dict(b=b)
                            if 0 <= i - 1 < n_it:
                                k_, tt, gname, dst, dkeys = items[i - 1]
                                s, e_ = TT[tt]
                                n = e_ - s
                                b = st[i - 1]["b"]
                                u_ = hnc[0] % 2
                                hnc[0] += 1
                                st[i - 1]["u"] = u_
                                sq_, rs_, q_ = sqh2[u_], rsh2[u_], qn2[u_]
                                P.op("act", lambda e: e.activation(out=sq_[:, :n], in_=ps[b][:, :n], func=AF.Square), r=[psk(b)], w=[("sqh", u_)])
                                b2 = P.nb()
                                P.op("pe", lambda e: e.matmul(ps[b2][:, :n], lhsT=bones_b, rhs=sq_[:, :n], start=True, stop=True), r=[("sqh", u_), "cmb"], w=[psk(b2)])
                                P.op("act", lambda e: e.activation(out=rs_[:, :n], in_=ps[b2][:, :n], func=AF.Ln, scale=1.0 / 64, bias=EPS), r=[psk(b2)], w=[("rsh", u_)])
                                P.op("act", lambda e: e.activation(out=rs_[:, :n], in_=rs_[:, :n], func=AF.Exp, scale=-0.5), r=[("rsh", u_)], w=[("rsh", u_)])
                                P.op("dve", lambda e: e.scalar_tensor_tensor(out=q_[:, :n], in0=ps[b][:, :n], scalar=pvi(gname, l), in1=rs_[:, :n],
                                                                             op0=ALU.mult, op1=ALU.mult), r=[psk(b), ("rsh", u_), "pv"], w=[("qn", u_)])
                                if tt > 0:
                                    b3 = P.nb()
                                    P.op("pe", lambda e: e.matmul(ps[b3][:, :n], lhsT=perm, rhs=q_[:, :n], start=True, stop=True), r=[("qn", u_), "cm"], w=[psk(b3)])
                                    st[i - 1]["b3"] = b3
                            if 0 <= i - 2 < n_it:
                                k_, tt, gname, dst, dkeys = items[i - 2]
                                s, e_ = TT[tt]
                                n = e_ - s
                                u_ = st[i - 2]["u"]
                                sq_, rs_, q_ = sqh2[u_], rsh2[u_], qn2[u_]
                                if tt > 0:
                                    b3 = st[i - 2]["b3"]
                                    P.op("dve", lambda e: e.tensor_tensor(out=q_[:, :n], in0=q_[:, :n], in1=ropeT[:, 0, s - C:e_ - C], op=ALU.mult),
                                         r=[("qn", u_), "rope"], w=[("qn", u_)])
                                    P.op("dve", lambda e: e.tensor_tensor(out=rs_[:, :n], in0=ps[b3][:, :n], in1=ropeT[:, 1, s - C:e_ - C], op=ALU.mult),
                                         r=[psk(b3), "rope"], w=[("rsh", u_)])
                                    for (p0, p1, d_) in dst:
                                        P.op("dve", lambda e: e.tensor_tensor(out=d_, in0=q_[p0:p1, :n], in1=rs_[p0:p1, :n], op=ALU.add),
                                             r=[("qn", u_), ("rsh", u_)], w=dkeys)
                                else:
                                    for (p0, p1, d_) in dst:
                                        P.op("act", lambda e: e.copy(out=d_, in_=q_[p0:p1, :n]), r=[("qn", u_)], w=dkeys)

                    k_items = []
                    for kvh in range(2):
                        c0 = 512 + kvh * 64
                        kk = wp_load([(0, 64, win_d[l][:, c0:c0 + 64]), (64, 128, win_d[l][:, c0:c0 + 64])])
                        for tt in all_tiles:
                            s, e_ = TT[tt]
                            k_items.append((kk, tt, "kg", [(0, 128, kT2[:, kvh, s:e_])], [("kT2", kvh, tt)]))
                    pnr_pipeline(k_items)
                    kv_ = wp_load([(0, 128, win_d[l][:, 640:768])])
                    for g4 in range(5):
                        chunks = list(range(g4 * 4, min(g4 * 4 + 4, NCH)))
                        b = P.nb()
                        for i, tc in enumerate(chunks):
                            tt_ = 0 if tc < 2 else 1 + (tc - 2) // 4
                            for kc in range(8):
                                P.op("pe", lambda e: e.matmul(ps[b][:, i * 128:(i + 1) * 128], lhsT=hT[:, kc, tc * 128:(tc + 1) * 128], rhs=wp[kv_][:, kc, :],
                                                              start=(kc == 0), stop=(kc == 7)),
                                     r=[("wp", kv_)] + hk([kc], [tt_]), w=[psk(b)], inc=(kc == 7 and i == len(chunks) - 1))
                        nch = len(chunks)
                        src = ps[b][:, 0:nch * 128].rearrange("p (i k d) -> p i k d", i=nch, k=2)
                        tc0 = chunks[0]
                        P.op("act", lambda e: e.copy(out=vaug[:, tc0:tc0 + nch, :, 0:64], in_=src), r=[psk(b)], w=[("vaug", g4, 0)])
                    vkeys = [("vaug", g4, 0) for g4 in range(5)] + ["vones"]

                    if ATT_STOP <= 2:
                        P.barrier(); break
                    SB = [0, 1, 2, 3]
                    sbc = [0]
                    ptc = [0]
                    qtc = [0]
                    for c in range(4):
                        kvh = c // 2
                        kq = wp_load([(0, 128, win_d[l][:, c * 128:(c + 1) * 128])])
                        pnr_pipeline([(kq, tt, "qg", [(0, 64, qZ[0][0:64, TT[tt][0]:TT[tt][1]]), (64, 128, qZ[1][64:128, TT[tt][0]:TT[tt][1]])], [("qT", tt)])
                                      for tt in q_tiles])
                        if ATT_STOP <= 3:
                            P.barrier(); break
                        for tt in q_tiles:
                            s, e_ = TT[tt]
                            n = e_ - s
                            kchunks = [0, 1] if tt == 0 else list(range(NCH))
                            po = (4, 5) if qtc[0] % 2 == 0 else (6, 7)
                            qtc[0] += 1
                            steps = [(j, kc) for kc in kchunks for j in range(2)]
                            pend = []

                            def emit_qk(j, kc):
                                sbk = SB[sbc[0] % 4]
                                sbc[0] += 1
                                ktt = 0 if kc < 2 else 1 + (kc - 2) // 4
                                P.op("pe", lambda e: e.matmul(ps[sbk][:, :n], lhsT=kT2[:, kvh, kc * 128:(kc + 1) * 128],
                                                              rhs=qZ[j][:, s:e_], start=True, stop=True),
                                     r=[("kT2", kvh, ktt), ("qT", tt), "qz0", "qz1"], w=[psk(sbk)])
                                return sbk

                            def emit_pv(j, kc, sbk, first, lastk):
                                pk = ptc[0] % NPT
                                ptc[0] += 1
                                P.op("act", lambda e: e.activation(out=pt[pk][:, :n], in_=ps[sbk][:, :n], func=AF.Exp, scale=0.125),
                                     r=[psk(sbk)], w=[("pt", pk)])
                                P.op("pe", lambda e: e.matmul(ps[po[j]][:, :n], lhsT=vaug[:, kc, kvh, :], rhs=pt[pk][:, :n],
                                                              start=first, stop=lastk),
                                     r=[("pt", pk)] + vkeys, w=[psk(po[j])])

                            LOOK = 2
                            for i, (j, kc) in enumerate(steps):
                                pend.append((j, kc, emit_qk(j, kc)))
                                if len(pend) > LOOK:
                                    pj, pkc, psb = pend.pop(0)
                                    emit_pv(pj, pkc, psb, pkc == kchunks[0], pkc == kchunks[-1])
                            while pend:
                                pj, pkc, psb = pend.pop(0)
                                emit_pv(pj, pkc, psb, pkc == kchunks[0], pkc == kchunks[-1])
                            for j in range(2):
                                P.op("dve", lambda e: e.reciprocal(out=rc[j][0:64, :n], in_=ps[po[j]][64:128, :n]), r=[psk(po[j])], w=[("rc", j)])
                                P.op("dve", lambda e: e.tensor_tensor(out=MX(c, j * 64, j * 64 + 64, s, e_), in0=ps[po[j]][0:64, :n], in1=rc[j][0:64, :n], op=ALU.mult),
                                     r=[psk(po[j]), ("rc", j)], w=mk([c], [tt]))
                    P.barrier()

                if dbg and stage == 3 and l == 0:
                    (mixA_box[0] is not None and P.dma("sp", dbgb_d[:, 0:4 * T], mixA_box[0][:].rearrange("p a b -> p (a b)"), "dbg", r=mk(range(4), range(5)))); P.dma("sp", dbgb_d[:, 4 * T:8 * T], mixR[:].rearrange("p a b -> p (a b)"), "dbg", r=mk(range(4, 8), range(5)))
                    break

                with contextlib.ExitStack() as so:
                    osq = [sb(so, "osq%d_%d" % (l, i), [128, 512], BF16) for i in range(2)]
                    ors = [sb(so, "ors%d_%d" % (l, i), [128, 512], F32) for i in range(2)]
                    oi = 0
                    for half, gname in ((0, "aog"), (1, "log")):
                        for tt in q_tiles:
                            s, e_ = TT[tt]
                            n = e_ - s
                            b = P.nb()
                            for c4 in range(4):
                                cc_ = half * 4 + c4
                                k = c4 % 2
                                P.op("act", lambda e: e.activation(out=osq[k][:, :n], in_=MX(cc_, c0=s, c1=e_), func=AF.Square), r=mk([cc_], [tt]), w=[("osq", k)])
                                P.op("pe", lambda e: e.matmul(ps[b][:, :n], lhsT=ones_b, rhs=osq[k][:, :n], start=(c4 == 0), stop=(c4 == 3)),
                                     r=[("osq", k), "cmb"], w=[psk(b)])
                            r_ = ors[oi % 2]
                            rk = ("ors", oi % 2)
                            oi += 1
                            P.op("act", lambda e: e.activation(out=r_[:, :n], in_=ps[b][:, :n], func=AF.Ln, scale=1.0 / 512, bias=EPS), r=[psk(b)], w=[rk])
                            P.op("act", lambda e: e.activation(out=r_[:, :n], in_=r_[:, :n], func=AF.Exp, scale=-0.5), r=[rk], w=[rk])
                            for c4 in range(4):
                                cc_ = half * 4 + c4
                                P.op("dve", lambda e: e.scalar_tensor_tensor(out=MX(cc_, c0=s, c1=e_), in0=MX(cc_, c0=s, c1=e_), scalar=pvi(gname, l, c4),
                                                                             in1=r_[:, :n], op0=ALU.mult, op1=ALU.mult), r=mk([cc_], [tt]) + [rk, "pv"], w=mk([cc_], [tt]))
                    if dbg and stage == 4 and l == 0:
                        (mixA_box[0] is not None and P.dma("sp", dbgb_d[:, 0:4 * T], mixA_box[0][:].rearrange("p a b -> p (a b)"), "dbg", r=mk(range(4), range(5)))); P.dma("sp", dbgb_d[:, 4 * T:8 * T], mixR[:].rearrange("p a b -> p (a b)"), "dbg", r=mk(range(4, 8), range(5)))
                    for o in range(8):
                        ko = wp_load([(0, 128, wout_d[l][:, o * 128:(o + 1) * 128])])
                        for tt in q_tiles:
                            s, e_ = TT[tt]
                            n = e_ - s
                            jj = 1 if tt == 0 else 0
                            b = P.nb()
                            for kc in range(8):
                                P.op("pe", lambda e: e.matmul(ps[b][:, :n], lhsT=wp[ko][:, kc, :], rhs=MX(kc, c0=s, c1=e_), start=(kc == 0), stop=(kc == 7)),
                                     r=[("wp", ko)] + mk([kc], [tt]), w=[psk(b)], inc=(kc == 7))
                            P.op("dve", lambda e: e.scalar_tensor_tensor(out=xT[:, o, s:e_], in0=ps[b][:, :n], scalar=modv[:, l, 2 * 8 + o, jj:jj + 1],
                                                                         in1=xT[:, o, s:e_], op0=ALU.mult, op1=ALU.add),
                                 r=[psk(b), ("modv", 2)] + xk([o], [tt]), w=xk([o], [tt]))
                    P.barrier()
            if dbg and stage in (1, 2, 3) and l == 0:
                break
            if dbg and stage == 4 and l == 0:
                P.dma("sp", dbgf_d[:, 0:8 * T], xT[:].rearrange("p a b -> p (a b)"), "dbg", r=xk(range(8), range(5)))
                break

            with contextlib.ExitStack() as se:
                NEP = 6
                ep = [sb(se, "ep%d_%d" % (l, i), [128, 4096], BF16) for i in range(NEP)]
                epc = [0]
                sel = sb(se, "sel%d" % l, [128, 16, 128], BF16)
                P.dma("pool", sel[:], cmb_d[:, 256:256 + 2048].rearrange("p (a m) -> p a m", m=128), "c_sel", w=["sel"])
                combT = sb(se, "combT%d" % l, [128, T], BF16)
                cbt = sb(se, "cbt%d" % l, [128, NCH, 16], F32)

                def ep_load(e_i):
                    ks = []
                    for wi, (wd_, pat) in enumerate(((wg_d, "g"), (wu_d, "u"), (wd_d, "d"))):
                        k = epc[0] % NEP
                        epc[0] += 1
                        if pat == "d":
                            P.dma("pool", ep[k][:].rearrange("p (j n) -> p j n", j=4), wd_[l][e_i].rearrange("(j p) n -> p j n", p=128), "ep%d" % k, w=[("ep", k)])
                        else:
                            P.dma("pool", ep[k][:].rearrange("p (j n) -> p j n", j=8), wd_[l][e_i].rearrange("(j p) n -> p j n", p=128), "ep%d" % k, w=[("ep", k)])
                        ks.append(k)
                    return ks

                m_tiles = lat_tiles if last else all_tiles
                ch0 = 2 if last else 0
                nchs = NCH - ch0
                eks = {0: ep_load(0)}
                with contextlib.ExitStack() as sg:
                    sg2 = contextlib.ExitStack()
                    rl = P.nb()
                    P.reserved.add(rl)
                    rwg = sb(sg2, "rwg%d" % l, [128, 2, 8, 16], F32)
                    cst = sb(sg2, "cst%d" % l, [16, 2], F32)
                    lgt = [sb(sg2, "lgt%d_%d" % (l, i), [16, 512], F32) for i in range(2)]
                    eT = sb(sg2, "eT%d" % l, [16, T], F32)
                    for jj in range(2):
                        for kc in range(8):
                            P.op("dve", lambda e: e.tensor_scalar(out=rwg[:, jj, kc, :], in0=rw[:, kc * 16:(kc + 1) * 16], scalar1=gsv[:, l, 1, kc, jj:jj + 1],
                                                                  scalar2=None, op0=ALU.mult), r=["rw", ("gsv", 1)], w=["rwg"])
                    bc = P.nb()
                    for kc in range(8):
                        P.op("pe", lambda e: e.matmul(ps[bc][0:16, 0:2], lhsT=rw[:, kc * 16:(kc + 1) * 16], rhs=modv[:, l, 3 * 8 + kc, :], start=(kc == 0), stop=(kc == 7)),
                             r=["rw", ("modv", 3)], w=[psk(bc)], inc=(kc == 7))
                    P.op("dve", lambda e: e.tensor_scalar(out=cst[:, :], in0=ps[bc][0:16, 0:2], scalar1=-1.0, scalar2=None, op0=ALU.mult), r=[psk(bc)], w=["cst"])

                    def router_tile(ti, tt, r_, rk):
                        s, e_ = TT[tt]
                        n = e_ - s
                        jj = 1 if tt == 0 else 0
                        b = P.nb()
                        for kc in range(8):
                            P.op("pe", lambda e: e.matmul(ps[b][0:16, :n], lhsT=rwg[:, jj, kc, :], rhs=xT[:, kc, s:e_], start=(kc == 0), stop=(kc == 7)),
                                 r=["rwg"] + xk([kc], [tt]), w=[psk(b)], inc=(kc == 7))
                        lg_ = lgt[ti % 2]
                        def fin():
                            P.op("dve", lambda e: e.tensor_tensor(out=lg_[:, :n], in0=ps[b][0:16, :n], in1=r_[0:16, :n], op=ALU.mult), r=[psk(b), rk], w=[("lgt", ti % 2)])
                            P.op("act", lambda e: e.activation(out=eT[:, s:e_], in_=lg_[:, :n], func=AF.Exp, scale=-1.0, bias=cst[:, jj:jj + 1]),
                                 r=[("lgt", ti % 2), "cst"], w=[("eT", tt)])
                        rdefer.append(fin)

                    rdefer = []

                    def flush_router(tt):
                        while rdefer:
                            rdefer.pop(0)()

                    norm_mod(l, 1, m_tiles, sg2, post_rstd=router_tile, after_tile=flush_router)
                    flush_router(None)
                    for ch in range(ch0, NCH):
                        tt_ = 0 if ch < 2 else 1 + (ch - 2) // 4
                        P.op("pe", lambda e: e.transpose(out=ps[rl][:, ch * 16:(ch + 1) * 16], in_=eT[0:16, ch * 128:(ch + 1) * 128], identity=ident[0:16, 0:16]),
                             r=[("eT", tt_), "cm"], w=[psk(rl)], inc=(ch == NCH - 1))
                    P.barrier()
                    sg2.close()

                    def rt(name):
                        return sb(sg, "%s_%d" % (name, l), [128, NCH * 16], F32)

                    sc = rt("r_sc"); bi = rt("r_bi"); selm = rt("r_sel")
                    m1 = rt("r_m1"); m2 = rt("r_m2"); mn = rt("r_mn"); gsum = rt("r_gs"); gmax = rt("r_gm"); ing = rt("r_in"); den = rt("r_den")
                    lo, hi = ch0 * 16, NCH * 16
                    nq = nchs * 4
                    P.op("dve", lambda e: e.tensor_scalar(out=sc[:, lo:hi], in0=ps[rl][:, lo:hi], scalar1=1.0, scalar2=None, op0=ALU.add), r=[psk(rl)], w=["r_sc"])
                    P.op("dve", lambda e: e.reciprocal(out=sc[:, lo:hi], in_=sc[:, lo:hi]), r=["r_sc"], w=["r_sc"])
                    orb = PV_OFF["rb"][0]
                    P.op("dve", lambda e: e.tensor_tensor(out=bi[:, lo:hi], in0=sc[:, lo:hi], in1=pv[:, orb + lo:orb + hi], op=ALU.add), r=["r_sc", "pv"], w=["r_bi"])
                    g4v = bi[:, lo:hi].rearrange("p (q i) -> p q i", i=4)
                    P.op("dve", lambda e: e.tensor_reduce(out=m1[:, 0:nq], in_=g4v, axis=AX.X, op=ALU.max), r=["r_bi"], w=["r_m1"])
                    first = True
                    for i in range(4):
                        for j in range(i + 1, 4):
                            if first:
                                P.op("dve", lambda e: e.tensor_tensor(out=m2[:, 0:nq], in0=g4v[:, :, i], in1=g4v[:, :, j], op=ALU.min), r=["r_bi"], w=["r_m2"])
                                first = False
                            else:
                                P.op("dve", lambda e: e.tensor_tensor(out=mn[:, 0:nq], in0=g4v[:, :, i], in1=g4v[:, :, j], op=ALU.min), r=["r_bi"], w=["r_mn"])
                                P.op("dve", lambda e: e.tensor_tensor(out=m2[:, 0:nq], in0=m2[:, 0:nq], in1=mn[:, 0:nq], op=ALU.max), r=["r_m2", "r_mn"], w=["r_m2"])
                    P.op("dve", lambda e: e.tensor_tensor(out=gsum[:, 0:nq], in0=m1[:, 0:nq], in1=m2[:, 0:nq], op=ALU.add), r=["r_m1", "r_m2"], w=["r_gs"])
                    gsv3 = gsum[:, 0:nq].rearrange("p (c g) -> p c g", g=4)
                    P.op("dve", lambda e: e.tensor_reduce(out=gmax[:, 0:nchs], in_=gsv3, axis=AX.X, op=ALU.max), r=["r_gs"], w=["r_gm"])
                    ing3 = ing[:, 0:nq].rearrange("p (c g) -> p c g", g=4)
                    for g in range(4):
                        P.op("dve", lambda e: e.tensor_tensor(out=ing3[:, :, g], in0=gsv3[:, :, g], in1=gmax[:, 0:nchs], op=ALU.is_ge), r=["r_gs", "r_gm"], w=["r_in"])
                    sel3 = selm[:, lo:hi].rearrange("p (q i) -> p q i", i=4)
                    for i in range(4):
                        P.op("dve", lambda e: e.tensor_tensor(out=sel3[:, :, i], in0=g4v[:, :, i], in1=m2[:, 0:nq], op=ALU.is_ge), r=["r_bi", "r_m2"], w=["r_sel"])
                        P.op("dve", lambda e: e.tensor_tensor(out=sel3[:, :, i], in0=sel3[:, :, i], in1=ing[:, 0:nq], op=ALU.mult), r=["r_sel", "r_in"], w=["r_sel"])
                    P.op("dve", lambda e: e.tensor_tensor(out=selm[:, lo:hi], in0=selm[:, lo:hi], in1=sc[:, lo:hi], op=ALU.mult), r=["r_sel", "r_sc"], w=["r_sel"])
                    P.op("dve", lambda e: e.tensor_reduce(out=den[:, 0:nchs], in_=selm[:, lo:hi].rearrange("p (c x) -> p c x", x=16), axis=AX.X, op=ALU.add),
                         r=["r_sel"], w=["r_den"])
                    P.op("dve", lambda e: e.reciprocal(out=den[:, 0:nchs], in_=den[:, 0:nchs]), r=["r_den"], w=["r_den"])
                    for ci in range(nchs):
                        ch = ch0 + ci
                        P.op("dve", lambda e: e.tensor_scalar(out=cbt[:, ch, :], in0=selm[:, ch * 16:(ch + 1) * 16], scalar1=den[:, ci:ci + 1], scalar2=None, op0=ALU.mult),
                             r=["r_sel", "r_den"], w=[("cbt", ch)])
                    for g4 in range((nchs + 3) // 4):
                        chs = list(range(ch0 + g4 * 4, min(ch0 + g4 * 4 + 4, NCH)))
                        b = P.nb()
                        for i, ch in enumerate(chs):
                            P.op("pe", lambda e: e.transpose(out=ps[b][0:16, i * 128:(i + 1) * 128], in_=cbt[:, ch, :], identity=ident),
                                 r=[("cbt", ch), "cm"], w=[psk(b)], inc=(i == len(chs) - 1))
                        P.op("act", lambda e: e.copy(out=combT[0:16, chs[0] * 128:(chs[-1] + 1) * 128], in_=ps[b][0:16, 0:len(chs) * 128]), r=[psk(b)], w=["combT"])
                    P.reserved.discard(rl)
                    P.barrier()

                if dbg and stage == 5 and l == 0:
                    P.dma("sp", dbgb_d[:, 0:8 * T], hT[:].rearrange("p a b -> p (a b)"), "dbg", r=hk(range(8), range(5)))
                    P.dma("sp", dbgf_d[:, 0:NCH * 16], cbt[:].rearrange("p a b -> p (a b)"), "dbg", r=[("cbt", ch) for ch in range(NCH)])
                    break

                with contextlib.ExitStack() as sh:
                    cbe = [sb(sh, "cbe%d_%d" % (l, i), [128, T], BF16) for i in range(2)]
                    sgt = [sb(sh, "sgt%d_%d" % (l, i), [128, 512], BF16) for i in range(2)]
                    hid = [sb(sh, "hid%d_%d" % (l, i), [128, 4, 512], BF16) for i in range(2)]
                    sgc = [0]
                    hdc = [0]
                    if l + 1 < NL:
                        adaw2 = [sb(sh, "adawL%d_%d" % (l, i), [128, 8, 256], BF16) for i in range(2)]
                        mods_begin(l + 1)
                        mp = [0]
                        mq = [0]
                    for ei in range(NE):
                        if ei + 1 < NE:
                            eks[ei + 1] = ep_load(ei + 1)
                        if l + 1 < NL:
                            while mq[0] < mp[0]:
                                mods_piece(l + 1, mq[0], adaw2, dma=False)
                                mq[0] += 1
                            for _ in range(2):
                                if mp[0] < NPIECE:
                                    mods_dma(l + 1, mp[0], adaw2)
                                    mp[0] += 1
                        kg_, ku_, kd_ = eks[ei]
                        wgv = ep[kg_][:].rearrange("p (j n) -> p j n", j=8)
                        wuv = ep[ku_][:].rearrange("p (j n) -> p j n", j=8)
                        wdv = ep[kd_][:].rearrange("p (j n) -> p j n", j=4)
                        cb_ = cbe[ei % 2]
                        ck = ("cbe", ei % 2)
                        for tt in m_tiles:
                            s, e_ = TT[tt]
                            n = e_ - s
                            b = P.nb()
                            P.op("pe", lambda e: e.matmul(ps[b][:, :n], lhsT=sel[0:16, ei, :], rhs=combT[0:16, s:e_], start=True, stop=True),
                                 r=["sel", "combT"], w=[psk(b)])
                            P.op("act", lambda e: e.copy(out=cb_[:, s:e_], in_=ps[b][:, :n]), r=[psk(b)], w=[ck])
                        for tt in m_tiles:
                            s, e_ = TT[tt]
                            n = e_ - s
                            jj = 1 if tt == 0 else 0
                            hd = hid[hdc[0] % 2]
                            hkey = ("hid", hdc[0] % 2)
                            hdc[0] += 1
                            for f in range(4):
                                bg = P.nb()
                                for kc in range(8):
                                    P.op("pe", lambda e: e.matmul(ps[bg][:, :n], lhsT=wgv[:, kc, f * 128:(f + 1) * 128], rhs=hT[:, kc, s:e_], start=(kc == 0), stop=(kc == 7)),
                                         r=[("ep", kg_)] + hk([kc], [tt]), w=[psk(bg)], inc=(kc == 7))
                                bu = P.nb()
                                for kc in range(8):
                                    P.op("pe", lambda e: e.matmul(ps[bu][:, :n], lhsT=wuv[:, kc, f * 128:(f + 1) * 128], rhs=hT[:, kc, s:e_], start=(kc == 0), stop=(kc == 7)),
                                         r=[("ep", ku_)] + hk([kc], [tt]), w=[psk(bu)], inc=(kc == 7))
                                sk = sgc[0] % 2
                                sgc[0] += 1
                                P.op("act", lambda e: e.activation(out=sgt[sk][:, :n], in_=ps[bg][:, :n], func=AF.Silu), r=[psk(bg)], w=[("sgt", sk)])
                                P.op("dve", lambda e: e.tensor_tensor(out=sgt[sk][:, :n], in0=sgt[sk][:, :n], in1=cb_[:, s:e_], op=ALU.mult), r=[("sgt", sk), ck], w=[("sgt", sk)])
                                P.op("dve", lambda e: e.tensor_tensor(out=hd[:, f, :n], in0=ps[bu][:, :n], in1=sgt[sk][:, :n], op=ALU.mult), r=[psk(bu), ("sgt", sk)], w=[hkey])
                            for o in range(8):
                                bo = P.nb()
                                for jc in range(4):
                                    P.op("pe", lambda e: e.matmul(ps[bo][:, :n], lhsT=wdv[:, jc, o * 128:(o + 1) * 128], rhs=hd[:, jc, :n], start=(jc == 0), stop=(jc == 3)),
                                         r=[("ep", kd_), hkey], w=[psk(bo)], inc=(jc == 3))
                                P.op("dve", lambda e: e.scalar_tensor_tensor(out=xT[:, o, s:e_], in0=ps[bo][:, :n], scalar=modv[:, l, 5 * 8 + o, jj:jj + 1],
                                                                             in1=xT[:, o, s:e_], op0=ALU.mult, op1=ALU.add),
                                     r=[psk(bo), ("modv", 5)] + xk([o], [tt]), w=xk([o], [tt]))
                    if l + 1 < NL:
                        assert mp[0] == NPIECE
                        while mq[0] < mp[0]:
                            mods_piece(l + 1, mq[0], adaw2, dma=False)
                            mq[0] += 1
                        mods_finish(l + 1)
                    P.barrier()
            if dbg and stage == 5 and l == 0:
                break
            if dbg and stage == 6 and l == 0:
                P.dma("sp", dbgf_d[:, 0:8 * T], xT[:].rearrange("p a b -> p (a b)"), "dbg", r=xk(range(8), range(5)))
                break

        if not dbg or stage >= 99:
            with contextlib.ExitStack() as sf:
                fsq = [sb(sf, "fsq%d" % i, [128, 512], BF16) for i in range(2)]
                frs = [sb(sf, "frs%d" % i, [128, 512], F32) for i in range(2)]
                yT = [sb(sf, "yT%d" % i, [128, 8, 512], F32) for i in range(2)]
                ob = [sb(sf, "ob%d" % i, [128, D], F32) for i in range(2)]
                obc = 0
                ofg = PV_OFF["fg"][0]
                obc_box = [0]

                def finA(ti, tt):
                    s, e_ = TT[tt]
                    n = e_ - s
                    b = P.nb()
                    for c in range(8):
                        k = c % 2
                        P.op("act", lambda e: e.activation(out=fsq[k][:, :n], in_=xT[:, c, s:e_], func=AF.Square), r=xk([c], [tt]), w=[("fsq", k)])
                        P.op("pe", lambda e: e.matmul(ps[b][:, :n], lhsT=ones_b, rhs=fsq[k][:, :n], start=(c == 0), stop=(c == 7)),
                             r=[("fsq", k), "cmb"], w=[psk(b)])
                    r_ = frs[ti % 2]
                    rk = ("frs", ti % 2)
                    P.op("act", lambda e: e.activation(out=r_[:, :n], in_=ps[b][:, :n], func=AF.Ln, scale=1.0 / D, bias=EPS), r=[psk(b)], w=[rk])
                    P.op("act", lambda e: e.activation(out=r_[:, :n], in_=r_[:, :n], func=AF.Exp, scale=-0.5), r=[rk], w=[rk])

                def finB(ti, tt):
                    s, e_ = TT[tt]
                    n = e_ - s
                    r_ = frs[ti % 2]
                    rk = ("frs", ti % 2)
                    y_ = yT[ti % 2]
                    for c in range(8):
                        P.op("dve", lambda e: e.scalar_tensor_tensor(out=y_[:, c, :n], in0=xT[:, c, s:e_], scalar=pv[:, ofg + c:ofg + c + 1], in1=r_[:, :n],
                                                                     op0=ALU.mult, op1=ALU.mult), r=xk([c], [tt]) + [rk, "pv"], w=[("yT", ti % 2, c)])
                    for i in range(4):
                        k = obc_box[0] % 2
                        obc_box[0] += 1
                        for half in range(2):
                            b2 = P.nb()
                            for q in range(4):
                                c = half * 4 + q
                                P.op("pe", lambda e: e.transpose(out=ps[b2][:, q * 128:(q + 1) * 128], in_=y_[:, c, i * 128:(i + 1) * 128], identity=ident),
                                     r=[("yT", ti % 2, c), "cm"], w=[psk(b2)], inc=(q == 3))
                            if half == 0:
                                P.op("act", lambda e: e.copy(out=ob[k][:, 0:512], in_=ps[b2][:, :]), r=[psk(b2)], w=[("ob", k, 0)])
                            else:
                                P.op("dve", lambda e: e.tensor_copy(out=ob[k][:, 512:1024], in_=ps[b2][:, :]), r=[psk(b2)], w=[("ob", k, 1)])
                        tok = (s - C) + i * 128
                        P.dma("sp", out_d[tok:tok + 128, :], ob[k][:], "o%d" % k, r=[("ob", k, 0), ("ob", k, 1)])

                ftiles = [1, 2, 3, 4]
                for i in range(len(ftiles) + 1):
                    if i < len(ftiles):
                        finA(i, ftiles[i])
                    if i >= 1:
                        finB(i - 1, ftiles[i - 1])
                P.barrier()
        for k, v in P.cnt.items():
            if k not in P.E and v > 0:
                P.E["sp"].wait_ge(P.sem[k], v)
                P._pw["sp"].append((k, v))
        P.check_deadlock()
    return nc


def _chunked(v):
    v = np.asarray(v, np.float32)
    k = v.shape[-1] // 128
    v = v.reshape(v.shape[:-1] + (k, 128))
    return np.moveaxis(v, -1, 0)


def _rope_tables():
    t = np.arange(S)
    r = (t // 64).astype(np.float32)
    col = (t % 64).astype(np.float32)
    inv = (np.float32(10000.0) ** (-np.arange(0, 32, 2, dtype=np.float32) / np.float32(32))).astype(np.float32)
    cosT = np.zeros((128, S), np.float32)
    sinT = np.zeros((128, S), np.float32)
    for p in range(128):
        d = p % 64
        axis, half, f = d // 32, (d % 32) // 16, d % 16
        pos = r if axis == 0 else col
        ang = (pos * inv[f]).astype(np.float32)
        cosT[p] = np.cos(ang)
        sinT[p] = np.sin(ang) * (-1.0 if half == 0 else 1.0)
    return np.stack([cosT, sinT]).astype(np.float32)


def _consts():
    ident = np.eye(128, dtype=np.float32)
    perm = np.zeros((128, 128), np.float32)
    for m in range(128):
        half = (m % 32) // 16
        k = m + 16 if half == 0 else m - 16
        perm[k, m] = 1.0
    cmat = np.concatenate([ident, perm], axis=1)
    ones = np.ones((128, 128), np.float32)
    bones = np.zeros((128, 128), np.float32)
    bones[0:64, 0:64] = 1.0
    bones[64:128, 64:128] = 1.0
    sel = np.zeros((128, 16, 128), np.float32)
    for e in range(16):
        sel[e, e, :] = 1.0
    cmatb = np.concatenate([ones, bones, sel.reshape(128, 2048)], axis=1)
    return cmat, cmatb


def _pvec(inp, b):
    pvv = np.zeros((128, NPV), np.float32)

    def put(name, arr):
        o, s = PV_OFF[name]
        n = int(np.prod(s))
        pvv[:, o:o + n] = np.asarray(arr, np.float32).reshape(128, n)

    cc = np.stack([inp["c"][b], inp["c_ctx"]], axis=0)
    put("cc", np.moveaxis(_chunked(cc), 1, 2))
    put("adab", _chunked(inp["ada_b"]))
    put("n1g", _chunked(inp["norm1_g"]))
    put("n2g", _chunked(inp["norm2_g"]))
    put("fg", _chunked(inp["final_g"]))
    put("qg", np.tile(inp["q_norm_g"].T, (2, 1)))
    put("kg", np.tile(inp["k_norm_g"].T, (2, 1)))
    put("cw", _chunked(inp["conv_w"]))
    put("cb", _chunked(inp["conv_b"]))
    put("lba", _chunked(inp["lru_ba"]))
    put("lbx", _chunked(inp["lru_bx"]))
    put("lam", _chunked(inp["lru_lambda"]))
    put("aog", _chunked(inp["attn_out_g"]))
    put("log", _chunked(inp["lru_out_g"]))
    put("rb", np.broadcast_to(inp["router_b"].reshape(1, 1, 16), (128, NCH, 16)))
    return pvv


def _lru_blockdiag(inp):
    out = np.zeros((NL, 128, 2, 2, 4, 128), np.float32)
    for gi, name in enumerate(("lru_wa", "lru_wx")):
        w = np.asarray(inp[name], np.float32)
        for c in range(4):
            out[:, 0:64, :, gi, c, 0:64] = np.transpose(w[:, :, 2 * c], (0, 2, 1, 3))
            out[:, 64:128, :, gi, c, 64:128] = np.transpose(w[:, :, 2 * c + 1], (0, 2, 1, 3))
    return out.reshape(NL, 128, 2048)


_CACHE = {}


def _get_nc():
    if "nc" not in _CACHE:
        _CACHE["nc"] = build()
    return _CACHE["nc"]


def make_in_maps(inp):
    inp = {k: np.asarray(v) for k, v in inp.items()}
    cmat, cmatb = _consts()
    rope = _rope_tables()
    lrubd = _lru_blockdiag(inp)
    rwl = np.ascontiguousarray(_chunked(inp["router_w"].T).transpose(0, 2, 1)).reshape(128, 128)
    shared = dict(cmat=cmat, cmatb=cmatb, rope=rope, ada_w=np.ascontiguousarray(inp["ada_w"], np.float32),
                  w_in=np.ascontiguousarray(inp["w_in"], np.float32), lrubd=lrubd,
                  w_out=np.ascontiguousarray(inp["w_out"], np.float32), rw=rwl,
                  wg=np.ascontiguousarray(inp["exp_w_gate"], np.float32), wu=np.ascontiguousarray(inp["exp_w_up"], np.float32),
                  wd=np.ascontiguousarray(inp["exp_w_down"], np.float32))
    maps = []
    for b in range(8):
        m = dict(shared)
        m["x"] = np.ascontiguousarray(inp["x"][b], np.float32)
        m["ctx"] = np.ascontiguousarray(inp["ctx"][b], np.float32)
        m["pvec"] = _pvec(inp, b)
        maps.append(m)
    return maps


def kernel(**inputs):
    nc = _get_nc()
    maps = make_in_maps(inputs)
    res = run_bass_kernel_spmd(nc, maps, core_ids=list(range(8)))
    return np.stack([np.asarray(r["out"], np.float32) for r in res.results], axis=0)
```

```python
import contextlib
import os
import numpy as np
import concourse.bass as bass
import concourse.mybir as mybir
from concourse.bass_utils import run_bass_kernel_spmd

F32 = mybir.dt.float32
BF16 = mybir.dt.bfloat16
AF = mybir.ActivationFunctionType
ALU = mybir.AluOpType
AX = mybir.AxisListType

D = 1024
S = 2048
C = 256
T = S + C
NL = 2
NE = 16
DE = 512
INW = 1792
TT = [(0, 256), (256, 768), (768, 1280), (1280, 1792), (1792, 2304)]
NCH = T // 128
EPS = 1e-6

PV_ITEMS = [
    ("cc", (8, 2)), ("adab", (NL, 48)), ("n1g", (NL, 8)), ("n2g", (NL, 8)), ("fg", (8,)),
    ("qg", (NL,)), ("kg", (NL,)), ("cw", (NL, 4, 4)), ("cb", (NL, 4)),
    ("lba", (NL, 2, 4)), ("lbx", (NL, 2, 4)), ("lam", (NL, 2, 4)),
    ("aog", (NL, 4)), ("log", (NL, 4)), ("rb", (NCH, 16)),
]
PV_OFF = {}
_o = 0
for _n, _s in PV_ITEMS:
    PV_OFF[_n] = (_o, _s)
    _o += int(np.prod(_s))
NPV = _o


class Prog:
    def __init__(self, nc, es):
        self.nc = nc
        self.es = es
        self.E = dict(pe=nc.tensor, act=nc.scalar, dve=nc.vector, pool=nc.gpsimd, sp=nc.sync)
        self.sem = {}
        self.cnt = {}
        for k in self.E:
            self.sem[k] = es.enter_context(nc.semaphore("sem_" + k))
            self.cnt[k] = 0
        self.seen = {k: {} for k in self.E}
        self.res = {}
        self.q = {k: [] for k in self.E}
        self._pw = {k: [] for k in self.E}
        self._bank = 0
        self.reserved = set()

    def _deps(self, eng, r, w, skip=None):
        need = {}
        for key in r:
            st = self.res.get(key)
            if st and st[0] is not None:
                k, v = st[0]
                if need.get(k, 0) < v:
                    need[k] = v
        for key in w:
            st = self.res.get(key)
            if st:
                if st[0] is not None:
                    k, v = st[0]
                    if need.get(k, 0) < v:
                        need[k] = v
                for k, v in st[1].items():
                    if need.get(k, 0) < v:
                        need[k] = v
        for k, v in need.items():
            if k == skip:
                continue
            if k == eng:
                if eng == "pe" or v > self.cnt[eng]:
                    continue
            if self.seen[eng].get(k, 0) < v:
                self.E[eng].wait_ge(self.sem[k], v)
                self.seen[eng][k] = v
                self._pw[eng].append((k, v))

    def _mark(self, tag, r, w):
        k, v = tag
        for key in r:
            st = self.res.setdefault(key, [None, {}])
            if st[1].get(k, 0) < v:
                st[1][k] = v
        for key in w:
            self.res[key] = [tag, {}]

    def op(self, eng, fn, r=(), w=(), inc=True):
        pr = [k for k in r if isinstance(k, tuple) and k[0] == "ps"]
        if pr:
            w = list(w) + pr
        self._deps(eng, r, w)
        inst = fn(self.E[eng])
        self.q[eng].append((self._pw[eng], (eng, 1) if inc else None))
        self._pw[eng] = []
        if inc:
            self.cnt[eng] += 1
            inst.then_inc(self.sem[eng], 1)
            tag = (eng, self.cnt[eng])
        else:
            tag = (eng, self.cnt[eng] + 1)
        self._mark(tag, r, w)
        return inst

    def dma(self, q, out, in_, semkey, r=(), w=(), **kw):
        if semkey not in self.sem:
            self.sem[semkey] = self.es.enter_context(self.nc.semaphore("d_" + semkey))
            self.cnt[semkey] = 0
        self._deps(q, r, w, skip=semkey)
        inst = self.E[q].dma_start(out=out, in_=in_, **kw)
        self.q[q].append((self._pw[q], (semkey, 16)))
        self._pw[q] = []
        self.cnt[semkey] += 16
        inst.then_inc(self.sem[semkey], 16)
        self._mark((semkey, self.cnt[semkey]), r, w)
        return inst

    def barrier(self):
        for e in self.E:
            for k, v in self.cnt.items():
                if k != e and v > 0 and self.seen[e].get(k, 0) < v:
                    self.E[e].wait_ge(self.sem[k], v)
                    self.seen[e][k] = v
                    self._pw[e].append((k, v))

    def check_deadlock(self):
        val = {k: 0 for k in self.cnt}
        pos = {k: 0 for k in self.E}
        for e in self.E:
            if self._pw[e]:
                self.q[e].append((self._pw[e], None))
                self._pw[e] = []
        progress = True
        while progress:
            progress = False
            for e in self.E:
                ql = self.q[e]
                while pos[e] < len(ql):
                    waits, inc = ql[pos[e]]
                    if all(val[k] >= v for k, v in waits):
                        if inc is not None:
                            val[inc[0]] += inc[1]
                        pos[e] += 1
                        progress = True
                    else:
                        break
        stuck = {e: (pos[e], len(self.q[e]), self.q[e][pos[e]][0]) for e in self.E if pos[e] < len(self.q[e])}
        if stuck:
            raise RuntimeError("DEADLOCK in semaphore plan: %r ; vals=%r" % (stuck, {k: val[k] for e in stuck for k, _ in stuck[e][2]}))

    def nb(self):
        while True:
            b = self._bank
            self._bank = (self._bank + 1) % 8
            if b not in self.reserved:
                return b


def build(stage=99, dbg=False):
    nc = bass.Bass("TRN2", target_bir_lowering=False)

    def dten(name, shape, dty=F32, kind="ExternalInput"):
        return nc.dram_tensor(name, shape, dty, kind=kind).ap()

    x_d = dten("x", [S, D])
    ctx_d = dten("ctx", [C, D])
    pv_d = dten("pvec", [128, NPV])
    cm_d = dten("cmat", [128, 256])
    cmb_d = dten("cmatb", [128, 256 + 2048])
    rope_d = dten("rope", [2, 128, S])
    adaw_d = dten("ada_w", [NL, D, 6 * D])
    win_d = dten("w_in", [NL, D, INW])
    lru_d = dten("lrubd", [NL, 128, 2048])
    wout_d = dten("w_out", [NL, D, D])
    rw_d = dten("rw", [128, 128])
    wg_d = dten("wg", [NL, NE, D, DE])
    wu_d = dten("wu", [NL, NE, D, DE])
    wd_d = dten("wd", [NL, NE, DE, D])
    out_d = dten("out", [S, D], kind="ExternalOutput")
    if dbg:
        dbgf_d = dten("dbgf", [128, 8 * T], F32, kind="ExternalOutput")
        dbgb_d = dten("dbgb", [128, 8 * T], BF16, kind="ExternalOutput")

    with contextlib.ExitStack() as es:
        P = Prog(nc, es)

        def sb(stack, name, shape, dty):
            return stack.enter_context(nc.sbuf_tensor("s_" + name, shape, dty))

        ps = [es.enter_context(nc.psum_tensor("ps%d" % i, [128, 512], F32)) for i in range(8)]

        def psk(b):
            return ("ps", b)

        xT = sb(es, "xT", [128, 8, T], F32)
        hT = sb(es, "hT", [128, 8, T], BF16)
        pv = sb(es, "pv", [128, NPV], F32)
        cm = sb(es, "cm", [128, 256], F32)
        cmb = sb(es, "cmb", [128, 256], BF16)
        scb = sb(es, "scb", [128, 8, 2], BF16)
        modv = sb(es, "modv", [128, NL, 48, 2], F32)
        gsv = sb(es, "gsv", [128, NL, 2, 8, 2], F32)
        hsp = sb(es, "hsp", [128, NL * 2 * 4], F32)
        hba = sb(es, "hba", [128, NL * 2 * 4], F32)
        hbx = sb(es, "hbx", [128, NL * 2 * 4], F32)
        rw = sb(es, "rw", [128, 128], F32)
        smalltmp = sb(es, "smalltmp", [128, 64], F32)

        ident = cm[:, 0:128]
        perm = cm[:, 128:256]
        ones_b = cmb[:, 0:128]
        bones_b = cmb[:, 128:256]

        def pvv(name):
            o, s = PV_OFF[name]
            n = int(np.prod(s))
            return pv[:, o:o + n]

        def pvi(name, *idx):
            o, s = PV_OFF[name]
            flat = 0
            for i, d in zip(idx, s):
                flat = flat * d + i
            return pv[:, o + flat:o + flat + 1]

        P.dma("sp", pv[:], pv_d, "c_pv", w=["pv"])
        P.dma("sp", cm[:], cm_d, "c_cm", w=["cm"])
        P.dma("sp", rw[:], rw_d, "c_rw", w=["rw"])
        P.dma("pool", cmb[:], cmb_d[:, 0:256], "c_cmb", w=["cmb"])

        o_cc = PV_OFF["cc"][0]
        P.op("act", lambda e: e.activation(out=scb[:].rearrange("p a b -> p (a b)"), in_=pv[:, o_cc:o_cc + 16],
                                           func=AF.Silu), r=["pv"], w=["scb"])
        P.op("act", lambda e: e.activation(out=smalltmp[:, 0:16], in_=pvv("lam"), func=AF.Exp, scale=-1.0),
             r=["pv"], w=["smalltmp"])
        P.op("act", lambda e: e.activation(out=smalltmp[:, 0:16], in_=smalltmp[:, 0:16], func=AF.Ln, bias=1.0),
             r=["smalltmp"], w=["smalltmp"])
        P.op("dve", lambda e: e.tensor_scalar(out=hsp[:], in0=smalltmp[:, 0:16], scalar1=-4.0, scalar2=None,
                                              op0=ALU.mult), r=["smalltmp"], w=["hsp"])
        P.op("dve", lambda e: e.tensor_scalar(out=hba[:], in0=pvv("lba"), scalar1=0.5, scalar2=None,
                                              op0=ALU.mult), r=["pv"], w=["hba"])
        P.op("dve", lambda e: e.tensor_scalar(out=hbx[:], in0=pvv("lbx"), scalar1=0.5, scalar2=None,
                                              op0=ALU.mult), r=["pv"], w=["hbx"])

        with contextlib.ExitStack() as s0:
            adaw = [sb(s0, "adaw%d" % i, [128, 8, 256], BF16) for i in range(2)]
            xin = [sb(s0, "xin%d" % i, [128, D], F32) for i in range(2)]
            def load_x(tc):
                k = tc % 2
                src = ctx_d[tc * 128:(tc + 1) * 128, :] if tc < 2 else x_d[(tc - 2) * 128:(tc - 1) * 128, :]
                P.dma("sp", xin[k][:], src, "xin%d" % k, w=[("xin", k)])
                tt = 0 if tc < 2 else 1 + (tc - 2) // 4
                for half in range(2):
                    b = P.nb()
                    for q in range(4):
                        c = half * 4 + q
                        P.op("pe", lambda e: e.transpose(out=ps[b][:, q * 128:(q + 1) * 128],
                                                         in_=xin[k][:, c * 128:(c + 1) * 128], identity=ident),
                             r=[("xin", k), "cm"], w=[psk(b)], inc=(q == 3))
                    eng = "act" if half == 0 else "dve"
                    if eng == "act":
                        P.op("act", lambda e: e.copy(out=xT[:, half * 4:half * 4 + 4, tc * 128:(tc + 1) * 128],
                                                     in_=ps[b][:].rearrange("p (q t) -> p q t", q=4)),
                             r=[psk(b)], w=[("xT", c_, tt) for c_ in range(half * 4, half * 4 + 4)])
                    else:
                        P.op("dve", lambda e: e.tensor_copy(out=xT[:, half * 4:half * 4 + 4, tc * 128:(tc + 1) * 128],
                                                            in_=ps[b][:].rearrange("p (q t) -> p q t", q=4)),
                             r=[psk(b)], w=[("xT", c_, tt) for c_ in range(half * 4, half * 4 + 4)])

            xi = 0
            NPIECE = 24
            mod_state = {}

            def mods_begin(l):
                pm = P.nb()
                P.reserved.add(pm)
                mod_state[l] = pm

            def mods_dma(l, j, bufs):
                k = j % 2
                P.dma("pool", bufs[k][:], adaw_d[l][:, j * 256:(j + 1) * 256].rearrange("(kc p) n -> p kc n", p=128),
                      "adaw%d" % k, w=[("adaw", k)])

            def mods_piece(l, j, bufs, dma=True):
                pm = mod_state[l]
                k = j % 2
                if dma:
                    mods_dma(l, j, bufs)
                for fc in range(2):
                    oc = j * 2 + fc
                    for kc in range(8):
                        P.op("pe", lambda e: e.matmul(ps[pm][:, oc * 2:oc * 2 + 2], lhsT=bufs[k][:, kc, fc * 128:(fc + 1) * 128],
                                                      rhs=scb[:, kc, :], start=(kc == 0), stop=(kc == 7)),
                             r=[("adaw", k), "scb"], w=[psk(pm)], inc=(kc == 7))

            def mods_finish(l, m0=0, m1=6, release=True):
                pm = mod_state[l]
                o_ab = PV_OFF["adab"][0] + l * 48
                for jj in range(2):
                    P.op("dve", lambda e: e.tensor_tensor(out=modv[:, l, m0 * 8:m1 * 8, jj],
                                                          in0=ps[pm][:, 0:96].rearrange("p (a b) -> p a b", b=2)[:, m0 * 8:m1 * 8, jj],
                                                          in1=pv[:, o_ab + m0 * 8:o_ab + m1 * 8], op=ALU.add),
                         r=[psk(pm), "pv"], w=[("modv", m_) for m_ in range(m0, m1)])
                if release:
                    P.reserved.discard(pm)
                for n_, (gname, mi) in enumerate((("n1g", 1), ("n2g", 4))):
                    if not (m0 <= mi < m1):
                        continue
                    og = PV_OFF[gname][0] + l * 8
                    for jj in range(2):
                        P.op("dve", lambda e: e.scalar_tensor_tensor(out=gsv[:, l, n_, :, jj], in0=modv[:, l, mi * 8:(mi + 1) * 8, jj],
                                                                     scalar=1.0, in1=pv[:, og:og + 8], op0=ALU.add, op1=ALU.mult),
                             r=[("modv", mi), "pv"], w=[("gsv", n_)])

            mods_begin(0)
            for j in range(8):
                mods_piece(0, j, adaw)
                for _ in range(2):
                    if xi < NCH:
                        load_x(xi); xi += 1
            mods_finish(0, 0, 2, release=False)
            while xi < NCH:
                load_x(xi); xi += 1
            P.barrier()

        if dbg and stage == 0:
            P.dma("sp", dbgf_d[:, 0:8 * T], xT[:].rearrange("p a b -> p (a b)"), "dbg", r=[("xT", c_, t_) for c_ in range(8) for t_ in range(5)])
            P.dma("sp", out_d[0:128, 0:192], modv[:].rearrange("p a b c -> p (a b c)"), "dbg", r=[("modv", m_) for m_ in range(6)])
            P.dma("sp", out_d[128:256, 0:64], gsv[:].rearrange("p a b c d -> p (a b c d)"), "dbg", r=[("gsv", 0), ("gsv", 1)])

        def xk(cs, tts):
            return [("xT", c_, t_) for c_ in cs for t_ in tts]

        def hk(cs, tts):
            return [("hT", c_, t_) for c_ in cs for t_ in tts]

        def norm_mod(l, nidx, tiles, stk, h2f=None, after_tile=None, post_rstd=None):
            shi = 0 if nidx == 0 else 3
            sq = [sb(stk, "nsq%d_%d_%d" % (l, nidx, i), [128, 512], BF16) for i in range(2)]
            rs = [sb(stk, "nrs%d_%d_%d" % (l, nidx, i), [128, 512], F32) for i in range(2)]
            tmp = [sb(stk, "ntmp%d_%d_%d" % (l, nidx, i), [128, 512], F32) for i in range(2)]
            def stageA(ti, tt):
                s, e_ = TT[tt]
                n = e_ - s
                b = P.nb()
                rk = ("nrs", ti % 2)
                for c in range(8):
                    k = c % 2
                    P.op("act", lambda e: e.activation(out=sq[k][:, :n], in_=xT[:, c, s:e_], func=AF.Square),
                         r=xk([c], [tt]), w=[("nsq", k)])
                    P.op("pe", lambda e: e.matmul(ps[b][:, :n], lhsT=ones_b, rhs=sq[k][:, :n], start=(c == 0), stop=(c == 7)),
                         r=[("nsq", k), "cmb"], w=[psk(b)])
                r_ = rs[ti % 2]
                P.op("act", lambda e: e.activation(out=r_[:, :n], in_=ps[b][:, :n], func=AF.Ln, scale=1.0 / D, bias=EPS),
                     r=[psk(b)], w=[rk])
                P.op("act", lambda e: e.activation(out=r_[:, :n], in_=r_[:, :n], func=AF.Exp, scale=-0.5), r=[rk], w=[rk])
                if post_rstd is not None:
                    post_rstd(ti, tt, r_, rk)

            def stageB(ti, tt):
                s, e_ = TT[tt]
                n = e_ - s
                jj = 1 if tt == 0 else 0
                rk = ("nrs", ti % 2)
                r_ = rs[ti % 2]
                for c in range(8):
                    k = c % 2
                    P.op("dve", lambda e: e.tensor_tensor(out=tmp[k][:, :n], in0=xT[:, c, s:e_], in1=r_[:, :n], op=ALU.mult),
                         r=xk([c], [tt]) + [rk], w=[("ntmp", k)])
                    if h2f is None:
                        P.op("act", lambda e: e.activation(out=hT[:, c, s:e_], in_=tmp[k][:, :n], func=AF.Identity,
                                                           scale=gsv[:, l, nidx, c, jj:jj + 1], bias=modv[:, l, shi * 8 + c, jj:jj + 1]),
                             r=[("ntmp", k), ("gsv", nidx), ("modv", shi)], w=hk([c], [tt]))
                    else:
                        P.op("act", lambda e: e.activation(out=h2f[:, c, :n], in_=tmp[k][:, :n], func=AF.Identity,
                                                           scale=gsv[:, l, nidx, c, jj:jj + 1], bias=modv[:, l, shi * 8 + c, jj:jj + 1]),
                             r=[("ntmp", k), ("gsv", nidx), ("modv", shi)], w=[("h2f", c)])
                        P.op("dve", lambda e: e.tensor_copy(out=hT[:, c, s:e_], in_=h2f[:, c, :n]),
                             r=[("h2f", c)], w=hk([c], [tt]))
                if after_tile is not None:
                    after_tile(tt)

            nt = len(tiles)
            for i in range(nt + 1):
                if i < nt:
                    stageA(i, tiles[i])
                if i - 1 >= 0:
                    stageB(i - 1, tiles[i - 1])

        def dump_and_finish():
            pass

        for l in range(NL):
            if dbg and stage == 0:
                break
            last = (l == NL - 1)
            all_tiles = [0, 1, 2, 3, 4]
            lat_tiles = [1, 2, 3, 4]
            q_tiles = lat_tiles if last else all_tiles
            with contextlib.ExitStack() as sm:
                with contextlib.ExitStack() as sn:
                    norm_mod(l, 0, all_tiles, sn)
                    P.barrier()
                if dbg and stage == 1 and l == 0:
                    P.dma("sp", dbgb_d[:, 0:8 * T], hT[:].rearrange("p a b -> p (a b)"), "dbg", r=hk(range(8), range(5)))
                    break
                NWP = 3
                wp = [sb(sm, "wp%d_%d" % (l, i), [128, 8, 128], BF16) for i in range(NWP)]
                wpc = [0]

                def wp_load(src_list):
                    k = wpc[0] % NWP
                    wpc[0] += 1
                    for (c0, c1, src) in src_list:
                        P.dma("pool", wp[k][:, :, c0:c1], src.rearrange("(kc p) n -> p kc n", p=128), "wp%d" % k, w=[("wp", k)])
                    return k

                def proj(k, tt, b):
                    s, e_ = TT[tt]
                    n = e_ - s
                    for kc in range(8):
                        P.op("pe", lambda e: e.matmul(ps[b][:, :n], lhsT=wp[k][:, kc, :], rhs=hT[:, kc, s:e_], start=(kc == 0), stop=(kc == 7)),
                             r=[("wp", k)] + hk([kc], [tt]), w=[psk(b)], inc=(kc == 7))

                mixR = sb(sm, "mixR%d" % l, [128, 4, T], BF16)
                mixA_box = [None]

                def MX(c_, p0=0, p1=128, c0=None, c1=None):
                    t_ = mixA_box[0] if c_ < 4 else mixR
                    return t_[p0:p1, c_ % 4, c0:c1]

                def mk(cs, tts):
                    return [("mixT", c_, t_) for c_ in cs for t_ in tts]

                with contextlib.ExitStack() as sl:
                    lw = sb(sl, "lw%d" % l, [128, 16, 128], BF16)
                    P.dma("pool", lw[:], lru_d[l].rearrange("p (a m) -> p a m", m=128), "c_lw", w=["lw"])
                    uh = sb(sl, "uh%d" % l, [128, T], F32)
                    cu = sb(sl, "cu%d" % l, [128, T], F32)
                    cub = sb(sl, "cub%d" % l, [128, T], BF16)
                    TA = sb(sl, "TA%d" % l, [128, T], F32)
                    TX = sb(sl, "TX%d" % l, [128, T], F32)
                    NM = sb(sl, "NM%d" % l, [128, T], F32)
                    gl = [sb(sl, "gl%d_%d" % (l, i), [128, 512], F32) for i in range(2)]
                    if l == 0:
                        adawL = [sb(sl, "adawS%d" % i, [128, 8, 256], BF16) for i in range(2)]
                        lp = [8]
                        lq = [8]
                        for _ in range(2):
                            mods_dma(0, lp[0], adawL)
                            lp[0] += 1
                    for c in range(4):
                        if l == 0:
                            for _ in range(4):
                                if lq[0] < NPIECE:
                                    mods_piece(0, lq[0], adawL, dma=False)
                                    lq[0] += 1
                                    if lp[0] < NPIECE:
                                        mods_dma(0, lp[0], adawL)
                                        lp[0] += 1
                        ku = wp_load([(0, 128, win_d[l][:, 768 + c * 128:768 + (c + 1) * 128])])
                        kg_ = wp_load([(0, 128, win_d[l][:, 1280 + c * 128:1280 + (c + 1) * 128])])
                        for tt in all_tiles:
                            s, e_ = TT[tt]
                            b = P.nb()
                            proj(ku, tt, b)
                            P.op("act", lambda e: e.copy(out=NM[:, s:e_], in_=ps[b][:, :e_ - s]), r=[psk(b)], w=["NM"])
                        P.op("dve", lambda e: e.tensor_scalar(out=cu[:, 0:T], in0=NM[:, 0:T], scalar1=pvi("cw", l, 2, c), scalar2=pvi("cb", l, c),
                                                              op0=ALU.mult, op1=ALU.add), r=["NM", "pv"], w=["cu"])
                        for (ga, gz) in ((0, C), (C, T)):
                            P.op("dve", lambda e: e.scalar_tensor_tensor(out=cu[:, ga + 2:gz], in0=NM[:, ga:gz - 2], scalar=pvi("cw", l, 0, c),
                                                                         in1=cu[:, ga + 2:gz], op0=ALU.mult, op1=ALU.add), r=["NM", "cu", "pv"], w=["cu"])
                            P.op("dve", lambda e: e.scalar_tensor_tensor(out=cu[:, ga + 1:gz], in0=NM[:, ga:gz - 1], scalar=pvi("cw", l, 1, c),
                                                                         in1=cu[:, ga + 1:gz], op0=ALU.mult, op1=ALU.add), r=["NM", "cu", "pv"], w=["cu"])
                            P.op("dve", lambda e: e.scalar_tensor_tensor(out=cu[:, ga:gz - 1], in0=NM[:, ga + 1:gz], scalar=pvi("cw", l, 3, c),
                                                                         in1=cu[:, ga:gz - 1], op0=ALU.mult, op1=ALU.add), r=["NM", "cu", "pv"], w=["cu"])
                        P.op("act", lambda e: e.copy(out=cub[:, :], in_=cu[:, :]), r=["cu"], w=["cub"])
                        for d in range(2):
                            li = (l * 2 + d) * 4 + c
                            for tt in all_tiles:
                                s, e_ = TT[tt]
                                n = e_ - s
                                ba_ = P.nb()
                                P.op("pe", lambda e: e.matmul(ps[ba_][:, :n], lhsT=lw[:, (d * 2 + 0) * 4 + c, :], rhs=cub[:, s:e_], start=True, stop=True),
                                     r=["lw", "cub"], w=[psk(ba_)])
                                P.op("act", lambda e: e.activation(out=TA[:, s:e_], in_=ps[ba_][:, :n], func=AF.Tanh, scale=0.5, bias=hba[:, li:li + 1]),
                                     r=[psk(ba_), "hba"], w=["TA"])
                            P.op("act", lambda e: e.activation(out=TA[:, :], in_=TA[:, :], func=AF.Exp, scale=hsp[:, li:li + 1], bias=hsp[:, li:li + 1]),
                                 r=["TA", "hsp"], w=["TA"])
                            P.op("dve", lambda e: e.scalar_tensor_tensor(out=NM[:, :], in0=TA[:, :], scalar=-1.0, in1=TA[:, :], op0=ALU.mult, op1=ALU.mult),
                                 r=["TA"], w=["NM"])
                            for tt in all_tiles:
                                s, e_ = TT[tt]
                                n = e_ - s
                                bx_ = P.nb()
                                P.op("pe", lambda e: e.matmul(ps[bx_][:, :n], lhsT=lw[:, (d * 2 + 1) * 4 + c, :], rhs=cub[:, s:e_], start=True, stop=True),
                                     r=["lw", "cub"], w=[psk(bx_)])
                                P.op("act", lambda e: e.activation(out=TX[:, s:e_], in_=ps[bx_][:, :n], func=AF.Tanh, scale=0.5, bias=hbx[:, li:li + 1]),
                                     r=[psk(bx_), "hbx"], w=["TX"])
                            P.op("act", lambda e: e.activation(out=NM[:, :], in_=NM[:, :], func=AF.Sqrt, scale=0.25, bias=0.25), r=["NM"], w=["NM"])
                            P.op("dve", lambda e: e.scalar_tensor_tensor(out=TX[:, :], in0=TX[:, :], scalar=1.0, in1=cu[:, :], op0=ALU.add, op1=ALU.mult),
                                 r=["TX", "cu"], w=["TX"])
                            P.op("dve", lambda e: e.tensor_tensor(out=TX[:, :], in0=TX[:, :], in1=NM[:, :], op=ALU.mult), r=["TX", "NM"], w=["TX"])
                            if d == 0:
                                P.op("dve", lambda e: e.tensor_tensor_scan(out=uh[:, 0:T], data0=TA[:, 0:T], data1=TX[:, 0:T], initial=0.0,
                                                                           op0=ALU.mult, op1=ALU.add), r=["TA", "TX", "cu"], w=["uh"])
                            else:
                                P.op("dve", lambda e: e.tensor_tensor_scan(out=NM[:, 0:C][:, ::-1], data0=TA[:, 0:C][:, ::-1], data1=TX[:, 0:C][:, ::-1],
                                                                           initial=0.0, op0=ALU.mult, op1=ALU.add), r=["TA", "TX"], w=["NM"])
                                P.op("dve", lambda e: e.tensor_tensor_scan(out=NM[:, C:T][:, ::-1], data0=TA[:, C:T][:, ::-1], data1=TX[:, C:T][:, ::-1],
                                                                           initial=NM[:, 0:1], op0=ALU.mult, op1=ALU.add), r=["TA", "TX", "NM"], w=["NM"])
                                P.op("dve", lambda e: e.tensor_tensor(out=uh[:, :], in0=uh[:, :], in1=NM[:, :], op=ALU.add), r=["uh", "NM"], w=["uh"])
                        for ti, tt in enumerate(q_tiles):
                            s, e_ = TT[tt]
                            n = e_ - s
                            b = P.nb()
                            proj(kg_, tt, b)
                            g_ = gl[ti % 2]
                            P.op("act", lambda e: e.activation(out=g_[:, :n], in_=ps[b][:, :n], func=AF.Gelu_apprx_tanh), r=[psk(b)], w=[("gl", ti % 2)])
                            P.op("dve", lambda e: e.tensor_tensor(out=MX(4 + c, c0=s, c1=e_), in0=uh[:, s:e_], in1=g_[:, :n], op=ALU.mult),
                                 r=["uh", ("gl", ti % 2)], w=mk([4 + c], [tt]))
                    if l == 0:
                        assert lq[0] == NPIECE
                        mods_finish(0, 2, 6)
                    P.barrier()

                if dbg and stage == 2 and l == 0:
                    (mixA_box[0] is not None and P.dma("sp", dbgb_d[:, 0:4 * T], mixA_box[0][:].rearrange("p a b -> p (a b)"), "dbg", r=mk(range(4), range(5)))); P.dma("sp", dbgb_d[:, 4 * T:8 * T], mixR[:].rearrange("p a b -> p (a b)"), "dbg", r=mk(range(4, 8), range(5)))
                    break

                mixA_box[0] = sb(sm, "mixA%d" % l, [128, 4, T], BF16)
                with contextlib.ExitStack() as sa:
                    ropeT = sb(sa, "rope%d" % l, [128, 2, S], BF16)
                    for a_ in range(2):
                        P.dma("pool", ropeT[:, a_, :], rope_d[a_], "c_rope", w=["rope"], max_dma_last_dim=4096)
                    ATT_STOP = int(os.environ.get("ATT_STOP", "9"))
                    kT2 = sb(sa, "kT2_%d" % l, [128, 2, T], BF16)
                    vaug = sb(sa, "vaug%d" % l, [128, NCH, 2, 128], BF16)
                    qZ = [sb(sa, "qZ%d_%d" % (l, i), [128, T], BF16) for i in range(2)]
                    NPT = 3
                    pt = [sb(sa, "pt%d_%d" % (l, i), [128, 512], BF16) for i in range(NPT)]
                    rc = [sb(sa, "rc%d_%d" % (l, i), [64, 512], F32) for i in range(2)]
                    sqh = sb(sa, "sqh%d" % l, [128, 512], BF16)
                    rsh = sb(sa, "rsh%d" % l, [128, 512], F32)
                    qn = sb(sa, "qn%d" % l, [128, 512], F32)
                    P.op("dve", lambda e: e.memset(vaug[:, :, :, 64:128], 1.0), w=["vones"])
                    P.op("dve", lambda e: e.memset(qZ[0][64:128, :], 0.0), w=["qz0"])
                    P.op("dve", lambda e: e.memset(qZ[1][0:64, :], 0.0), w=["qz1"])

                    sqh2 = [sqh, sb(sa, "sqhB%d" % l, [128, 512], BF16)]
                    rsh2 = [rsh, sb(sa, "rshB%d" % l, [128, 512], F32)]
                    qn2 = [qn, sb(sa, "qnB%d" % l, [128, 512], F32)]
                    hnc = [0]

                    def pnr_pipeline(items):
                        st = {}
                        n_it = len(items)
                        for i in range(n_it + 2):
                            if i < n_it:
                                k_, tt, gname, dst, dkeys = items[i]
                                b = P.nb()
                                proj(k_, tt, b)
                                st[i] = dict(b=b)
                            if 0 <= i - 1 < n_it:
                                k_, tt, gname, dst, dkeys = items[i - 1]
                                s, e_ = TT[tt]
                                n = e_ - s
                                b = st[i - 1]["b"]
                                u_ = hnc[0] % 2
                                hnc[0] += 1
                                st[i - 1]["u"] = u_
                                sq_, rs_, q_ = sqh2[u_], rsh2[u_], qn2[u_]
                                P.op("act", lambda e: e.activation(out=sq_[:, :n], in_=ps[b][:, :n], func=AF.Square), r=[psk(b)], w=[("sqh", u_)])
                                b2 = P.nb()
                                P.op("pe", lambda e: e.matmul(ps[b2][:, :n], lhsT=bones_b, rhs=sq_[:, :n], start=True, stop=True), r=[("sqh", u_), "cmb"], w=[psk(b2)])
                                P.op("act", lambda e: e.activation(out=rs_[:, :n], in_=ps[b2][:, :n], func=AF.Ln, scale=1.0 / 64, bias=EPS), r=[psk(b2)], w=[("rsh", u_)])
                                P.op("act", lambda e: e.activation(out=rs_[:, :n], in_=rs_[:, :n], func=AF.Exp, scale=-0.5), r=[("rsh", u_)], w=[("rsh", u_)])
                                P.op("dve", lambda e: e.scalar_tensor_tensor(out=q_[:, :n], in0=ps[b][:, :n], scalar=pvi(gname, l), in1=rs_[:, :n],
                                                                             op0=ALU.mult, op1=ALU.mult), r=[psk(b), ("rsh", u_), "pv"], w=[("qn", u_)])
                                if tt > 0:
                                    b3 = P.nb()
                                    P.op("pe", lambda e: e.matmul(ps[b3][:, :n], lhsT=perm, rhs=q_[:, :n], start=True, stop=True), r=[("qn", u_), "cm"], w=[psk(b3)])
                                    st[i - 1]["b3"] = b3
                            if 0 <= i - 2 < n_it:
                                k_, tt, gname, dst, dkeys = items[i - 2]
                                s, e_ = TT[tt]
                                n = e_ - s
                                u_ = st[i - 2]["u"]
                                sq_, rs_, q_ = sqh2[u_], rsh2[u_], qn2[u_]
                                if tt > 0:
                                    b3 = st[i - 2]["b3"]
                                    P.op("dve", lambda e: e.tensor_tensor(out=q_[:, :n], in0=q_[:, :n], in1=ropeT[:, 0, s - C:e_ - C], op=ALU.mult),
                                         r=[("qn", u_), "rope"], w=[("qn", u_)])
                                    P.op("dve", lambda e: e.tensor_tensor(out=rs_[:, :n], in0=ps[b3][:, :n], in1=ropeT[:, 1, s - C:e_ - C], op=ALU.mult),
                                         r=[psk(b3), "rope"], w=[("rsh", u_)])
                                    for (p0, p1, d_) in dst:
                                        P.op("dve", lambda e: e.tensor_tensor(out=d_, in0=q_[p0:p1, :n], in1=rs_[p0:p1, :n], op=ALU.add),
                                             r=[("qn", u_), ("rsh", u_)], w=dkeys)
                                else:
                                    for (p0, p1, d_) in dst:
                                        P.op("act", lambda e: e.copy(out=d_, in_=q_[p0:p1, :n]), r=[("qn", u_)], w=dkeys)

                    k_items = []
                    for kvh in range(2):
                        c0 = 512 + kvh * 64
                        kk = wp_load([(0, 64, win_d[l][:, c0:c0 + 64]), (64, 128, win_d[l][:, c0:c0 + 64])])
                        for tt in all_tiles:
                            s, e_ = TT[tt]
                            k_items.append((kk, tt, "kg", [(0, 128, kT2[:, kvh, s:e_])], [("kT2", kvh, tt)]))
                    pnr_pipeline(k_items)
                    kv_ = wp_load([(0, 128, win_d[l][:, 640:768])])
                    for g4 in range(5):
                        chunks = list(range(g4 * 4, min(g4 * 4 + 4, NCH)))
                        b = P.nb()
                        for i, tc in enumerate(chunks):
                            tt_ = 0 if tc < 2 else 1 + (tc - 2) // 4
                            for kc in range(8):
                                P.op("pe", lambda e: e.matmul(ps[b][:, i * 128:(i + 1) * 128], lhsT=hT[:, kc, tc * 128:(tc + 1) * 128], rhs=wp[kv_][:, kc, :],
                                                              start=(kc == 0), stop=(kc == 7)),
                                     r=[("wp", kv_)] + hk([kc], [tt_]), w=[psk(b)], inc=(kc == 7 and i == len(chunks) - 1))
                        nch = len(chunks)
                        src = ps[b][:, 0:nch * 128].rearrange("p (i k d) -> p i k d", i=nch, k=2)
                        tc0 = chunks[0]
                        P.op("act", lambda e: e.copy(out=vaug[:, tc0:tc0 + nch, :, 0:64], in_=src), r=[psk(b)], w=[("vaug", g4, 0)])
                    vkeys = [("vaug", g4, 0) for g4 in range(5)] + ["vones"]

                    if ATT_STOP <= 2:
                        P.barrier(); break
                    SB = [0, 1, 2, 3]
                    sbc = [0]
                    ptc = [0]
                    qtc = [0]
                    for c in range(4):
                        kvh = c // 2
                        kq = wp_load([(0, 128, win_d[l][:, c * 128:(c + 1) * 128])])
                        pnr_pipeline([(kq, tt, "qg", [(0, 64, qZ[0][0:64, TT[tt][0]:TT[tt][1]]), (64, 128, qZ[1][64:128, TT[tt][0]:TT[tt][1]])], [("qT", tt)])
                                      for tt in q_tiles])
                        if ATT_STOP <= 3:
                            P.barrier(); break
                        gsteps = []
                        for tt in q_tiles:
                            kchunks = [0, 1] if tt == 0 else list(range(NCH))
                            po = (4, 5) if qtc[0] % 2 == 0 else (6, 7)
                            qtc[0] += 1
                            for kc in kchunks:
                                for j in range(2):
                                    gsteps.append((tt, po, j, kc, kc == kchunks[0], kc == kchunks[-1]))

                        def emit_qk(tt, j, kc):
                            s, e_ = TT[tt]
                            n = e_ - s
                            sbk = SB[sbc[0] % 4]
                            sbc[0] += 1
                            ktt = 0 if kc < 2 else 1 + (kc - 2) // 4
                            P.op("pe", lambda e: e.matmul(ps[sbk][:, :n], lhsT=kT2[:, kvh, kc * 128:(kc + 1) * 128],
                                                          rhs=qZ[j][:, s:e_], start=True, stop=True),
                                 r=[("kT2", kvh, ktt), ("qT", tt), "qz0", "qz1"], w=[psk(sbk)])
                            return sbk

                        def emit_pv(tt, po, j, kc, first, lastk, sbk):
                            s, e_ = TT[tt]
                            n = e_ - s
                            pk = ptc[0] % NPT
                            ptc[0] += 1
                            P.op("act", lambda e: e.activation(out=pt[pk][:, :n], in_=ps[sbk][:, :n], func=AF.Exp, scale=0.125),
                                 r=[psk(sbk)], w=[("pt", pk)])
                            P.op("pe", lambda e: e.matmul(ps[po[j]][:, :n], lhsT=vaug[:, kc, kvh, :], rhs=pt[pk][:, :n],
                                                          start=first, stop=lastk),
                                 r=[("pt", pk)] + vkeys, w=[psk(po[j])])
                            if lastk:
                                P.op("dve", lambda e: e.reciprocal(out=rc[j][0:64, :n], in_=ps[po[j]][64:128, :n]), r=[psk(po[j])], w=[("rc", j)])
                                P.op("dve", lambda e: e.tensor_tensor(out=MX(c, j * 64, j * 64 + 64, s, e_), in0=ps[po[j]][0:64, :n], in1=rc[j][0:64, :n], op=ALU.mult),
                                     r=[psk(po[j]), ("rc", j)], w=mk([c], [tt]))

                        LOOK = 2
                        pend = []
                        for st_ in gsteps:
                            pend.append(st_ + (emit_qk(st_[0], st_[2], st_[3]),))
                            if len(pend) > LOOK:
                                emit_pv(*pend.pop(0))
                        while pend:
                            emit_pv(*pend.pop(0))
                    P.barrier()

                if dbg and stage == 3 and l == 0:
                    (mixA_box[0] is not None and P.dma("sp", dbgb_d[:, 0:4 * T], mixA_box[0][:].rearrange("p a b -> p (a b)"), "dbg", r=mk(range(4), range(5)))); P.dma("sp", dbgb_d[:, 4 * T:8 * T], mixR[:].rearrange("p a b -> p (a b)"), "dbg", r=mk(range(4, 8), range(5)))
                    break

                with contextlib.ExitStack() as so:
                    osq = [sb(so, "osq%d_%d" % (l, i), [128, 512], BF16) for i in range(2)]
                    ors = [sb(so, "ors%d_%d" % (l, i), [128, 512], F32) for i in range(2)]
                    oi = 0
                    for half, gname in ((0, "aog"), (1, "log")):
                        for tt in q_tiles:
                            s, e_ = TT[tt]
                            n = e_ - s
                            b = P.nb()
                            for c4 in range(4):
                                cc_ = half * 4 + c4
                                k = c4 % 2
                                P.op("act", lambda e: e.activation(out=osq[k][:, :n], in_=MX(cc_, c0=s, c1=e_), func=AF.Square), r=mk([cc_], [tt]), w=[("osq", k)])
                                P.op("pe", lambda e: e.matmul(ps[b][:, :n], lhsT=ones_b, rhs=osq[k][:, :n], start=(c4 == 0), stop=(c4 == 3)),
                                     r=[("osq", k), "cmb"], w=[psk(b)])
                            r_ = ors[oi % 2]
                            rk = ("ors", oi % 2)
                            oi += 1
                            P.op("act", lambda e: e.activation(out=r_[:, :n], in_=ps[b][:, :n], func=AF.Ln, scale=1.0 / 512, bias=EPS), r=[psk(b)], w=[rk])
                            P.op("act", lambda e: e.activation(out=r_[:, :n], in_=r_[:, :n], func=AF.Exp, scale=-0.5), r=[rk], w=[rk])
                            for c4 in range(4):
                                cc_ = half * 4 + c4
                                P.op("dve", lambda e: e.scalar_tensor_tensor(out=MX(cc_, c0=s, c1=e_), in0=MX(cc_, c0=s, c1=e_), scalar=pvi(gname, l, c4),
                                                                             in1=r_[:, :n], op0=ALU.mult, op1=ALU.mult), r=mk([cc_], [tt]) + [rk, "pv"], w=mk([cc_], [tt]))
                    if dbg and stage == 4 and l == 0:
                        (mixA_box[0] is not None and P.dma("sp", dbgb_d[:, 0:4 * T], mixA_box[0][:].rearrange("p a b -> p (a b)"), "dbg", r=mk(range(4), range(5)))); P.dma("sp", dbgb_d[:, 4 * T:8 * T], mixR[:].rearrange("p a b -> p (a b)"), "dbg", r=mk(range(4, 8), range(5)))
                    for o in range(8):
                        ko = wp_load([(0, 128, wout_d[l][:, o * 128:(o + 1) * 128])])
                        for tt in q_tiles:
                            s, e_ = TT[tt]
                            n = e_ - s
                            jj = 1 if tt == 0 else 0
                            b = P.nb()
                            for kc in range(8):
                                P.op("pe", lambda e: e.matmul(ps[b][:, :n], lhsT=wp[ko][:, kc, :], rhs=MX(kc, c0=s, c1=e_), start=(kc == 0), stop=(kc == 7)),
                                     r=[("wp", ko)] + mk([kc], [tt]), w=[psk(b)], inc=(kc == 7))
                            P.op("dve", lambda e: e.scalar_tensor_tensor(out=xT[:, o, s:e_], in0=ps[b][:, :n], scalar=modv[:, l, 2 * 8 + o, jj:jj + 1],
                                                                         in1=xT[:, o, s:e_], op0=ALU.mult, op1=ALU.add),
                                 r=[psk(b), ("modv", 2)] + xk([o], [tt]), w=xk([o], [tt]))
                    P.barrier()
            if dbg and stage in (1, 2, 3) and l == 0:
                break
            if dbg and stage == 4 and l == 0:
                P.dma("sp", dbgf_d[:, 0:8 * T], xT[:].rearrange("p a b -> p (a b)"), "dbg", r=xk(range(8), range(5)))
                break

            with contextlib.ExitStack() as se:
                NEP = 6
                ep = [sb(se, "ep%d_%d" % (l, i), [128, 4096], BF16) for i in range(NEP)]
                epc = [0]
                sel = sb(se, "sel%d" % l, [128, 16, 128], BF16)
                P.dma("pool", sel[:], cmb_d[:, 256:256 + 2048].rearrange("p (a m) -> p a m", m=128), "c_sel", w=["sel"])
                combT = sb(se, "combT%d" % l, [128, T], BF16)
                cbt = sb(se, "cbt%d" % l, [128, NCH, 16], F32)

                def ep_load(e_i):
                    ks = []
                    for wi, (wd_, pat) in enumerate(((wg_d, "g"), (wu_d, "u"), (wd_d, "d"))):
                        k = epc[0] % NEP
                        epc[0] += 1
                        if pat == "d":
                            P.dma("pool", ep[k][:].rearrange("p (j n) -> p j n", j=4), wd_[l][e_i].rearrange("(j p) n -> p j n", p=128), "ep%d" % k, w=[("ep", k)])
                        else:
                            P.dma("pool", ep[k][:].rearrange("p (j n) -> p j n", j=8), wd_[l][e_i].rearrange("(j p) n -> p j n", p=128), "ep%d" % k, w=[("ep", k)])
                        ks.append(k)
                    return ks

                m_tiles = lat_tiles if last else all_tiles
                ch0 = 2 if last else 0
                nchs = NCH - ch0
                eks = {0: ep_load(0)}
                with contextlib.ExitStack() as sg:
                    sg2 = contextlib.ExitStack()
                    rl = P.nb()
                    P.reserved.add(rl)
                    rwg = sb(sg2, "rwg%d" % l, [128, 2, 8, 16], F32)
                    cst = sb(sg2, "cst%d" % l, [16, 2], F32)
                    lgt = [sb(sg2, "lgt%d_%d" % (l, i), [16, 512], F32) for i in range(2)]
                    eT = sb(sg2, "eT%d" % l, [16, T], F32)
                    for jj in range(2):
                        for kc in range(8):
                            P.op("dve", lambda e: e.tensor_scalar(out=rwg[:, jj, kc, :], in0=rw[:, kc * 16:(kc + 1) * 16], scalar1=gsv[:, l, 1, kc, jj:jj + 1],
                                                                  scalar2=None, op0=ALU.mult), r=["rw", ("gsv", 1)], w=["rwg"])
                    bc = P.nb()
                    for kc in range(8):
                        P.op("pe", lambda e: e.matmul(ps[bc][0:16, 0:2], lhsT=rw[:, kc * 16:(kc + 1) * 16], rhs=modv[:, l, 3 * 8 + kc, :], start=(kc == 0), stop=(kc == 7)),
                             r=["rw", ("modv", 3)], w=[psk(bc)], inc=(kc == 7))
                    P.op("dve", lambda e: e.tensor_scalar(out=cst[:, :], in0=ps[bc][0:16, 0:2], scalar1=-1.0, scalar2=None, op0=ALU.mult), r=[psk(bc)], w=["cst"])

                    def router_tile(ti, tt, r_, rk):
                        s, e_ = TT[tt]
                        n = e_ - s
                        jj = 1 if tt == 0 else 0
                        b = P.nb()
                        for kc in range(8):
                            P.op("pe", lambda e: e.matmul(ps[b][0:16, :n], lhsT=rwg[:, jj, kc, :], rhs=xT[:, kc, s:e_], start=(kc == 0), stop=(kc == 7)),
                                 r=["rwg"] + xk([kc], [tt]), w=[psk(b)], inc=(kc == 7))
                        lg_ = lgt[ti % 2]
                        def fin():
                            P.op("dve", lambda e: e.tensor_tensor(out=lg_[:, :n], in0=ps[b][0:16, :n], in1=r_[0:16, :n], op=ALU.mult), r=[psk(b), rk], w=[("lgt", ti % 2)])
                            P.op("act", lambda e: e.activation(out=eT[:, s:e_], in_=lg_[:, :n], func=AF.Exp, scale=-1.0, bias=cst[:, jj:jj + 1]),
                                 r=[("lgt", ti % 2), "cst"], w=[("eT", tt)])
                        rdefer.append(fin)

                    rdefer = []

                    def flush_router(tt):
                        while rdefer:
                            rdefer.pop(0)()

                    norm_mod(l, 1, m_tiles, sg2, post_rstd=router_tile, after_tile=flush_router)
                    flush_router(None)
                    for ch in range(ch0, NCH):
                        tt_ = 0 if ch < 2 else 1 + (ch - 2) // 4
                        P.op("pe", lambda e: e.transpose(out=ps[rl][:, ch * 16:(ch + 1) * 16], in_=eT[0:16, ch * 128:(ch + 1) * 128], identity=ident[0:16, 0:16]),
                             r=[("eT", tt_), "cm"], w=[psk(rl)], inc=(ch == NCH - 1))
                    P.barrier()
                    sg2.close()

                    def rt(name):
                        return sb(sg, "%s_%d" % (name, l), [128, NCH * 16], F32)

                    sc = rt("r_sc"); bi = rt("r_bi"); selm = rt("r_sel")
                    m1 = rt("r_m1"); m2 = rt("r_m2"); mn = rt("r_mn"); gsum = rt("r_gs"); gmax = rt("r_gm"); ing = rt("r_in"); den = rt("r_den")
                    lo, hi = ch0 * 16, NCH * 16
                    nq = nchs * 4
                    P.op("dve", lambda e: e.tensor_scalar(out=sc[:, lo:hi], in0=ps[rl][:, lo:hi], scalar1=1.0, scalar2=None, op0=ALU.add), r=[psk(rl)], w=["r_sc"])
                    P.op("dve", lambda e: e.reciprocal(out=sc[:, lo:hi], in_=sc[:, lo:hi]), r=["r_sc"], w=["r_sc"])
                    orb = PV_OFF["rb"][0]
                    P.op("dve", lambda e: e.tensor_tensor(out=bi[:, lo:hi], in0=sc[:, lo:hi], in1=pv[:, orb + lo:orb + hi], op=ALU.add), r=["r_sc", "pv"], w=["r_bi"])
                    g4v = bi[:, lo:hi].rearrange("p (q i) -> p q i", i=4)
                    P.op("dve", lambda e: e.tensor_reduce(out=m1[:, 0:nq], in_=g4v, axis=AX.X, op=ALU.max), r=["r_bi"], w=["r_m1"])
                    first = True
                    for i in range(4):
                        for j in range(i + 1, 4):
                            if first:
                                P.op("dve", lambda e: e.tensor_tensor(out=m2[:, 0:nq], in0=g4v[:, :, i], in1=g4v[:, :, j], op=ALU.min), r=["r_bi"], w=["r_m2"])
                                first = False
                            else:
                                P.op("dve", lambda e: e.tensor_tensor(out=mn[:, 0:nq], in0=g4v[:, :, i], in1=g4v[:, :, j], op=ALU.min), r=["r_bi"], w=["r_mn"])
                                P.op("dve", lambda e: e.tensor_tensor(out=m2[:, 0:nq], in0=m2[:, 0:nq], in1=mn[:, 0:nq], op=ALU.max), r=["r_m2", "r_mn"], w=["r_m2"])
                    P.op("dve", lambda e: e.tensor_tensor(out=gsum[:, 0:nq], in0=m1[:, 0:nq], in1=m2[:, 0:nq], op=ALU.add), r=["r_m1", "r_m2"], w=["r_gs"])
                    gsv3 = gsum[:, 0:nq].rearrange("p (c g) -> p c g", g=4)
                    P.op("dve", lambda e: e.tensor_reduce(out=gmax[:, 0:nchs], in_=gsv3, axis=AX.X, op=ALU.max), r=["r_gs"], w=["r_gm"])
                    ing3 = ing[:, 0:nq].rearrange("p (c g) -> p c g", g=4)
                    for g in range(4):
                        P.op("dve", lambda e: e.tensor_tensor(out=ing3[:, :, g], in0=gsv3[:, :, g], in1=gmax[:, 0:nchs], op=ALU.is_ge), r=["r_gs", "r_gm"], w=["r_in"])
                    sel3 = selm[:, lo:hi].rearrange("p (q i) -> p q i", i=4)
                    for i in range(4):
                        P.op("dve", lambda e: e.tensor_tensor(out=sel3[:, :, i], in0=g4v[:, :, i], in1=m2[:, 0:nq], op=ALU.is_ge), r=["r_bi", "r_m2"], w=["r_sel"])
                        P.op("dve", lambda e: e.tensor_tensor(out=sel3[:, :, i], in0=sel3[:, :, i], in1=ing[:, 0:nq], op=ALU.mult), r=["r_sel", "r_in"], w=["r_sel"])
                    P.op("dve", lambda e: e.tensor_tensor(out=selm[:, lo:hi], in0=selm[:, lo:hi], in1=sc[:, lo:hi], op=ALU.mult), r=["r_sel", "r_sc"], w=["r_sel"])
                    P.op("dve", lambda e: e.tensor_reduce(out=den[:, 0:nchs], in_=selm[:, lo:hi].rearrange("p (c x) -> p c x", x=16), axis=AX.X, op=ALU.add),
                         r=["r_sel"], w=["r_den"])
                    P.op("dve", lambda e: e.reciprocal(out=den[:, 0:nchs], in_=den[:, 0:nchs]), r=["r_den"], w=["r_den"])
                    for ci in range(nchs):
                        ch = ch0 + ci
                        P.op("dve", lambda e: e.tensor_scalar(out=cbt[:, ch, :], in0=selm[:, ch * 16:(ch + 1) * 16], scalar1=den[:, ci:ci + 1], scalar2=None, op0=ALU.mult),
                             r=["r_sel", "r_den"], w=[("cbt", ch)])
                    for g4 in range((nchs + 3) // 4):
                        chs = list(range(ch0 + g4 * 4, min(ch0 + g4 * 4 + 4, NCH)))
                        b = P.nb()
                        for i, ch in enumerate(chs):
                            P.op("pe", lambda e: e.transpose(out=ps[b][0:16, i * 128:(i + 1) * 128], in_=cbt[:, ch, :], identity=ident),
                                 r=[("cbt", ch), "cm"], w=[psk(b)], inc=(i == len(chs) - 1))
                        P.op("act", lambda e: e.copy(out=combT[0:16, chs[0] * 128:(chs[-1] + 1) * 128], in_=ps[b][0:16, 0:len(chs) * 128]), r=[psk(b)], w=["combT"])
                    P.reserved.discard(rl)
                    P.barrier()

                if dbg and stage == 5 and l == 0:
                    P.dma("sp", dbgb_d[:, 0:8 * T], hT[:].rearrange("p a b -> p (a b)"), "dbg", r=hk(range(8), range(5)))
                    P.dma("sp", dbgf_d[:, 0:NCH * 16], cbt[:].rearrange("p a b -> p (a b)"), "dbg", r=[("cbt", ch) for ch in range(NCH)])
                    break

                with contextlib.ExitStack() as sh:
                    cbe = [sb(sh, "cbe%d_%d" % (l, i), [128, T], BF16) for i in range(2)]
                    sgt = [sb(sh, "sgt%d_%d" % (l, i), [128, 512], BF16) for i in range(2)]
                    hid = [sb(sh, "hid%d_%d" % (l, i), [128, 4, 512], BF16) for i in range(2)]
                    sgc = [0]
                    hdc = [0]
                    if l + 1 < NL:
                        adaw2 = [sb(sh, "adawL%d_%d" % (l, i), [128, 8, 256], BF16) for i in range(2)]
                        mods_begin(l + 1)
                        mp = [0]
                        mq = [0]
                    for ei in range(NE):
                        if ei + 1 < NE:
                            eks[ei + 1] = ep_load(ei + 1)
                        if l + 1 < NL:
                            while mq[0] < mp[0]:
                                mods_piece(l + 1, mq[0], adaw2, dma=False)
                                mq[0] += 1
                            for _ in range(2):
                                if mp[0] < NPIECE:
                                    mods_dma(l + 1, mp[0], adaw2)
                                    mp[0] += 1
                        kg_, ku_, kd_ = eks[ei]
                        wgv = ep[kg_][:].rearrange("p (j n) -> p j n", j=8)
                        wuv = ep[ku_][:].rearrange("p (j n) -> p j n", j=8)
                        wdv = ep[kd_][:].rearrange("p (j n) -> p j n", j=4)
                        cb_ = cbe[ei % 2]
                        ck = ("cbe", ei % 2)
                        for tt in m_tiles:
                            s, e_ = TT[tt]
                            n = e_ - s
                            b = P.nb()
                            P.op("pe", lambda e: e.matmul(ps[b][:, :n], lhsT=sel[0:16, ei, :], rhs=combT[0:16, s:e_], start=True, stop=True),
                                 r=["sel", "combT"], w=[psk(b)])
                            P.op("act", lambda e: e.copy(out=cb_[:, s:e_], in_=ps[b][:, :n]), r=[psk(b)], w=[ck])
                        for tt in m_tiles:
                            s, e_ = TT[tt]
                            n = e_ - s
                            jj = 1 if tt == 0 else 0
                            hd = hid[hdc[0] % 2]
                            hkey = ("hid", hdc[0] % 2)
                            hdc[0] += 1
                            for f in range(4):
                                bg = P.nb()
                                for kc in range(8):
                                    P.op("pe", lambda e: e.matmul(ps[bg][:, :n], lhsT=wgv[:, kc, f * 128:(f + 1) * 128], rhs=hT[:, kc, s:e_], start=(kc == 0), stop=(kc == 7)),
                                         r=[("ep", kg_)] + hk([kc], [tt]), w=[psk(bg)], inc=(kc == 7))
                                bu = P.nb()
                                for kc in range(8):
                                    P.op("pe", lambda e: e.matmul(ps[bu][:, :n], lhsT=wuv[:, kc, f * 128:(f + 1) * 128], rhs=hT[:, kc, s:e_], start=(kc == 0), stop=(kc == 7)),
                                         r=[("ep", ku_)] + hk([kc], [tt]), w=[psk(bu)], inc=(kc == 7))
                                sk = sgc[0] % 2
                                sgc[0] += 1
                                P.op("act", lambda e: e.activation(out=sgt[sk][:, :n], in_=ps[bg][:, :n], func=AF.Silu), r=[psk(bg)], w=[("sgt", sk)])
                                P.op("dve", lambda e: e.tensor_tensor(out=sgt[sk][:, :n], in0=sgt[sk][:, :n], in1=cb_[:, s:e_], op=ALU.mult), r=[("sgt", sk), ck], w=[("sgt", sk)])
                                P.op("dve", lambda e: e.tensor_tensor(out=hd[:, f, :n], in0=ps[bu][:, :n], in1=sgt[sk][:, :n], op=ALU.mult), r=[psk(bu), ("sgt", sk)], w=[hkey])
                            for o in range(8):
                                bo = P.nb()
                                for jc in range(4):
                                    P.op("pe", lambda e: e.matmul(ps[bo][:, :n], lhsT=wdv[:, jc, o * 128:(o + 1) * 128], rhs=hd[:, jc, :n], start=(jc == 0), stop=(jc == 3)),
                                         r=[("ep", kd_), hkey], w=[psk(bo)], inc=(jc == 3))
                                P.op("dve", lambda e: e.scalar_tensor_tensor(out=xT[:, o, s:e_], in0=ps[bo][:, :n], scalar=modv[:, l, 5 * 8 + o, jj:jj + 1],
                                                                             in1=xT[:, o, s:e_], op0=ALU.mult, op1=ALU.add),
                                     r=[psk(bo), ("modv", 5)] + xk([o], [tt]), w=xk([o], [tt]))
                    if l + 1 < NL:
                        assert mp[0] == NPIECE
                        while mq[0] < mp[0]:
                            mods_piece(l + 1, mq[0], adaw2, dma=False)
                            mq[0] += 1
                        mods_finish(l + 1)
                    P.barrier()
            if dbg and stage == 5 and l == 0:
                break
            if dbg and stage == 6 and l == 0:
                P.dma("sp", dbgf_d[:, 0:8 * T], xT[:].rearrange("p a b -> p (a b)"), "dbg", r=xk(range(8), range(5)))
                break

        if not dbg or stage >= 99:
            with contextlib.ExitStack() as sf:
                fsq = [sb(sf, "fsq%d" % i, [128, 512], BF16) for i in range(2)]
                frs = [sb(sf, "frs%d" % i, [128, 512], F32) for i in range(2)]
                yT = [sb(sf, "yT%d" % i, [128, 8, 512], F32) for i in range(2)]
                ob = [sb(sf, "ob%d" % i, [128, D], F32) for i in range(2)]
                obc = 0
                ofg = PV_OFF["fg"][0]
                obc_box = [0]

                def finA(ti, tt):
                    s, e_ = TT[tt]
                    n = e_ - s
                    b = P.nb()
                    for c in range(8):
                        k = c % 2
                        P.op("act", lambda e: e.activation(out=fsq[k][:, :n], in_=xT[:, c, s:e_], func=AF.Square), r=xk([c], [tt]), w=[("fsq", k)])
                        P.op("pe", lambda e: e.matmul(ps[b][:, :n], lhsT=ones_b, rhs=fsq[k][:, :n], start=(c == 0), stop=(c == 7)),
                             r=[("fsq", k), "cmb"], w=[psk(b)])
                    r_ = frs[ti % 2]
                    rk = ("frs", ti % 2)
                    P.op("act", lambda e: e.activation(out=r_[:, :n], in_=ps[b][:, :n], func=AF.Ln, scale=1.0 / D, bias=EPS), r=[psk(b)], w=[rk])
                    P.op("act", lambda e: e.activation(out=r_[:, :n], in_=r_[:, :n], func=AF.Exp, scale=-0.5), r=[rk], w=[rk])

                def finB(ti, tt):
                    s, e_ = TT[tt]
                    n = e_ - s
                    r_ = frs[ti % 2]
                    rk = ("frs", ti % 2)
                    y_ = yT[ti % 2]
                    for c in range(8):
                        P.op("dve", lambda e: e.scalar_tensor_tensor(out=y_[:, c, :n], in0=xT[:, c, s:e_], scalar=pv[:, ofg + c:ofg + c + 1], in1=r_[:, :n],
                                                                     op0=ALU.mult, op1=ALU.mult), r=xk([c], [tt]) + [rk, "pv"], w=[("yT", ti % 2, c)])
                    for i in range(4):
                        k = obc_box[0] % 2
                        obc_box[0] += 1
                        for half in range(2):
                            b2 = P.nb()
                            for q in range(4):
                                c = half * 4 + q
                                P.op("pe", lambda e: e.transpose(out=ps[b2][:, q * 128:(q + 1) * 128], in_=y_[:, c, i * 128:(i + 1) * 128], identity=ident),
                                     r=[("yT", ti % 2, c), "cm"], w=[psk(b2)], inc=(q == 3))
                            if half == 0:
                                P.op("act", lambda e: e.copy(out=ob[k][:, 0:512], in_=ps[b2][:, :]), r=[psk(b2)], w=[("ob", k, 0)])
                            else:
                                P.op("dve", lambda e: e.tensor_copy(out=ob[k][:, 512:1024], in_=ps[b2][:, :]), r=[psk(b2)], w=[("ob", k, 1)])
                        tok = (s - C) + i * 128
                        P.dma("sp", out_d[tok:tok + 128, :], ob[k][:], "o%d" % k, r=[("ob", k, 0), ("ob", k, 1)])

                ftiles = [1, 2, 3, 4]
                for i in range(len(ftiles) + 1):
                    if i < len(ftiles):
                        finA(i, ftiles[i])
                    if i >= 1:
                        finB(i - 1, ftiles[i - 1])
                P.barrier()
        for k, v in P.cnt.items():
            if k not in P.E and v > 0:
                P.E["sp"].wait_ge(P.sem[k], v)
                P._pw["sp"].append((k, v))
        P.check_deadlock()
    return nc


def _chunked(v):
    v = np.asarray(v, np.float32)
    k = v.shape[-1] // 128
    v = v.reshape(v.shape[:-1] + (k, 128))
    return np.moveaxis(v, -1, 0)


def _rope_tables():
    t = np.arange(S)
    r = (t // 64).astype(np.float32)
    col = (t % 64).astype(np.float32)
    inv = (np.float32(10000.0) ** (-np.arange(0, 32, 2, dtype=np.float32) / np.float32(32))).astype(np.float32)
    cosT = np.zeros((128, S), np.float32)
    sinT = np.zeros((128, S), np.float32)
    for p in range(128):
        d = p % 64
        axis, half, f = d // 32, (d % 32) // 16, d % 16
        pos = r if axis == 0 else col
        ang = (pos * inv[f]).astype(np.float32)
        cosT[p] = np.cos(ang)
        sinT[p] = np.sin(ang) * (-1.0 if half == 0 else 1.0)
    return np.stack([cosT, sinT]).astype(np.float32)


def _consts():
    ident = np.eye(128, dtype=np.float32)
    perm = np.zeros((128, 128), np.float32)
    for m in range(128):
        half = (m % 32) // 16
        k = m + 16 if half == 0 else m - 16
        perm[k, m] = 1.0
    cmat = np.concatenate([ident, perm], axis=1)
    ones = np.ones((128, 128), np.float32)
    bones = np.zeros((128, 128), np.float32)
    bones[0:64, 0:64] = 1.0
    bones[64:128, 64:128] = 1.0
    sel = np.zeros((128, 16, 128), np.float32)
    for e in range(16):
        sel[e, e, :] = 1.0
    cmatb = np.concatenate([ones, bones, sel.reshape(128, 2048)], axis=1)
    return cmat, cmatb


def _pvec(inp, b):
    pvv = np.zeros((128, NPV), np.float32)

    def put(name, arr):
        o, s = PV_OFF[name]
        n = int(np.prod(s))
        pvv[:, o:o + n] = np.asarray(arr, np.float32).reshape(128, n)

    cc = np.stack([inp["c"][b], inp["c_ctx"]], axis=0)
    put("cc", np.moveaxis(_chunked(cc), 1, 2))
    put("adab", _chunked(inp["ada_b"]))
    put("n1g", _chunked(inp["norm1_g"]))
    put("n2g", _chunked(inp["norm2_g"]))
    put("fg", _chunked(inp["final_g"]))
    put("qg", np.tile(inp["q_norm_g"].T, (2, 1)))
    put("kg", np.tile(inp["k_norm_g"].T, (2, 1)))
    put("cw", _chunked(inp["conv_w"]))
    put("cb", _chunked(inp["conv_b"]))
    put("lba", _chunked(inp["lru_ba"]))
    put("lbx", _chunked(inp["lru_bx"]))
    put("lam", _chunked(inp["lru_lambda"]))
    put("aog", _chunked(inp["attn_out_g"]))
    put("log", _chunked(inp["lru_out_g"]))
    put("rb", np.broadcast_to(inp["router_b"].reshape(1, 1, 16), (128, NCH, 16)))
    return pvv


def _lru_blockdiag(inp):
    out = np.zeros((NL, 128, 2, 2, 4, 128), np.float32)
    for gi, name in enumerate(("lru_wa", "lru_wx")):
        w = np.asarray(inp[name], np.float32)
        for c in range(4):
            out[:, 0:64, :, gi, c, 0:64] = np.transpose(w[:, :, 2 * c], (0, 2, 1, 3))
            out[:, 64:128, :, gi, c, 64:128] = np.transpose(w[:, :, 2 * c + 1], (0, 2, 1, 3))
    return out.reshape(NL, 128, 2048)


_CACHE = {}


def _get_nc():
    if "nc" not in _CACHE:
        _CACHE["nc"] = build()
    return _CACHE["nc"]


def make_in_maps(inp):
    inp = {k: np.asarray(v) for k, v in inp.items()}
    cmat, cmatb = _consts()
    rope = _rope_tables()
    lrubd = _lru_blockdiag(inp)
    rwl = np.ascontiguousarray(_chunked(inp["router_w"].T).transpose(0, 2, 1)).reshape(128, 128)
    shared = dict(cmat=cmat, cmatb=cmatb, rope=rope, ada_w=np.ascontiguousarray(inp["ada_w"], np.float32),
                  w_in=np.ascontiguousarray(inp["w_in"], np.float32), lrubd=lrubd,
                  w_out=np.ascontiguousarray(inp["w_out"], np.float32), rw=rwl,
                  wg=np.ascontiguousarray(inp["exp_w_gate"], np.float32), wu=np.ascontiguousarray(inp["exp_w_up"], np.float32),
                  wd=np.ascontiguousarray(inp["exp_w_down"], np.float32))
    maps = []
    for b in range(8):
        m = dict(shared)
        m["x"] = np.ascontiguousarray(inp["x"][b], np.float32)
        m["ctx"] = np.ascontiguousarray(inp["ctx"][b], np.float32)
        m["pvec"] = _pvec(inp, b)
        maps.append(m)
    return maps


def kernel(**inputs):
    nc = _get_nc()
    maps = make_in_maps(inputs)
    res = run_bass_kernel_spmd(nc, maps, core_ids=list(range(8)))
    return np.stack([np.asarray(r["out"], np.float32) for r in res.results], axis=0)
```

```python
import contextlib
import os
import numpy as np
import concourse.bass as bass
import concourse.mybir as mybir
from concourse.bass_utils import run_bass_kernel_spmd

F32 = mybir.dt.float32
BF16 = mybir.dt.bfloat16
AF = mybir.ActivationFunctionType
ALU = mybir.AluOpType
AX = mybir.AxisListType

D = 1024
S = 2048
C = 256
T = S + C
NL = 2
NE = 16
DE = 512
INW = 1792
TT = [(0, 256), (256, 768), (768, 1280), (1280, 1792), (1792, 2304)]
NCH = T // 128
EPS = 1e-6

PV_ITEMS = [
    ("cc", (8, 2)), ("adab", (NL, 48)), ("n1g", (NL, 8)), ("n2g", (NL, 8)), ("fg", (8,)),
    ("qg", (NL,)), ("kg", (NL,)), ("cw", (NL, 4, 4)), ("cb", (NL, 4)),
    ("lba", (NL, 2, 4)), ("lbx", (NL, 2, 4)), ("lam", (NL, 2, 4)),
    ("aog", (NL, 4)), ("log", (NL, 4)), ("rb", (NCH, 16)),
]
PV_OFF = {}
_o = 0
for _n, _s in PV_ITEMS:
    PV_OFF[_n] = (_o, _s)
    _o += int(np.prod(_s))
NPV = _o


class Prog:
    def __init__(self, nc, es):
        self.nc = nc
        self.es = es
        self.E = dict(pe=nc.tensor, act=nc.scalar, dve=nc.vector, pool=nc.gpsimd, sp=nc.sync)
        self.sem = {}
        self.cnt = {}
        for k in self.E:
            self.sem[k] = es.enter_context(nc.semaphore("sem_" + k))
            self.cnt[k] = 0
        self.seen = {k: {} for k in self.E}
        self.res = {}
        self.q = {k: [] for k in self.E}
        self._pw = {k: [] for k in self.E}
        self._bank = 0
        self.reserved = set()

    def _deps(self, eng, r, w, skip=None):
        need = {}
        for key in r:
            st = self.res.get(key)
            if st and st[0] is not None:
                k, v = st[0]
                if need.get(k, 0) < v:
                    need[k] = v
        for key in w:
            st = self.res.get(key)
            if st:
                if st[0] is not None:
                    k, v = st[0]
                    if need.get(k, 0) < v:
                        need[k] = v
                for k, v in st[1].items():
                    if need.get(k, 0) < v:
                        need[k] = v
        for k, v in need.items():
            if k == skip:
                continue
            if k == eng:
                if eng == "pe" or v > self.cnt[eng]:
                    continue
            if self.seen[eng].get(k, 0) < v:
                self.E[eng].wait_ge(self.sem[k], v)
                self.seen[eng][k] = v
                self._pw[eng].append((k, v))

    def _mark(self, tag, r, w):
        k, v = tag
        for key in r:
            st = self.res.setdefault(key, [None, {}])
            if st[1].get(k, 0) < v:
                st[1][k] = v
        for key in w:
            self.res[key] = [tag, {}]

    def op(self, eng, fn, r=(), w=(), inc=True):
        pr = [k for k in r if isinstance(k, tuple) and k[0] == "ps"]
        if pr:
            w = list(w) + pr
        self._deps(eng, r, w)
        inst = fn(self.E[eng])
        self.q[eng].append((self._pw[eng], (eng, 1) if inc else None))
        self._pw[eng] = []
        if inc:
            self.cnt[eng] += 1
            inst.then_inc(self.sem[eng], 1)
            tag = (eng, self.cnt[eng])
        else:
            tag = (eng, self.cnt[eng] + 1)
        self._mark(tag, r, w)
        return inst

    def dma(self, q, out, in_, semkey, r=(), w=(), **kw):
        if semkey not in self.sem:
            self.sem[semkey] = self.es.enter_context(self.nc.semaphore("d_" + semkey))
            self.cnt[semkey] = 0
        self._deps(q, r, w, skip=semkey)
        inst = self.E[q].dma_start(out=out, in_=in_, **kw)
        self.q[q].append((self._pw[q], (semkey, 16)))
        self._pw[q] = []
        self.cnt[semkey] += 16
        inst.then_inc(self.sem[semkey], 16)
        self._mark((semkey, self.cnt[semkey]), r, w)
        return inst

    def barrier(self):
        for e in self.E:
            for k, v in self.cnt.items():
                if k != e and v > 0 and self.seen[e].get(k, 0) < v:
                    self.E[e].wait_ge(self.sem[k], v)
                    self.seen[e][k] = v
                    self._pw[e].append((k, v))

    def check_deadlock(self):
        val = {k: 0 for k in self.cnt}
        pos = {k: 0 for k in self.E}
        for e in self.E:
            if self._pw[e]:
                self.q[e].append((self._pw[e], None))
                self._pw[e] = []
        progress = True
        while progress:
            progress = False
            for e in self.E:
                ql = self.q[e]
                while pos[e] < len(ql):
                    waits, inc = ql[pos[e]]
                    if all(val[k] >= v for k, v in waits):
                        if inc is not None:
                            val[inc[0]] += inc[1]
                        pos[e] += 1
                        progress = True
                    else:
                        break
        stuck = {e: (pos[e], len(self.q[e]), self.q[e][pos[e]][0]) for e in self.E if pos[e] < len(self.q[e])}
        if stuck:
            raise RuntimeError("DEADLOCK in semaphore plan: %r ; vals=%r" % (stuck, {k: val[k] for e in stuck for k, _ in stuck[e][2]}))

    def nb(self):
        while True:
            b = self._bank
            self._bank = (self._bank + 1) % 8
            if b not in self.reserved:
                return b


def build(stage=99, dbg=False):
    nc = bass.Bass("TRN2", target_bir_lowering=False)

    def dten(name, shape, dty=F32, kind="ExternalInput"):
        return nc.dram_tensor(name, shape, dty, kind=kind).ap()

    x_d = dten("x", [S, D])
    ctx_d = dten("ctx", [C, D])
    pv_d = dten("pvec", [128, NPV])
    cm_d = dten("cmat", [128, 256])
    cmb_d = dten("cmatb", [128, 256 + 2048])
    rope_d = dten("rope", [2, 128, S])
    adaw_d = dten("ada_w", [NL, D, 6 * D])
    win_d = dten("w_in", [NL, D, INW])
    lru_d = dten("lrubd", [NL, 128, 2048])
    wout_d = dten("w_out", [NL, D, D])
    rw_d = dten("rw", [128, 128])
    wg_d = dten("wg", [NL, NE, D, DE])
    wu_d = dten("wu", [NL, NE, D, DE])
    wd_d = dten("wd", [NL, NE, DE, D])
    out_d = dten("out", [S, D], kind="ExternalOutput")
    if dbg:
        dbgf_d = dten("dbgf", [128, 8 * T], F32, kind="ExternalOutput")
        dbgb_d = dten("dbgb", [128, 8 * T], BF16, kind="ExternalOutput")

    with contextlib.ExitStack() as es:
        P = Prog(nc, es)

        def sb(stack, name, shape, dty):
            return stack.enter_context(nc.sbuf_tensor("s_" + name, shape, dty))

        ps = [es.enter_context(nc.psum_tensor("ps%d" % i, [128, 512], F32)) for i in range(8)]

        def psk(b):
            return ("ps", b)

        xT = sb(es, "xT", [128, 8, T], F32)
        hT = sb(es, "hT", [128, 8, T], BF16)
        pv = sb(es, "pv", [128, NPV], F32)
        cm = sb(es, "cm", [128, 256], F32)
        cmb = sb(es, "cmb", [128, 256], BF16)
        scb = sb(es, "scb", [128, 8, 2], BF16)
        modv = sb(es, "modv", [128, NL, 48, 2], F32)
        gsv = sb(es, "gsv", [128, NL, 2, 8, 2], F32)
        hsp = sb(es, "hsp", [128, NL * 2 * 4], F32)
        hba = sb(es, "hba", [128, NL * 2 * 4], F32)
        hbx = sb(es, "hbx", [128, NL * 2 * 4], F32)
        rw = sb(es, "rw", [128, 128], F32)
        smalltmp = sb(es, "smalltmp", [128, 64], F32)

        ident = cm[:, 0:128]
        perm = cm[:, 128:256]
        ones_b = cmb[:, 0:128]
        bones_b = cmb[:, 128:256]

        def pvv(name):
            o, s = PV_OFF[name]
            n = int(np.prod(s))
            return pv[:, o:o + n]

        def pvi(name, *idx):
            o, s = PV_OFF[name]
            flat = 0
            for i, d in zip(idx, s):
                flat = flat * d + i
            return pv[:, o + flat:o + flat + 1]

        P.dma("sp", pv[:], pv_d, "c_pv", w=["pv"])
        P.dma("sp", cm[:], cm_d, "c_cm", w=["cm"])
        P.dma("sp", rw[:], rw_d, "c_rw", w=["rw"])
        P.dma("pool", cmb[:], cmb_d[:, 0:256], "c_cmb", w=["cmb"])

        o_cc = PV_OFF["cc"][0]
        P.op("act", lambda e: e.activation(out=scb[:].rearrange("p a b -> p (a b)"), in_=pv[:, o_cc:o_cc + 16],
                                           func=AF.Silu), r=["pv"], w=["scb"])
        P.op("act", lambda e: e.activation(out=smalltmp[:, 0:16], in_=pvv("lam"), func=AF.Exp, scale=-1.0),
             r=["pv"], w=["smalltmp"])
        P.op("act", lambda e: e.activation(out=smalltmp[:, 0:16], in_=smalltmp[:, 0:16], func=AF.Ln, bias=1.0),
             r=["smalltmp"], w=["smalltmp"])
        P.op("dve", lambda e: e.tensor_scalar(out=hsp[:], in0=smalltmp[:, 0:16], scalar1=-4.0, scalar2=None,
                                              op0=ALU.mult), r=["smalltmp"], w=["hsp"])
        P.op("dve", lambda e: e.tensor_scalar(out=hba[:], in0=pvv("lba"), scalar1=0.5, scalar2=None,
                                              op0=ALU.mult), r=["pv"], w=["hba"])
        P.op("dve", lambda e: e.tensor_scalar(out=hbx[:], in0=pvv("lbx"), scalar1=0.5, scalar2=None,
                                              op0=ALU.mult), r=["pv"], w=["hbx"])

        with contextlib.ExitStack() as s0:
            adaw = [sb(s0, "adaw%d" % i, [128, 8, 256], BF16) for i in range(2)]
            xin = [sb(s0, "xin%d" % i, [128, D], F32) for i in range(2)]
            def load_x(tc):
                k = tc % 2
                src = ctx_d[tc * 128:(tc + 1) * 128, :] if tc < 2 else x_d[(tc - 2) * 128:(tc - 1) * 128, :]
                P.dma("sp", xin[k][:], src, "xin%d" % k, w=[("xin", k)])
                tt = 0 if tc < 2 else 1 + (tc - 2) // 4
                for half in range(2):
                    b = P.nb()
                    for q in range(4):
                        c = half * 4 + q
                        P.op("pe", lambda e: e.transpose(out=ps[b][:, q * 128:(q + 1) * 128],
                                                         in_=xin[k][:, c * 128:(c + 1) * 128], identity=ident),
                             r=[("xin", k), "cm"], w=[psk(b)], inc=(q == 3))
                    eng = "act" if half == 0 else "dve"
                    if eng == "act":
                        P.op("act", lambda e: e.copy(out=xT[:, half * 4:half * 4 + 4, tc * 128:(tc + 1) * 128],
                                                     in_=ps[b][:].rearrange("p (q t) -> p q t", q=4)),
                             r=[psk(b)], w=[("xT", c_, tt) for c_ in range(half * 4, half * 4 + 4)])
                    else:
                        P.op("dve", lambda e: e.tensor_copy(out=xT[:, half * 4:half * 4 + 4, tc * 128:(tc + 1) * 128],
                                                            in_=ps[b][:].rearrange("p (q t) -> p q t", q=4)),
                             r=[psk(b)], w=[("xT", c_, tt) for c_ in range(half * 4, half * 4 + 4)])

            xi = 0
            NPIECE = 24
            mod_state = {}

            def mods_begin(l):
                pm = P.nb()
                P.reserved.add(pm)
                mod_state[l] = pm

            def mods_dma(l, j, bufs):
                k = j % 2
                P.dma("pool", bufs[k][:], adaw_d[l][:, j * 256:(j + 1) * 256].rearrange("(kc p) n -> p kc n", p=128),
                      "adaw%d" % k, w=[("adaw", k)])

            def mods_piece(l, j, bufs, dma=True):
                pm = mod_state[l]
                k = j % 2
                if dma:
                    mods_dma(l, j, bufs)
                for fc in range(2):
                    oc = j * 2 + fc
                    for kc in range(8):
                        P.op("pe", lambda e: e.matmul(ps[pm][:, oc * 2:oc * 2 + 2], lhsT=bufs[k][:, kc, fc * 128:(fc + 1) * 128],
                                                      rhs=scb[:, kc, :], start=(kc == 0), stop=(kc == 7)),
                             r=[("adaw", k), "scb"], w=[psk(pm)], inc=(kc == 7))

            def mods_finish(l, m0=0, m1=6, release=True):
                pm = mod_state[l]
                o_ab = PV_OFF["adab"][0] + l * 48
                for jj in range(2):
                    P.op("dve", lambda e: e.tensor_tensor(out=modv[:, l, m0 * 8:m1 * 8, jj],
                                                          in0=ps[pm][:, 0:96].rearrange("p (a b) -> p a b", b=2)[:, m0 * 8:m1 * 8, jj],
                                                          in1=pv[:, o_ab + m0 * 8:o_ab + m1 * 8], op=ALU.add),
                         r=[psk(pm), "pv"], w=[("modv", m_) for m_ in range(m0, m1)])
                if release:
                    P.reserved.discard(pm)
                for n_, (gname, mi) in enumerate((("n1g", 1), ("n2g", 4))):
                    if not (m0 <= mi < m1):
                        continue
                    og = PV_OFF[gname][0] + l * 8
                    for jj in range(2):
                        P.op("dve", lambda e: e.scalar_tensor_tensor(out=gsv[:, l, n_, :, jj], in0=modv[:, l, mi * 8:(mi + 1) * 8, jj],
                                                                     scalar=1.0, in1=pv[:, og:og + 8], op0=ALU.add, op1=ALU.mult),
                             r=[("modv", mi), "pv"], w=[("gsv", n_)])

            mods_begin(0)
            for j in range(8):
                mods_piece(0, j, adaw)
                for _ in range(2):
                    if xi < NCH:
                        load_x(xi); xi += 1
            mods_finish(0, 0, 2, release=False)
            while xi < NCH:
                load_x(xi); xi += 1
            P.barrier()

        if dbg and stage == 0:
            P.dma("sp", dbgf_d[:, 0:8 * T], xT[:].rearrange("p a b -> p (a b)"), "dbg", r=[("xT", c_, t_) for c_ in range(8) for t_ in range(5)])
            P.dma("sp", out_d[0:128, 0:192], modv[:].rearrange("p a b c -> p (a b c)"), "dbg", r=[("modv", m_) for m_ in range(6)])
            P.dma("sp", out_d[128:256, 0:64], gsv[:].rearrange("p a b c d -> p (a b c d)"), "dbg", r=[("gsv", 0), ("gsv", 1)])

        def xk(cs, tts):
            return [("xT", c_, t_) for c_ in cs for t_ in tts]

        def hk(cs, tts):
            return [("hT", c_, t_) for c_ in cs for t_ in tts]

        def norm_mod(l, nidx, tiles, stk, h2f=None, after_tile=None, post_rstd=None):
            shi = 0 if nidx == 0 else 3
            sq = [sb(stk, "nsq%d_%d_%d" % (l, nidx, i), [128, 512], BF16) for i in range(2)]
            rs = [sb(stk, "nrs%d_%d_%d" % (l, nidx, i), [128, 512], F32) for i in range(2)]
            tmp = [sb(stk, "ntmp%d_%d_%d" % (l, nidx, i), [128, 512], F32) for i in range(2)]
            def stageA(ti, tt):
                s, e_ = TT[tt]
                n = e_ - s
                b = P.nb()
                rk = ("nrs", ti % 2)
                for c in range(8):
                    k = c % 2
                    P.op("act", lambda e: e.activation(out=sq[k][:, :n], in_=xT[:, c, s:e_], func=AF.Square),
                         r=xk([c], [tt]), w=[("nsq", k)])
                    P.op("pe", lambda e: e.matmul(ps[b][:, :n], lhsT=ones_b, rhs=sq[k][:, :n], start=(c == 0), stop=(c == 7)),
                         r=[("nsq", k), "cmb"], w=[psk(b)])
                r_ = rs[ti % 2]
                P.op("act", lambda e: e.activation(out=r_[:, :n], in_=ps[b][:, :n], func=AF.Ln, scale=1.0 / D, bias=EPS),
                     r=[psk(b)], w=[rk])
                P.op("act", lambda e: e.activation(out=r_[:, :n], in_=r_[:, :n], func=AF.Exp, scale=-0.5), r=[rk], w=[rk])
                if post_rstd is not None:
                    post_rstd(ti, tt, r_, rk)

            def stageB(ti, tt):
                s, e_ = TT[tt]
                n = e_ - s
                jj = 1 if tt == 0 else 0
                rk = ("nrs", ti % 2)
                r_ = rs[ti % 2]
                for c in range(8):
                    k = c % 2
                    P.op("dve", lambda e: e.tensor_tensor(out=tmp[k][:, :n], in0=xT[:, c, s:e_], in1=r_[:, :n], op=ALU.mult),
                         r=xk([c], [tt]) + [rk], w=[("ntmp", k)])
                    if h2f is None:
                        P.op("act", lambda e: e.activation(out=hT[:, c, s:e_], in_=tmp[k][:, :n], func=AF.Identity,
                                                           scale=gsv[:, l, nidx, c, jj:jj + 1], bias=modv[:, l, shi * 8 + c, jj:jj + 1]),
                             r=[("ntmp", k), ("gsv", nidx), ("modv", shi)], w=hk([c], [tt]))
                    else:
                        P.op("act", lambda e: e.activation(out=h2f[:, c, :n], in_=tmp[k][:, :n], func=AF.Identity,
                                                           scale=gsv[:, l, nidx, c, jj:jj + 1], bias=modv[:, l, shi * 8 + c, jj:jj + 1]),
                             r=[("ntmp", k), ("gsv", nidx), ("modv", shi)], w=[("h2f", c)])
                        P.op("dve", lambda e: e.tensor_copy(out=hT[:, c, s:e_], in_=h2f[:, c, :n]),
                             r=[("h2f", c)], w=hk([c], [tt]))
                if after_tile is not None:
                    after_tile(tt)

            nt = len(tiles)
            for i in range(nt + 1):
                if i < nt:
                    stageA(i, tiles[i])
                if i - 1 >= 0:
                    stageB(i - 1, tiles[i - 1])

        def dump_and_finish():
            pass

        for l in range(NL):
            if dbg and stage == 0:
                break
            last = (l == NL - 1)
            all_tiles = [0, 1, 2, 3, 4]
            lat_tiles = [1, 2, 3, 4]
            q_tiles = lat_tiles if last else all_tiles
            with contextlib.ExitStack() as sm:
                with contextlib.ExitStack() as sn:
                    norm_mod(l, 0, all_tiles, sn)
                    P.barrier()
                if dbg and stage == 1 and l == 0:
                    P.dma("sp", dbgb_d[:, 0:8 * T], hT[:].rearrange("p a b -> p (a b)"), "dbg", r=hk(range(8), range(5)))
                    break
                NWP = 3
                wp = [sb(sm, "wp%d_%d" % (l, i), [128, 8, 128], BF16) for i in range(NWP)]
                wpc = [0]

                def wp_load(src_list):
                    k = wpc[0] % NWP
                    wpc[0] += 1
                    for (c0, c1, src) in src_list:
                        P.dma("pool", wp[k][:, :, c0:c1], src.rearrange("(kc p) n -> p kc n", p=128), "wp%d" % k, w=[("wp", k)])
                    return k

                def proj(k, tt, b):
                    s, e_ = TT[tt]
                    n = e_ - s
                    for kc in range(8):
                        P.op("pe", lambda e: e.matmul(ps[b][:, :n], lhsT=wp[k][:, kc, :], rhs=hT[:, kc, s:e_], start=(kc == 0), stop=(kc == 7)),
                             r=[("wp", k)] + hk([kc], [tt]), w=[psk(b)], inc=(kc == 7))

                mixR = sb(sm, "mixR%d" % l, [128, 4, T], BF16)
                mixA_box = [None]

                def MX(c_, p0=0, p1=128, c0=None, c1=None):
                    t_ = mixA_box[0] if c_ < 4 else mixR
                    return t_[p0:p1, c_ % 4, c0:c1]

                def mk(cs, tts):
                    return [("mixT", c_, t_) for c_ in cs for t_ in tts]

                with contextlib.ExitStack() as sl:
                    lw = sb(sl, "lw%d" % l, [128, 16, 128], BF16)
                    P.dma("pool", lw[:], lru_d[l].rearrange("p (a m) -> p a m", m=128), "c_lw", w=["lw"])
                    uh = sb(sl, "uh%d" % l, [128, T], F32)
                    cu = sb(sl, "cu%d" % l, [128, T], F32)
                    cub = sb(sl, "cub%d" % l, [128, T], BF16)
                    TA = sb(sl, "TA%d" % l, [128, T], F32)
                    TX = sb(sl, "TX%d" % l, [128, T], F32)
                    NM = sb(sl, "NM%d" % l, [128, T], F32)
                    gl = [sb(sl, "gl%d_%d" % (l, i), [128, 512], F32) for i in range(2)]
                    if l == 0:
                        adawL = [sb(sl, "adawS%d" % i, [128, 8, 256], BF16) for i in range(2)]
                        lp = [8]
                        lq = [8]
                        for _ in range(2):
                            mods_dma(0, lp[0], adawL)
                            lp[0] += 1
                    for c in range(4):
                        if l == 0:
                            for _ in range(4):
                                if lq[0] < NPIECE:
                                    mods_piece(0, lq[0], adawL, dma=False)
                                    lq[0] += 1
                                    if lp[0] < NPIECE:
                                        mods_dma(0, lp[0], adawL)
                                        lp[0] += 1
                        ku = wp_load([(0, 128, win_d[l][:, 768 + c * 128:768 + (c + 1) * 128])])
                        kg_ = wp_load([(0, 128, win_d[l][:, 1280 + c * 128:1280 + (c + 1) * 128])])
                        for tt in all_tiles:
                            s, e_ = TT[tt]
                            b = P.nb()
                            proj(ku, tt, b)
                            P.op("act", lambda e: e.copy(out=NM[:, s:e_], in_=ps[b][:, :e_ - s]), r=[psk(b)], w=["NM"])
                        P.op("dve", lambda e: e.tensor_scalar(out=cu[:, 0:T], in0=NM[:, 0:T], scalar1=pvi("cw", l, 2, c), scalar2=pvi("cb", l, c),
                                                              op0=ALU.mult, op1=ALU.add), r=["NM", "pv"], w=["cu"])
                        for (ga, gz) in ((0, C), (C, T)):
                            P.op("dve", lambda e: e.scalar_tensor_tensor(out=cu[:, ga + 2:gz], in0=NM[:, ga:gz - 2], scalar=pvi("cw", l, 0, c),
                                                                         in1=cu[:, ga + 2:gz], op0=ALU.mult, op1=ALU.add), r=["NM", "cu", "pv"], w=["cu"])
                            P.op("dve", lambda e: e.scalar_tensor_tensor(out=cu[:, ga + 1:gz], in0=NM[:, ga:gz - 1], scalar=pvi("cw", l, 1, c),
                                                                         in1=cu[:, ga + 1:gz], op0=ALU.mult, op1=ALU.add), r=["NM", "cu", "pv"], w=["cu"])
                            P.op("dve", lambda e: e.scalar_tensor_tensor(out=cu[:, ga:gz - 1], in0=NM[:, ga + 1:gz], scalar=pvi("cw", l, 3, c),
                                                                         in1=cu[:, ga:gz - 1], op0=ALU.mult, op1=ALU.add), r=["NM", "cu", "pv"], w=["cu"])
                        P.op("act", lambda e: e.copy(out=cub[:, :], in_=cu[:, :]), r=["cu"], w=["cub"])
                        for d in range(2):
                            li = (l * 2 + d) * 4 + c
                            for tt in all_tiles:
                                s, e_ = TT[tt]
                                n = e_ - s
                                ba_ = P.nb()
                                P.op("pe", lambda e: e.matmul(ps[ba_][:, :n], lhsT=lw[:, (d * 2 + 0) * 4 + c, :], rhs=cub[:, s:e_], start=True, stop=True),
                                     r=["lw", "cub"], w=[psk(ba_)])
                                P.op("act", lambda e: e.activation(out=TA[:, s:e_], in_=ps[ba_][:, :n], func=AF.Tanh, scale=0.5, bias=hba[:, li:li + 1]),
                                     r=[psk(ba_), "hba"], w=["TA"])
                            P.op("act", lambda e: e.activation(out=TA[:, :], in_=TA[:, :], func=AF.Exp, scale=hsp[:, li:li + 1], bias=hsp[:, li:li + 1]),
                                 r=["TA", "hsp"], w=["TA"])
                            P.op("dve", lambda e: e.scalar_tensor_tensor(out=NM[:, :], in0=TA[:, :], scalar=-1.0, in1=TA[:, :], op0=ALU.mult, op1=ALU.mult),
                                 r=["TA"], w=["NM"])
                            for tt in all_tiles:
                                s, e_ = TT[tt]
                                n = e_ - s
                                bx_ = P.nb()
                                P.op("pe", lambda e: e.matmul(ps[bx_][:, :n], lhsT=lw[:, (d * 2 + 1) * 4 + c, :], rhs=cub[:, s:e_], start=True, stop=True),
                                     r=["lw", "cub"], w=[psk(bx_)])
                                P.op("act", lambda e: e.activation(out=TX[:, s:e_], in_=ps[bx_][:, :n], func=AF.Tanh, scale=0.5, bias=hbx[:, li:li + 1]),
                                     r=[psk(bx_), "hbx"], w=["TX"])
                            P.op("act", lambda e: e.activation(out=NM[:, :], in_=NM[:, :], func=AF.Sqrt, scale=0.25, bias=0.25), r=["NM"], w=["NM"])
                            P.op("dve", lambda e: e.scalar_tensor_tensor(out=TX[:, :], in0=TX[:, :], scalar=1.0, in1=cu[:, :], op0=ALU.add, op1=ALU.mult),
                                 r=["TX", "cu"], w=["TX"])
                            P.op("dve", lambda e: e.tensor_tensor(out=TX[:, :], in0=TX[:, :], in1=NM[:, :], op=ALU.mult), r=["TX", "NM"], w=["TX"])
                            if d == 0:
                                P.op("dve", lambda e: e.tensor_tensor_scan(out=uh[:, 0:T], data0=TA[:, 0:T], data1=TX[:, 0:T], initial=0.0,
                                                                           op0=ALU.mult, op1=ALU.add), r=["TA", "TX", "cu"], w=["uh"])
                            else:
                                P.op("dve", lambda e: e.tensor_tensor_scan(out=NM[:, 0:C][:, ::-1], data0=TA[:, 0:C][:, ::-1], data1=TX[:, 0:C][:, ::-1],
                                                                           initial=0.0, op0=ALU.mult, op1=ALU.add), r=["TA", "TX"], w=["NM"])
                                P.op("dve", lambda e: e.tensor_tensor_scan(out=NM[:, C:T][:, ::-1], data0=TA[:, C:T][:, ::-1], data1=TX[:, C:T][:, ::-1],
                                                                           initial=NM[:, 0:1], op0=ALU.mult, op1=ALU.add), r=["TA", "TX", "NM"], w=["NM"])
                                P.op("dve", lambda e: e.tensor_tensor(out=uh[:, :], in0=uh[:, :], in1=NM[:, :], op=ALU.add), r=["uh", "NM"], w=["uh"])
                        for ti, tt in enumerate(q_tiles):
                            s, e_ = TT[tt]
                            n = e_ - s
                            b = P.nb()
                            proj(kg_, tt, b)
                            g_ = gl[ti % 2]
                            P.op("act", lambda e: e.activation(out=g_[:, :n], in_=ps[b][:, :n], func=AF.Gelu_apprx_tanh), r=[psk(b)], w=[("gl", ti % 2)])
                            P.op("dve", lambda e: e.tensor_tensor(out=MX(4 + c, c0=s, c1=e_), in0=uh[:, s:e_], in1=g_[:, :n], op=ALU.mult),
                                 r=["uh", ("gl", ti % 2)], w=mk([4 + c], [tt]))
                    if l == 0:
                        assert lq[0] == NPIECE
                        mods_finish(0, 2, 6)
                    P.barrier()

                if dbg and stage == 2 and l == 0:
                    (mixA_box[0] is not None and P.dma("sp", dbgb_d[:, 0:4 * T], mixA_box[0][:].rearrange("p a b -> p (a b)"), "dbg", r=mk(range(4), range(5)))); P.dma("sp", dbgb_d[:, 4 * T:8 * T], mixR[:].rearrange("p a b -> p (a b)"), "dbg", r=mk(range(4, 8), range(5)))
                    break

                mixA_box[0] = sb(sm, "mixA%d" % l, [128, 4, T], BF16)
                with contextlib.ExitStack() as sa:
                    ropeT = sb(sa, "rope%d" % l, [128, 2, S], BF16)
                    for a_ in range(2):
                        P.dma("pool", ropeT[:, a_, :], rope_d[a_], "c_rope", w=["rope"], max_dma_last_dim=4096)
                    ATT_STOP = int(os.environ.get("ATT_STOP", "9"))
                    kT2 = sb(sa, "kT2_%d" % l, [128, 2, T], BF16)
                    vaug = sb(sa, "vaug%d" % l, [128, NCH, 2, 128], BF16)
                    qZ = [sb(sa, "qZ%d_%d" % (l, i), [128, T], BF16) for i in range(2)]
                    NPT = 3
                    pt = [sb(sa, "pt%d_%d" % (l, i), [128, 512], BF16) for i in range(NPT)]
                    rc = [sb(sa, "rc%d_%d" % (l, i), [64, 512], F32) for i in range(2)]
                    sqh = sb(sa, "sqh%d" % l, [128, 512], BF16)
                    rsh = sb(sa, "rsh%d" % l, [128, 512], F32)
                    qn = sb(sa, "qn%d" % l, [128, 512], F32)
                    P.op("dve", lambda e: e.memset(vaug[:, :, :, 64:128], 1.0), w=["vones"])
                    P.op("dve", lambda e: e.memset(qZ[0][64:128, :], 0.0), w=["qz0"])
                    P.op("dve", lambda e: e.memset(qZ[1][0:64, :], 0.0), w=["qz1"])

                    sqh2 = [sqh, sb(sa, "sqhB%d" % l, [128, 512], BF16)]
                    rsh2 = [rsh, sb(sa, "rshB%d" % l, [128, 512], F32)]
                    qn2 = [qn, sb(sa, "qnB%d" % l, [128, 512], F32)]
                    hnc = [0]

                    def pnr_pipeline(items):
                        st = {}
                        n_it = len(items)
                        for i in range(n_it + 2):
                            if i < n_it:
                                k_, tt, gname, dst, dkeys = items[i]
                                b = P.nb()
                                proj(k_, tt, b)
                                st[i] = dict(b=b)
                            if 0 <= i - 1 < n_it:
                                k_, tt, gname, dst, dkeys = items[i - 1]
                                s, e_ = TT[tt]
                                n = e_ - s
                                b = st[i - 1]["b"]
                                u_ = hnc[0] % 2
                                hnc[0] += 1
                                st[i - 1]["u"] = u_
                                sq_, rs_, q_ = sqh2[u_], rsh2[u_], qn2[u_]
                                P.op("act", lambda e: e.activation(out=sq_[:, :n], in_=ps[b][:, :n], func=AF.Square), r=[psk(b)], w=[("sqh", u_)])
                                b2 = P.nb()
                                P.op("pe", lambda e: e.matmul(ps[b2][:, :n], lhsT=bones_b, rhs=sq_[:, :n], start=True, stop=True), r=[("sqh", u_), "cmb"], w=[psk(b2)])
                                P.op("act", lambda e: e.activation(out=rs_[:, :n], in_=ps[b2][:, :n], func=AF.Ln, scale=1.0 / 64, bias=EPS), r=[psk(b2)], w=[("rsh", u_)])
                                P.op("act", lambda e: e.activation(out=rs_[:, :n], in_=rs_[:, :n], func=AF.Exp, scale=-0.5), r=[("rsh", u_)], w=[("rsh", u_)])
                                P.op("dve", lambda e: e.scalar_tensor_tensor(out=q_[:, :n], in0=ps[b][:, :n], scalar=pvi(gname, l), in1=rs_[:, :n],
                                                                             op0=ALU.mult, op1=ALU.mult), r=[psk(b), ("rsh", u_), "pv"], w=[("qn", u_)])
                                if tt > 0:
                                    b3 = P.nb()
                                    P.op("pe", lambda e: e.matmul(ps[b3][:, :n], lhsT=perm, rhs=q_[:, :n], start=True, stop=True), r=[("qn", u_), "cm"], w=[psk(b3)])
                                    st[i - 1]["b3"] = b3
                            if 0 <= i - 2 < n_it:
                                k_, tt, gname, dst, dkeys = items[i - 2]
                                s, e_ = TT[tt]
                                n = e_ - s
                                u_ = st[i - 2]["u"]
                                sq_, rs_, q_ = sqh2[u_], rsh2[u_], qn2[u_]
                                if tt > 0:
                                    b3 = st[i - 2]["b3"]
                                    P.op("dve", lambda e: e.tensor_tensor(out=q_[:, :n], in0=q_[:, :n], in1=ropeT[:, 0, s - C:e_ - C], op=ALU.mult),
                                         r=[("qn", u_), "rope"], w=[("qn", u_)])
                                    P.op("dve", lambda e: e.tensor_tensor(out=rs_[:, :n], in0=ps[b3][:, :n], in1=ropeT[:, 1, s - C:e_ - C], op=ALU.mult),
                                         r=[psk(b3), "rope"], w=[("rsh", u_)])
                                    for (p0, p1, d_) in dst:
                                        P.op("dve", lambda e: e.tensor_tensor(out=d_, in0=q_[p0:p1, :n], in1=rs_[p0:p1, :n], op=ALU.add),
                                             r=[("qn", u_), ("rsh", u_)], w=dkeys)
                                else:
                                    for (p0, p1, d_) in dst:
                                        P.op("act", lambda e: e.copy(out=d_, in_=q_[p0:p1, :n]), r=[("qn", u_)], w=dkeys)

                    k_items = []
                    for kvh in range(2):
                        c0 = 512 + kvh * 64
                        kk = wp_load([(0, 64, win_d[l][:, c0:c0 + 64]), (64, 128, win_d[l][:, c0:c0 + 64])])
                        for tt in all_tiles:
                            s, e_ = TT[tt]
                            k_items.append((kk, tt, "kg", [(0, 128, kT2[:, kvh, s:e_])], [("kT2", kvh, tt)]))
                    pnr_pipeline(k_items)
                    kv_ = wp_load([(0, 128, win_d[l][:, 640:768])])
                    for g4 in range(5):
                        chunks = list(range(g4 * 4, min(g4 * 4 + 4, NCH)))
                        b = P.nb()
                        for i, tc in enumerate(chunks):
                            tt_ = 0 if tc < 2 else 1 + (tc - 2) // 4
                            for kc in range(8):
                                P.op("pe", lambda e: e.matmul(ps[b][:, i * 128:(i + 1) * 128], lhsT=hT[:, kc, tc * 128:(tc + 1) * 128], rhs=wp[kv_][:, kc, :],
                                                              start=(kc == 0), stop=(kc == 7)),
                                     r=[("wp", kv_)] + hk([kc], [tt_]), w=[psk(b)], inc=(kc == 7 and i == len(chunks) - 1))
                        nch = len(chunks)
                        src = ps[b][:, 0:nch * 128].rearrange("p (i k d) -> p i k d", i=nch, k=2)
                        tc0 = chunks[0]
                        P.op("act", lambda e: e.copy(out=vaug[:, tc0:tc0 + nch, :, 0:64], in_=src), r=[psk(b)], w=[("vaug", g4, 0)])
                    vkeys = [("vaug", g4, 0) for g4 in range(5)] + ["vones"]

                    if ATT_STOP <= 2:
                        P.barrier(); break
                    SB = [0, 1, 2, 3]
                    sbc = [0]
                    ptc = [0]
                    qtc = [0]
                    for c in range(4):
                        kvh = c // 2
                        kq = wp_load([(0, 128, win_d[l][:, c * 128:(c + 1) * 128])])
                        pnr_pipeline([(kq, tt, "qg", [(0, 64, qZ[0][0:64, TT[tt][0]:TT[tt][1]]), (64, 128, qZ[1][64:128, TT[tt][0]:TT[tt][1]])], [("qT", tt)])
                                      for tt in q_tiles])
                        if ATT_STOP <= 3:
                            P.barrier(); break
                        gsteps = []
                        for tt in q_tiles:
                            kchunks = [0, 1] if tt == 0 else list(range(NCH))
                            po = (4, 5) if qtc[0] % 2 == 0 else (6, 7)
                            qtc[0] += 1
                            for kc in kchunks:
                                for j in range(2):
                                    gsteps.append((tt, po, j, kc, kc == kchunks[0], kc == kchunks[-1]))

                        def emit_qk(tt, j, kc):
                            s, e_ = TT[tt]
                            n = e_ - s
                            sbk = SB[sbc[0] % 4]
                            sbc[0] += 1
                            ktt = 0 if kc < 2 else 1 + (kc - 2) // 4
                            P.op("pe", lambda e: e.matmul(ps[sbk][:, :n], lhsT=kT2[:, kvh, kc * 128:(kc + 1) * 128],
                                                          rhs=qZ[j][:, s:e_], start=True, stop=True),
                                 r=[("kT2", kvh, ktt), ("qT", tt), "qz0", "qz1"], w=[psk(sbk)])
                            return sbk

                        def emit_pv(tt, po, j, kc, first, lastk, sbk):
                            s, e_ = TT[tt]
                            n = e_ - s
                            pk = ptc[0] % NPT
                            ptc[0] += 1
                            P.op("act", lambda e: e.activation(out=pt[pk][:, :n], in_=ps[sbk][:, :n], func=AF.Exp, scale=0.125),
                                 r=[psk(sbk)], w=[("pt", pk)])
                            P.op("pe", lambda e: e.matmul(ps[po[j]][:, :n], lhsT=vaug[:, kc, kvh, :], rhs=pt[pk][:, :n],
                                                          start=first, stop=lastk),
                                 r=[("pt", pk)] + vkeys, w=[psk(po[j])])
                            if lastk:
                                P.op("dve", lambda e: e.reciprocal(out=rc[j][0:64, :n], in_=ps[po[j]][64:128, :n]), r=[psk(po[j])], w=[("rc", j)])
                                P.op("dve", lambda e: e.tensor_tensor(out=MX(c, j * 64, j * 64 + 64, s, e_), in0=ps[po[j]][0:64, :n], in1=rc[j][0:64, :n], op=ALU.mult),
                                     r=[psk(po[j]), ("rc", j)], w=mk([c], [tt]))

                        LOOK = 3
                        pend = []
                        for st_ in gsteps:
                            pend.append(st_ + (emit_qk(st_[0], st_[2], st_[3]),))
                            if len(pend) > LOOK:
                                emit_pv(*pend.pop(0))
                        while pend:
                            emit_pv(*pend.pop(0))
                    P.barrier()

                if dbg and stage == 3 and l == 0:
                    (mixA_box[0] is not None and P.dma("sp", dbgb_d[:, 0:4 * T], mixA_box[0][:].rearrange("p a b -> p (a b)"), "dbg", r=mk(range(4), range(5)))); P.dma("sp", dbgb_d[:, 4 * T:8 * T], mixR[:].rearrange("p a b -> p (a b)"), "dbg", r=mk(range(4, 8), range(5)))
                    break

                with contextlib.ExitStack() as so:
                    osq = [sb(so, "osq%d_%d" % (l, i), [128, 512], BF16) for i in range(2)]
                    ors = [sb(so, "ors%d_%d" % (l, i), [128, 512], F32) for i in range(2)]
                    oi = 0
                    for half, gname in ((0, "aog"), (1, "log")):
                        for tt in q_tiles:
                            s, e_ = TT[tt]
                            n = e_ - s
                            b = P.nb()
                            for c4 in range(4):
                                cc_ = half * 4 + c4
                                k = c4 % 2
                                P.op("act", lambda e: e.activation(out=osq[k][:, :n], in_=MX(cc_, c0=s, c1=e_), func=AF.Square), r=mk([cc_], [tt]), w=[("osq", k)])
                                P.op("pe", lambda e: e.matmul(ps[b][:, :n], lhsT=ones_b, rhs=osq[k][:, :n], start=(c4 == 0), stop=(c4 == 3)),
                                     r=[("osq", k), "cmb"], w=[psk(b)])
                            r_ = ors[oi % 2]
                            rk = ("ors", oi % 2)
                            oi += 1
                            P.op("act", lambda e: e.activation(out=r_[:, :n], in_=ps[b][:, :n], func=AF.Ln, scale=1.0 / 512, bias=EPS), r=[psk(b)], w=[rk])
                            P.op("act", lambda e: e.activation(out=r_[:, :n], in_=r_[:, :n], func=AF.Exp, scale=-0.5), r=[rk], w=[rk])
                            for c4 in range(4):
                                cc_ = half * 4 + c4
                                P.op("dve", lambda e: e.scalar_tensor_tensor(out=MX(cc_, c0=s, c1=e_), in0=MX(cc_, c0=s, c1=e_), scalar=pvi(gname, l, c4),
                                                                             in1=r_[:, :n], op0=ALU.mult, op1=ALU.mult), r=mk([cc_], [tt]) + [rk, "pv"], w=mk([cc_], [tt]))
                    if dbg and stage == 4 and l == 0:
                        (mixA_box[0] is not None and P.dma("sp", dbgb_d[:, 0:4 * T], mixA_box[0][:].rearrange("p a b -> p (a b)"), "dbg", r=mk(range(4), range(5)))); P.dma("sp", dbgb_d[:, 4 * T:8 * T], mixR[:].rearrange("p a b -> p (a b)"), "dbg", r=mk(range(4, 8), range(5)))
                    for o in range(8):
                        ko = wp_load([(0, 128, wout_d[l][:, o * 128:(o + 1) * 128])])
                        for tt in q_tiles:
                            s, e_ = TT[tt]
                            n = e_ - s
                            jj = 1 if tt == 0 else 0
                            b = P.nb()
                            for kc in range(8):
                                P.op("pe", lambda e: e.matmul(ps[b][:, :n], lhsT=wp[ko][:, kc, :], rhs=MX(kc, c0=s, c1=e_), start=(kc == 0), stop=(kc == 7)),
                                     r=[("wp", ko)] + mk([kc], [tt]), w=[psk(b)], inc=(kc == 7))
                            P.op("dve", lambda e: e.scalar_tensor_tensor(out=xT[:, o, s:e_], in0=ps[b][:, :n], scalar=modv[:, l, 2 * 8 + o, jj:jj + 1],
                                                                         in1=xT[:, o, s:e_], op0=ALU.mult, op1=ALU.add),
                                 r=[psk(b), ("modv", 2)] + xk([o], [tt]), w=xk([o], [tt]))
                    P.barrier()
            if dbg and stage in (1, 2, 3) and l == 0:
                break
            if dbg and stage == 4 and l == 0:
                P.dma("sp", dbgf_d[:, 0:8 * T], xT[:].rearrange("p a b -> p (a b)"), "dbg", r=xk(range(8), range(5)))
                break

            with contextlib.ExitStack() as se:
                NEP = 6
                ep = [sb(se, "ep%d_%d" % (l, i), [128, 4096], BF16) for i in range(NEP)]
                epc = [0]
                sel = sb(se, "sel%d" % l, [128, 16, 128], BF16)
                P.dma("pool", sel[:], cmb_d[:, 256:256 + 2048].rearrange("p (a m) -> p a m", m=128), "c_sel", w=["sel"])
                combT = sb(se, "combT%d" % l, [128, T], BF16)
                cbt = sb(se, "cbt%d" % l, [128, NCH, 16], F32)

                def ep_load(e_i):
                    ks = []
                    for wi, (wd_, pat) in enumerate(((wg_d, "g"), (wu_d, "u"), (wd_d, "d"))):
                        k = epc[0] % NEP
                        epc[0] += 1
                        if pat == "d":
                            P.dma("pool", ep[k][:].rearrange("p (j n) -> p j n", j=4), wd_[l][e_i].rearrange("(j p) n -> p j n", p=128), "ep%d" % k, w=[("ep", k)])
                        else:
                            P.dma("pool", ep[k][:].rearrange("p (j n) -> p j n", j=8), wd_[l][e_i].rearrange("(j p) n -> p j n", p=128), "ep%d" % k, w=[("ep", k)])
                        ks.append(k)
                    return ks

                m_tiles = lat_tiles if last else all_tiles
                ch0 = 2 if last else 0
                nchs = NCH - ch0
                eks = {0: ep_load(0)}
                with contextlib.ExitStack() as sg:
                    sg2 = contextlib.ExitStack()
                    rl = P.nb()
                    P.reserved.add(rl)
                    rwg = sb(sg2, "rwg%d" % l, [128, 2, 8, 16], F32)
                    cst = sb(sg2, "cst%d" % l, [16, 2], F32)
                    lgt = [sb(sg2, "lgt%d_%d" % (l, i), [16, 512], F32) for i in range(2)]
                    eT = sb(sg2, "eT%d" % l, [16, T], F32)
                    for jj in range(2):
                        for kc in range(8):
                            P.op("dve", lambda e: e.tensor_scalar(out=rwg[:, jj, kc, :], in0=rw[:, kc * 16:(kc + 1) * 16], scalar1=gsv[:, l, 1, kc, jj:jj + 1],
                                                                  scalar2=None, op0=ALU.mult), r=["rw", ("gsv", 1)], w=["rwg"])
                    bc = P.nb()
                    for kc in range(8):
                        P.op("pe", lambda e: e.matmul(ps[bc][0:16, 0:2], lhsT=rw[:, kc * 16:(kc + 1) * 16], rhs=modv[:, l, 3 * 8 + kc, :], start=(kc == 0), stop=(kc == 7)),
                             r=["rw", ("modv", 3)], w=[psk(bc)], inc=(kc == 7))
                    P.op("dve", lambda e: e.tensor_scalar(out=cst[:, :], in0=ps[bc][0:16, 0:2], scalar1=-1.0, scalar2=None, op0=ALU.mult), r=[psk(bc)], w=["cst"])

                    def router_tile(ti, tt, r_, rk):
                        s, e_ = TT[tt]
                        n = e_ - s
                        jj = 1 if tt == 0 else 0
                        b = P.nb()
                        for kc in range(8):
                            P.op("pe", lambda e: e.matmul(ps[b][0:16, :n], lhsT=rwg[:, jj, kc, :], rhs=xT[:, kc, s:e_], start=(kc == 0), stop=(kc == 7)),
                                 r=["rwg"] + xk([kc], [tt]), w=[psk(b)], inc=(kc == 7))
                        lg_ = lgt[ti % 2]
                        def fin():
                            P.op("dve", lambda e: e.tensor_tensor(out=lg_[:, :n], in0=ps[b][0:16, :n], in1=r_[0:16, :n], op=ALU.mult), r=[psk(b), rk], w=[("lgt", ti % 2)])
                            P.op("act", lambda e: e.activation(out=eT[:, s:e_], in_=lg_[:, :n], func=AF.Exp, scale=-1.0, bias=cst[:, jj:jj + 1]),
                                 r=[("lgt", ti % 2), "cst"], w=[("eT", tt)])
                        rdefer.append(fin)

                    rdefer = []

                    def flush_router(tt):
                        while rdefer:
                            rdefer.pop(0)()

                    norm_mod(l, 1, m_tiles, sg2, post_rstd=router_tile, after_tile=flush_router)
                    flush_router(None)
                    for ch in range(ch0, NCH):
                        tt_ = 0 if ch < 2 else 1 + (ch - 2) // 4
                        P.op("pe", lambda e: e.transpose(out=ps[rl][:, ch * 16:(ch + 1) * 16], in_=eT[0:16, ch * 128:(ch + 1) * 128], identity=ident[0:16, 0:16]),
                             r=[("eT", tt_), "cm"], w=[psk(rl)], inc=(ch == NCH - 1))
                    P.barrier()
                    sg2.close()

                    def rt(name):
                        return sb(sg, "%s_%d" % (name, l), [128, NCH * 16], F32)

                    sc = rt("r_sc"); bi = rt("r_bi"); selm = rt("r_sel")
                    m1 = rt("r_m1"); m2 = rt("r_m2"); mn = rt("r_mn"); gsum = rt("r_gs"); gmax = rt("r_gm"); ing = rt("r_in"); den = rt("r_den")
                    lo, hi = ch0 * 16, NCH * 16
                    nq = nchs * 4
                    P.op("dve", lambda e: e.tensor_scalar(out=sc[:, lo:hi], in0=ps[rl][:, lo:hi], scalar1=1.0, scalar2=None, op0=ALU.add), r=[psk(rl)], w=["r_sc"])
                    P.op("dve", lambda e: e.reciprocal(out=sc[:, lo:hi], in_=sc[:, lo:hi]), r=["r_sc"], w=["r_sc"])
                    orb = PV_OFF["rb"][0]
                    P.op("dve", lambda e: e.tensor_tensor(out=bi[:, lo:hi], in0=sc[:, lo:hi], in1=pv[:, orb + lo:orb + hi], op=ALU.add), r=["r_sc", "pv"], w=["r_bi"])
                    g4v = bi[:, lo:hi].rearrange("p (q i) -> p q i", i=4)
                    P.op("dve", lambda e: e.tensor_reduce(out=m1[:, 0:nq], in_=g4v, axis=AX.X, op=ALU.max), r=["r_bi"], w=["r_m1"])
                    first = True
                    for i in range(4):
                        for j in range(i + 1, 4):
                            if first:
                                P.op("dve", lambda e: e.tensor_tensor(out=m2[:, 0:nq], in0=g4v[:, :, i], in1=g4v[:, :, j], op=ALU.min), r=["r_bi"], w=["r_m2"])
                                first = False
                            else:
                                P.op("dve", lambda e: e.tensor_tensor(out=mn[:, 0:nq], in0=g4v[:, :, i], in1=g4v[:, :, j], op=ALU.min), r=["r_bi"], w=["r_mn"])
                                P.op("dve", lambda e: e.tensor_tensor(out=m2[:, 0:nq], in0=m2[:, 0:nq], in1=mn[:, 0:nq], op=ALU.max), r=["r_m2", "r_mn"], w=["r_m2"])
                    P.op("dve", lambda e: e.tensor_tensor(out=gsum[:, 0:nq], in0=m1[:, 0:nq], in1=m2[:, 0:nq], op=ALU.add), r=["r_m1", "r_m2"], w=["r_gs"])
                    gsv3 = gsum[:, 0:nq].rearrange("p (c g) -> p c g", g=4)
                    P.op("dve", lambda e: e.tensor_reduce(out=gmax[:, 0:nchs], in_=gsv3, axis=AX.X, op=ALU.max), r=["r_gs"], w=["r_gm"])
                    ing3 = ing[:, 0:nq].rearrange("p (c g) -> p c g", g=4)
                    for g in range(4):
                        P.op("dve", lambda e: e.tensor_tensor(out=ing3[:, :, g], in0=gsv3[:, :, g], in1=gmax[:, 0:nchs], op=ALU.is_ge), r=["r_gs", "r_gm"], w=["r_in"])
                    sel3 = selm[:, lo:hi].rearrange("p (q i) -> p q i", i=4)
                    for i in range(4):
                        P.op("dve", lambda e: e.tensor_tensor(out=sel3[:, :, i], in0=g4v[:, :, i], in1=m2[:, 0:nq], op=ALU.is_ge), r=["r_bi", "r_m2"], w=["r_sel"])
                        P.op("dve", lambda e: e.tensor_tensor(out=sel3[:, :, i], in0=sel3[:, :, i], in1=ing[:, 0:nq], op=ALU.mult), r=["r_sel", "r_in"], w=["r_sel"])
                    P.op("dve", lambda e: e.tensor_tensor(out=selm[:, lo:hi], in0=selm[:, lo:hi], in1=sc[:, lo:hi], op=ALU.mult), r=["r_sel", "r_sc"], w=["r_sel"])
                    P.op("dve", lambda e: e.tensor_reduce(out=den[:, 0:nchs], in_=selm[:, lo:hi].rearrange("p (c x) -> p c x", x=16), axis=AX.X, op=ALU.add),
                         r=["r_sel"], w=["r_den"])
                    P.op("dve", lambda e: e.reciprocal(out=den[:, 0:nchs], in_=den[:, 0:nchs]), r=["r_den"], w=["r_den"])
                    for ci in range(nchs):
                        ch = ch0 + ci
                        P.op("dve", lambda e: e.tensor_scalar(out=cbt[:, ch, :], in0=selm[:, ch * 16:(ch + 1) * 16], scalar1=den[:, ci:ci + 1], scalar2=None, op0=ALU.mult),
                             r=["r_sel", "r_den"], w=[("cbt", ch)])
                    for g4 in range((nchs + 3) // 4):
                        chs = list(range(ch0 + g4 * 4, min(ch0 + g4 * 4 + 4, NCH)))
                        b = P.nb()
                        for i, ch in enumerate(chs):
                            P.op("pe", lambda e: e.transpose(out=ps[b][0:16, i * 128:(i + 1) * 128], in_=cbt[:, ch, :], identity=ident),
                                 r=[("cbt", ch), "cm"], w=[psk(b)], inc=(i == len(chs) - 1))
                        P.op("act", lambda e: e.copy(out=combT[0:16, chs[0] * 128:(chs[-1] + 1) * 128], in_=ps[b][0:16, 0:len(chs) * 128]), r=[psk(b)], w=["combT"])
                    P.reserved.discard(rl)
                    P.barrier()

                if dbg and stage == 5 and l == 0:
                    P.dma("sp", dbgb_d[:, 0:8 * T], hT[:].rearrange("p a b -> p (a b)"), "dbg", r=hk(range(8), range(5)))
                    P.dma("sp", dbgf_d[:, 0:NCH * 16], cbt[:].rearrange("p a b -> p (a b)"), "dbg", r=[("cbt", ch) for ch in range(NCH)])
                    break

                with contextlib.ExitStack() as sh:
                    cbe = [sb(sh, "cbe%d_%d" % (l, i), [128, T], BF16) for i in range(2)]
                    sgt = [sb(sh, "sgt%d_%d" % (l, i), [128, 512], BF16) for i in range(2)]
                    hid = [sb(sh, "hid%d_%d" % (l, i), [128, 4, 512], BF16) for i in range(2)]
                    sgc = [0]
                    hdc = [0]
                    if l + 1 < NL:
                        adaw2 = [sb(sh, "adawL%d_%d" % (l, i), [128, 8, 256], BF16) for i in range(2)]
                        mods_begin(l + 1)
                        mp = [0]
                        mq = [0]
                    for ei in range(NE):
                        if ei + 1 < NE:
                            eks[ei + 1] = ep_load(ei + 1)
                        if l + 1 < NL:
                            while mq[0] < mp[0]:
                                mods_piece(l + 1, mq[0], adaw2, dma=False)
                                mq[0] += 1
                            for _ in range(2):
                                if mp[0] < NPIECE:
                                    mods_dma(l + 1, mp[0], adaw2)
                                    mp[0] += 1
                        kg_, ku_, kd_ = eks[ei]
                        wgv = ep[kg_][:].rearrange("p (j n) -> p j n", j=8)
                        wuv = ep[ku_][:].rearrange("p (j n) -> p j n", j=8)
                        wdv = ep[kd_][:].rearrange("p (j n) -> p j n", j=4)
                        cb_ = cbe[ei % 2]
                        ck = ("cbe", ei % 2)
                        for tt in m_tiles:
                            s, e_ = TT[tt]
                            n = e_ - s
                            b = P.nb()
                            P.op("pe", lambda e: e.matmul(ps[b][:, :n], lhsT=sel[0:16, ei, :], rhs=combT[0:16, s:e_], start=True, stop=True),
                                 r=["sel", "combT"], w=[psk(b)])
                            P.op("act", lambda e: e.copy(out=cb_[:, s:e_], in_=ps[b][:, :n]), r=[psk(b)], w=[ck])
                        for tt in m_tiles:
                            s, e_ = TT[tt]
                            n = e_ - s
                            jj = 1 if tt == 0 else 0
                            hd = hid[hdc[0] % 2]
                            hkey = ("hid", hdc[0] % 2)
                            hdc[0] += 1
                            for f in range(4):
                                bg = P.nb()
                                for kc in range(8):
                                    P.op("pe", lambda e: e.matmul(ps[bg][:, :n], lhsT=wgv[:, kc, f * 128:(f + 1) * 128], rhs=hT[:, kc, s:e_], start=(kc == 0), stop=(kc == 7)),
                                         r=[("ep", kg_)] + hk([kc], [tt]), w=[psk(bg)], inc=(kc == 7))
                                bu = P.nb()
                                for kc in range(8):
                                    P.op("pe", lambda e: e.matmul(ps[bu][:, :n], lhsT=wuv[:, kc, f * 128:(f + 1) * 128], rhs=hT[:, kc, s:e_], start=(kc == 0), stop=(kc == 7)),
                                         r=[("ep", ku_)] + hk([kc], [tt]), w=[psk(bu)], inc=(kc == 7))
                                sk = sgc[0] % 2
                                sgc[0] += 1
                                P.op("act", lambda e: e.activation(out=sgt[sk][:, :n], in_=ps[bg][:, :n], func=AF.Silu), r=[psk(bg)], w=[("sgt", sk)])
                                P.op("dve", lambda e: e.tensor_tensor(out=sgt[sk][:, :n], in0=sgt[sk][:, :n], in1=cb_[:, s:e_], op=ALU.mult), r=[("sgt", sk), ck], w=[("sgt", sk)])
                                P.op("dve", lambda e: e.tensor_tensor(out=hd[:, f, :n], in0=ps[bu][:, :n], in1=sgt[sk][:, :n], op=ALU.mult), r=[psk(bu), ("sgt", sk)], w=[hkey])
                            for o in range(8):
                                bo = P.nb()
                                for jc in range(4):
                                    P.op("pe", lambda e: e.matmul(ps[bo][:, :n], lhsT=wdv[:, jc, o * 128:(o + 1) * 128], rhs=hd[:, jc, :n], start=(jc == 0), stop=(jc == 3)),
                                         r=[("ep", kd_), hkey], w=[psk(bo)], inc=(jc == 3))
                                P.op("dve", lambda e: e.scalar_tensor_tensor(out=xT[:, o, s:e_], in0=ps[bo][:, :n], scalar=modv[:, l, 5 * 8 + o, jj:jj + 1],
                                                                             in1=xT[:, o, s:e_], op0=ALU.mult, op1=ALU.add),
                                     r=[psk(bo), ("modv", 5)] + xk([o], [tt]), w=xk([o], [tt]))
                    if l + 1 < NL:
                        assert mp[0] == NPIECE
                        while mq[0] < mp[0]:
                            mods_piece(l + 1, mq[0], adaw2, dma=False)
                            mq[0] += 1
                        mods_finish(l + 1)
                    P.barrier()
            if dbg and stage == 5 and l == 0:
                break
            if dbg and stage == 6 and l == 0:
                P.dma("sp", dbgf_d[:, 0:8 * T], xT[:].rearrange("p a b -> p (a b)"), "dbg", r=xk(range(8), range(5)))
                break

        if not dbg or stage >= 99:
            with contextlib.ExitStack() as sf:
                fsq = [sb(sf, "fsq%d" % i, [128, 512], BF16) for i in range(2)]
                frs = [sb(sf, "frs%d" % i, [128, 512], F32) for i in range(2)]
                yT = [sb(sf, "yT%d" % i, [128, 8, 512], F32) for i in range(2)]
                ob = [sb(sf, "ob%d" % i, [128, D], F32) for i in range(2)]
                obc = 0
                ofg = PV_OFF["fg"][0]
                obc_box = [0]

                def finA(ti, tt):
                    s, e_ = TT[tt]
                    n = e_ - s
                    b = P.nb()
                    for c in range(8):
                        k = c % 2
                        P.op("act", lambda e: e.activation(out=fsq[k][:, :n], in_=xT[:, c, s:e_], func=AF.Square), r=xk([c], [tt]), w=[("fsq", k)])
                        P.op("pe", lambda e: e.matmul(ps[b][:, :n], lhsT=ones_b, rhs=fsq[k][:, :n], start=(c == 0), stop=(c == 7)),
                             r=[("fsq", k), "cmb"], w=[psk(b)])
                    r_ = frs[ti % 2]
                    rk = ("frs", ti % 2)
                    P.op("act", lambda e: e.activation(out=r_[:, :n], in_=ps[b][:, :n], func=AF.Ln, scale=1.0 / D, bias=EPS), r=[psk(b)], w=[rk])
                    P.op("act", lambda e: e.activation(out=r_[:, :n], in_=r_[:, :n], func=AF.Exp, scale=-0.5), r=[rk], w=[rk])

                def finB(ti, tt):
                    s, e_ = TT[tt]
                    n = e_ - s
                    r_ = frs[ti % 2]
                    rk = ("frs", ti % 2)
                    y_ = yT[ti % 2]
                    for c in range(8):
                        P.op("dve", lambda e: e.scalar_tensor_tensor(out=y_[:, c, :n], in0=xT[:, c, s:e_], scalar=pv[:, ofg + c:ofg + c + 1], in1=r_[:, :n],
                                                                     op0=ALU.mult, op1=ALU.mult), r=xk([c], [tt]) + [rk, "pv"], w=[("yT", ti % 2, c)])
                    for i in range(4):
                        k = obc_box[0] % 2
                        obc_box[0] += 1
                        for half in range(2):
                            b2 = P.nb()
                            for q in range(4):
                                c = half * 4 + q
                                P.op("pe", lambda e: e.transpose(out=ps[b2][:, q * 128:(q + 1) * 128], in_=y_[:, c, i * 128:(i + 1) * 128], identity=ident),
                                     r=[("yT", ti % 2, c), "cm"], w=[psk(b2)], inc=(q == 3))
                            if half == 0:
                                P.op("act", lambda e: e.copy(out=ob[k][:, 0:512], in_=ps[b2][:, :]), r=[psk(b2)], w=[("ob", k, 0)])
                            else:
                                P.op("dve", lambda e: e.tensor_copy(out=ob[k][:, 512:1024], in_=ps[b2][:, :]), r=[psk(b2)], w=[("ob", k, 1)])
                        tok = (s - C) + i * 128
                        P.dma("sp", out_d[tok:tok + 128, :], ob[k][:], "o%d" % k, r=[("ob", k, 0), ("ob", k, 1)])

                ftiles = [1, 2, 3, 4]
                for i in range(len(ftiles) + 1):
                    if i < len(ftiles):
                        finA(i, ftiles[i])
                    if i >= 1:
                        finB(i - 1, ftiles[i - 1])
                P.barrier()
        for k, v in P.cnt.items():
            if k not in P.E and v > 0:
                P.E["sp"].wait_ge(P.sem[k], v)
                P._pw["sp"].append((k, v))
        P.check_deadlock()
    return nc


def _chunked(v):
    v = np.asarray(v, np.float32)
    k = v.shape[-1] // 128
    v = v.reshape(v.shape[:-1] + (k, 128))
    return np.moveaxis(v, -1, 0)


def _rope_tables():
    t = np.arange(S)
    r = (t // 64).astype(np.float32)
    col = (t % 64).astype(np.float32)
    inv = (np.float32(10000.0) ** (-np.arange(0, 32, 2, dtype=np.float32) / np.float32(32))).astype(np.float32)
    cosT = np.zeros((128, S), np.float32)
    sinT = np.zeros((128, S), np.float32)
    for p in range(128):
        d = p % 64
        axis, half, f = d // 32, (d % 32) // 16, d % 16
        pos = r if axis == 0 else col
        ang = (pos * inv[f]).astype(np.float32)
        cosT[p] = np.cos(ang)
        sinT[p] = np.sin(ang) * (-1.0 if half == 0 else 1.0)
    return np.stack([cosT, sinT]).astype(np.float32)


def _consts():
    ident = np.eye(128, dtype=np.float32)
    perm = np.zeros((128, 128), np.float32)
    for m in range(128):
        half = (m % 32) // 16
        k = m + 16 if half == 0 else m - 16
        perm[k, m] = 1.0
    cmat = np.concatenate([ident, perm], axis=1)
    ones = np.ones((128, 128), np.float32)
    bones = np.zeros((128, 128), np.float32)
    bones[0:64, 0:64] = 1.0
    bones[64:128, 64:128] = 1.0
    sel = np.zeros((128, 16, 128), np.float32)
    for e in range(16):
        sel[e, e, :] = 1.0
    cmatb = np.concatenate([ones, bones, sel.reshape(128, 2048)], axis=1)
    return cmat, cmatb


def _pvec(inp, b):
    pvv = np.zeros((128, NPV), np.float32)

    def put(name, arr):
        o, s = PV_OFF[name]
        n = int(np.prod(s))
        pvv[:, o:o + n] = np.asarray(arr, np.float32).reshape(128, n)

    cc = np.stack([inp["c"][b], inp["c_ctx"]], axis=0)
    put("cc", np.moveaxis(_chunked(cc), 1, 2))
    put("adab", _chunked(inp["ada_b"]))
    put("n1g", _chunked(inp["norm1_g"]))
    put("n2g", _chunked(inp["norm2_g"]))
    put("fg", _chunked(inp["final_g"]))
    put("qg", np.tile(inp["q_norm_g"].T, (2, 1)))
    put("kg", np.tile(inp["k_norm_g"].T, (2, 1)))
    put("cw", _chunked(inp["conv_w"]))
    put("cb", _chunked(inp["conv_b"]))
    put("lba", _chunked(inp["lru_ba"]))
    put("lbx", _chunked(inp["lru_bx"]))
    put("lam", _chunked(inp["lru_lambda"]))
    put("aog", _chunked(inp["attn_out_g"]))
    put("log", _chunked(inp["lru_out_g"]))
    put("rb", np.broadcast_to(inp["router_b"].reshape(1, 1, 16), (128, NCH, 16)))
    return pvv


def _lru_blockdiag(inp):
    out = np.zeros((NL, 128, 2, 2, 4, 128), np.float32)
    for gi, name in enumerate(("lru_wa", "lru_wx")):
        w = np.asarray(inp[name], np.float32)
        for c in range(4):
            out[:, 0:64, :, gi, c, 0:64] = np.transpose(w[:, :, 2 * c], (0, 2, 1, 3))
            out[:, 64:128, :, gi, c, 64:128] = np.transpose(w[:, :, 2 * c + 1], (0, 2, 1, 3))
    return out.reshape(NL, 128, 2048)


_CACHE = {}


def _get_nc():
    if "nc" not in _CACHE:
        _CACHE["nc"] = build()
    return _CACHE["nc"]


def make_in_maps(inp):
    inp = {k: np.asarray(v) for k, v in inp.items()}
    cmat, cmatb = _consts()
    rope = _rope_tables()
    lrubd = _lru_blockdiag(inp)
    rwl = np.ascontiguousarray(_chunked(inp["router_w"].T).transpose(0, 2, 1)).reshape(128, 128)
    shared = dict(cmat=cmat, cmatb=cmatb, rope=rope, ada_w=np.ascontiguousarray(inp["ada_w"], np.float32),
                  w_in=np.ascontiguousarray(inp["w_in"], np.float32), lrubd=lrubd,
                  w_out=np.ascontiguousarray(inp["w_out"], np.float32), rw=rwl,
                  wg=np.ascontiguousarray(inp["exp_w_gate"], np.float32), wu=np.ascontiguousarray(inp["exp_w_up"], np.float32),
                  wd=np.ascontiguousarray(inp["exp_w_down"], np.float32))
    maps = []
    for b in range(8):
        m = dict(shared)
        m["x"] = np.ascontiguousarray(inp["x"][b], np.float32)
        m["ctx"] = np.ascontiguousarray(inp["ctx"][b], np.float32)
        m["pvec"] = _pvec(inp, b)
        maps.append(m)
    return maps


def kernel(**inputs):
    nc = _get_nc()
    maps = make_in_maps(inputs)
    res = run_bass_kernel_spmd(nc, maps, core_ids=list(range(8)))
    return np.stack([np.asarray(r["out"], np.float32) for r in res.results], axis=0)
```

```python
import contextlib
import os
import numpy as np
import concourse.bass as bass
import concourse.mybir as mybir
from concourse.bass_utils import run_bass_kernel_spmd

F32 = mybir.dt.float32
BF16 = mybir.dt.bfloat16
AF = mybir.ActivationFunctionType
ALU = mybir.AluOpType
AX = mybir.AxisListType

D = 1024
S = 2048
C = 256
T = S + C
NL = 2
NE = 16
DE = 512
INW = 1792
TT = [(0, 256), (256, 768), (768, 1280), (1280, 1792), (1792, 2304)]
NCH = T // 128
EPS = 1e-6

PV_ITEMS = [
    ("cc", (8, 2)), ("adab", (NL, 48)), ("n1g", (NL, 8)), ("n2g", (NL, 8)), ("fg", (8,)),
    ("qg", (NL,)), ("kg", (NL,)), ("cw", (NL, 4, 4)), ("cb", (NL, 4)),
    ("lba", (NL, 2, 4)), ("lbx", (NL, 2, 4)), ("lam", (NL, 2, 4)),
    ("aog", (NL, 4)), ("log", (NL, 4)), ("rb", (NCH, 16)),
]
PV_OFF = {}
_o = 0
for _n, _s in PV_ITEMS:
    PV_OFF[_n] = (_o, _s)
    _o += int(np.prod(_s))
NPV = _o


class Prog:
    def __init__(self, nc, es):
        self.nc = nc
        self.es = es
        self.E = dict(pe=nc.tensor, act=nc.scalar, dve=nc.vector, pool=nc.gpsimd, sp=nc.sync)
        self.sem = {}
        self.cnt = {}
        for k in self.E:
            self.sem[k] = es.enter_context(nc.semaphore("sem_" + k))
            self.cnt[k] = 0
        self.seen = {k: {} for k in self.E}
        self.res = {}
        self.q = {k: [] for k in self.E}
        self._pw = {k: [] for k in self.E}
        self._bank = 0
        self.reserved = set()

    def _deps(self, eng, r, w, skip=None):
        need = {}
        for key in r:
            st = self.res.get(key)
            if st and st[0] is not None:
                k, v = st[0]
                if need.get(k, 0) < v:
                    need[k] = v
        for key in w:
            st = self.res.get(key)
            if st:
                if st[0] is not None:
                    k, v = st[0]
                    if need.get(k, 0) < v:
                        need[k] = v
                for k, v in st[1].items():
                    if need.get(k, 0) < v:
                        need[k] = v
        for k, v in need.items():
            if k == skip:
                continue
            if k == eng:
                if eng == "pe" or v > self.cnt[eng]:
                    continue
            if self.seen[eng].get(k, 0) < v:
                self.E[eng].wait_ge(self.sem[k], v)
                self.seen[eng][k] = v
                self._pw[eng].append((k, v))

    def _mark(self, tag, r, w):
        k, v = tag
        for key in r:
            st = self.res.setdefault(key, [None, {}])
            if st[1].get(k, 0) < v:
                st[1][k] = v
        for key in w:
            self.res[key] = [tag, {}]

    def op(self, eng, fn, r=(), w=(), inc=True):
        pr = [k for k in r if isinstance(k, tuple) and k[0] == "ps"]
        if pr:
            w = list(w) + pr
        self._deps(eng, r, w)
        inst = fn(self.E[eng])
        self.q[eng].append((self._pw[eng], (eng, 1) if inc else None))
        self._pw[eng] = []
        if inc:
            self.cnt[eng] += 1
            inst.then_inc(self.sem[eng], 1)
            tag = (eng, self.cnt[eng])
        else:
            tag = (eng, self.cnt[eng] + 1)
        self._mark(tag, r, w)
        return inst

    def dma(self, q, out, in_, semkey, r=(), w=(), **kw):
        if semkey not in self.sem:
            self.sem[semkey] = self.es.enter_context(self.nc.semaphore("d_" + semkey))
            self.cnt[semkey] = 0
        self._deps(q, r, w, skip=semkey)
        inst = self.E[q].dma_start(out=out, in_=in_, **kw)
        self.q[q].append((self._pw[q], (semkey, 16)))
        self._pw[q] = []
        self.cnt[semkey] += 16
        inst.then_inc(self.sem[semkey], 16)
        self._mark((semkey, self.cnt[semkey]), r, w)
        return inst

    def barrier(self):
        for e in self.E:
            for k, v in self.cnt.items():
                if k != e and v > 0 and self.seen[e].get(k, 0) < v:
                    self.E[e].wait_ge(self.sem[k], v)
                    self.seen[e][k] = v
                    self._pw[e].append((k, v))

    def check_deadlock(self):
        val = {k: 0 for k in self.cnt}
        pos = {k: 0 for k in self.E}
        for e in self.E:
            if self._pw[e]:
                self.q[e].append((self._pw[e], None))
                self._pw[e] = []
        progress = True
        while progress:
            progress = False
            for e in self.E:
                ql = self.q[e]
                while pos[e] < len(ql):
                    waits, inc = ql[pos[e]]
                    if all(val[k] >= v for k, v in waits):
                        if inc is not None:
                            val[inc[0]] += inc[1]
                        pos[e] += 1
                        progress = True
                    else:
                        break
        stuck = {e: (pos[e], len(self.q[e]), self.q[e][pos[e]][0]) for e in self.E if pos[e] < len(self.q[e])}
        if stuck:
            raise RuntimeError("DEADLOCK in semaphore plan: %r ; vals=%r" % (stuck, {k: val[k] for e in stuck for k, _ in stuck[e][2]}))

    def nb(self):
        while True:
            b = self._bank
            self._bank = (self._bank + 1) % 8
            if b not in self.reserved:
                return b


def build(stage=99, dbg=False):
    nc = bass.Bass("TRN2", target_bir_lowering=False)

    def dten(name, shape, dty=F32, kind="ExternalInput"):
        return nc.dram_tensor(name, shape, dty, kind=kind).ap()

    x_d = dten("x", [S, D])
    ctx_d = dten("ctx", [C, D])
    pv_d = dten("pvec", [128, NPV])
    cm_d = dten("cmat", [128, 256])
    cmb_d = dten("cmatb", [128, 256 + 2048])
    rope_d = dten("rope", [2, 128, S])
    adaw_d = dten("ada_w", [NL, D, 6 * D])
    win_d = dten("w_in", [NL, D, INW])
    lru_d = dten("lrubd", [NL, 128, 2048])
    wout_d = dten("w_out", [NL, D, D])
    rw_d = dten("rw", [128, 128])
    wg_d = dten("wg", [NL, NE, D, DE])
    wu_d = dten("wu", [NL, NE, D, DE])
    wd_d = dten("wd", [NL, NE, DE, D])
    out_d = dten("out", [S, D], kind="ExternalOutput")
    if dbg:
        dbgf_d = dten("dbgf", [128, 8 * T], F32, kind="ExternalOutput")
        dbgb_d = dten("dbgb", [128, 8 * T], BF16, kind="ExternalOutput")

    with contextlib.ExitStack() as es:
        P = Prog(nc, es)

        def sb(stack, name, shape, dty):
            return stack.enter_context(nc.sbuf_tensor("s_" + name, shape, dty))

        ps = [es.enter_context(nc.psum_tensor("ps%d" % i, [128, 512], F32)) for i in range(8)]

        def psk(b):
            return ("ps", b)

        xT = sb(es, "xT", [128, 8, T], F32)
        hT = sb(es, "hT", [128, 8, T], BF16)
        pv = sb(es, "pv", [128, NPV], F32)
        cm = sb(es, "cm", [128, 256], F32)
        cmb = sb(es, "cmb", [128, 256], BF16)
        scb = sb(es, "scb", [128, 8, 2], BF16)
        modv = sb(es, "modv", [128, NL, 48, 2], F32)
        gsv = sb(es, "gsv", [128, NL, 2, 8, 2], F32)
        hsp = sb(es, "hsp", [128, NL * 2 * 4], F32)
        hba = sb(es, "hba", [128, NL * 2 * 4], F32)
        hbx = sb(es, "hbx", [128, NL * 2 * 4], F32)
        rw = sb(es, "rw", [128, 128], F32)
        smalltmp = sb(es, "smalltmp", [128, 64], F32)

        ident = cm[:, 0:128]
        perm = cm[:, 128:256]
        ones_b = cmb[:, 0:128]
        bones_b = cmb[:, 128:256]

        def pvv(name):
            o, s = PV_OFF[name]
            n = int(np.prod(s))
            return pv[:, o:o + n]

        def pvi(name, *idx):
            o, s = PV_OFF[name]
            flat = 0
            for i, d in zip(idx, s):
                flat = flat * d + i
            return pv[:, o + flat:o + flat + 1]

        P.dma("sp", pv[:], pv_d, "c_pv", w=["pv"])
        P.dma("sp", cm[:], cm_d, "c_cm", w=["cm"])
        P.dma("sp", rw[:], rw_d, "c_rw", w=["rw"])
        P.dma("pool", cmb[:], cmb_d[:, 0:256], "c_cmb", w=["cmb"])

        o_cc = PV_OFF["cc"][0]
        P.op("act", lambda e: e.activation(out=scb[:].rearrange("p a b -> p (a b)"), in_=pv[:, o_cc:o_cc + 16],
                                           func=AF.Silu), r=["pv"], w=["scb"])
        P.op("act", lambda e: e.activation(out=smalltmp[:, 0:16], in_=pvv("lam"), func=AF.Exp, scale=-1.0),
             r=["pv"], w=["smalltmp"])
        P.op("act", lambda e: e.activation(out=smalltmp[:, 0:16], in_=smalltmp[:, 0:16], func=AF.Ln, bias=1.0),
             r=["smalltmp"], w=["smalltmp"])
        P.op("dve", lambda e: e.tensor_scalar(out=hsp[:], in0=smalltmp[:, 0:16], scalar1=-4.0, scalar2=None,
                                              op0=ALU.mult), r=["smalltmp"], w=["hsp"])
        P.op("dve", lambda e: e.tensor_scalar(out=hba[:], in0=pvv("lba"), scalar1=0.5, scalar2=None,
                                              op0=ALU.mult), r=["pv"], w=["hba"])
        P.op("dve", lambda e: e.tensor_scalar(out=hbx[:], in0=pvv("lbx"), scalar1=0.5, scalar2=None,
                                              op0=ALU.mult), r=["pv"], w=["hbx"])

        with contextlib.ExitStack() as s0:
            adaw = [sb(s0, "adaw%d" % i, [128, 8, 256], BF16) for i in range(2)]
            xin = [sb(s0, "xin%d" % i, [128, D], F32) for i in range(2)]
            def load_x(tc):
                k = tc % 2
                src = ctx_d[tc * 128:(tc + 1) * 128, :] if tc < 2 else x_d[(tc - 2) * 128:(tc - 1) * 128, :]
                P.dma("sp", xin[k][:], src, "xin%d" % k, w=[("xin", k)])
                tt = 0 if tc < 2 else 1 + (tc - 2) // 4
                for half in range(2):
                    b = P.nb()
                    for q in range(4):
                        c = half * 4 + q
                        P.op("pe", lambda e: e.transpose(out=ps[b][:, q * 128:(q + 1) * 128],
                                                         in_=xin[k][:, c * 128:(c + 1) * 128], identity=ident),
                             r=[("xin", k), "cm"], w=[psk(b)], inc=(q == 3))
                    eng = "act" if half == 0 else "dve"
                    if eng == "act":
                        P.op("act", lambda e: e.copy(out=xT[:, half * 4:half * 4 + 4, tc * 128:(tc + 1) * 128],
                                                     in_=ps[b][:].rearrange("p (q t) -> p q t", q=4)),
                             r=[psk(b)], w=[("xT", c_, tt) for c_ in range(half * 4, half * 4 + 4)])
                    else:
                        P.op("dve", lambda e: e.tensor_copy(out=xT[:, half * 4:half * 4 + 4, tc * 128:(tc + 1) * 128],
                                                            in_=ps[b][:].rearrange("p (q t) -> p q t", q=4)),
                             r=[psk(b)], w=[("xT", c_, tt) for c_ in range(half * 4, half * 4 + 4)])

            xi = 0
            NPIECE = 24
            mod_state = {}

            def mods_begin(l):
                pm = P.nb()
                P.reserved.add(pm)
                mod_state[l] = pm

            def mods_dma(l, j, bufs):
                k = j % 2
                P.dma("pool", bufs[k][:], adaw_d[l][:, j * 256:(j + 1) * 256].rearrange("(kc p) n -> p kc n", p=128),
                      "adaw%d" % k, w=[("adaw", k)])

            def mods_piece(l, j, bufs, dma=True):
                pm = mod_state[l]
                k = j % 2
                if dma:
                    mods_dma(l, j, bufs)
                for fc in range(2):
                    oc = j * 2 + fc
                    for kc in range(8):
                        P.op("pe", lambda e: e.matmul(ps[pm][:, oc * 2:oc * 2 + 2], lhsT=bufs[k][:, kc, fc * 128:(fc + 1) * 128],
                                                      rhs=scb[:, kc, :], start=(kc == 0), stop=(kc == 7)),
                             r=[("adaw", k), "scb"], w=[psk(pm)], inc=(kc == 7))

            def mods_finish(l, m0=0, m1=6, release=True):
                pm = mod_state[l]
                o_ab = PV_OFF["adab"][0] + l * 48
                for jj in range(2):
                    P.op("dve", lambda e: e.tensor_tensor(out=modv[:, l, m0 * 8:m1 * 8, jj],
                                                          in0=ps[pm][:, 0:96].rearrange("p (a b) -> p a b", b=2)[:, m0 * 8:m1 * 8, jj],
                                                          in1=pv[:, o_ab + m0 * 8:o_ab + m1 * 8], op=ALU.add),
                         r=[psk(pm), "pv"], w=[("modv", m_) for m_ in range(m0, m1)])
                if release:
                    P.reserved.discard(pm)
                for n_, (gname, mi) in enumerate((("n1g", 1), ("n2g", 4))):
                    if not (m0 <= mi < m1):
                        continue
                    og = PV_OFF[gname][0] + l * 8
                    for jj in range(2):
                        P.op("dve", lambda e: e.scalar_tensor_tensor(out=gsv[:, l, n_, :, jj], in0=modv[:, l, mi * 8:(mi + 1) * 8, jj],
                                                                     scalar=1.0, in1=pv[:, og:og + 8], op0=ALU.add, op1=ALU.mult),
                             r=[("modv", mi), "pv"], w=[("gsv", n_)])

            mods_begin(0)
            for j in range(8):
                mods_piece(0, j, adaw)
                for _ in range(2):
                    if xi < NCH:
                        load_x(xi); xi += 1
            mods_finish(0, 0, 2, release=False)
            while xi < NCH:
                load_x(xi); xi += 1
            P.barrier()

        if dbg and stage == 0:
            P.dma("sp", dbgf_d[:, 0:8 * T], xT[:].rearrange("p a b -> p (a b)"), "dbg", r=[("xT", c_, t_) for c_ in range(8) for t_ in range(5)])
            P.dma("sp", out_d[0:128, 0:192], modv[:].rearrange("p a b c -> p (a b c)"), "dbg", r=[("modv", m_) for m_ in range(6)])
            P.dma("sp", out_d[128:256, 0:64], gsv[:].rearrange("p a b c d -> p (a b c d)"), "dbg", r=[("gsv", 0), ("gsv", 1)])

        def xk(cs, tts):
            return [("xT", c_, t_) for c_ in cs for t_ in tts]

        def hk(cs, tts):
            return [("hT", c_, t_) for c_ in cs for t_ in tts]

        def norm_mod(l, nidx, tiles, stk, h2f=None, after_tile=None, post_rstd=None):
            shi = 0 if nidx == 0 else 3
            sq = [sb(stk, "nsq%d_%d_%d" % (l, nidx, i), [128, 512], BF16) for i in range(2)]
            rs = [sb(stk, "nrs%d_%d_%d" % (l, nidx, i), [128, 512], F32) for i in range(2)]
            tmp = [sb(stk, "ntmp%d_%d_%d" % (l, nidx, i), [128, 512], F32) for i in range(2)]
            def stageA(ti, tt):
                s, e_ = TT[tt]
                n = e_ - s
                b = P.nb()
                rk = ("nrs", ti % 2)
                for c in range(8):
                    k = c % 2
                    P.op("act", lambda e: e.activation(out=sq[k][:, :n], in_=xT[:, c, s:e_], func=AF.Square),
                         r=xk([c], [tt]), w=[("nsq", k)])
                    P.op("pe", lambda e: e.matmul(ps[b][:, :n], lhsT=ones_b, rhs=sq[k][:, :n], start=(c == 0), stop=(c == 7)),
                         r=[("nsq", k), "cmb"], w=[psk(b)])
                r_ = rs[ti % 2]
                P.op("act", lambda e: e.activation(out=r_[:, :n], in_=ps[b][:, :n], func=AF.Ln, scale=1.0 / D, bias=EPS),
                     r=[psk(b)], w=[rk])
                P.op("act", lambda e: e.activation(out=r_[:, :n], in_=r_[:, :n], func=AF.Exp, scale=-0.5), r=[rk], w=[rk])
                if post_rstd is not None:
                    post_rstd(ti, tt, r_, rk)

            def stageB(ti, tt):
                s, e_ = TT[tt]
                n = e_ - s
                jj = 1 if tt == 0 else 0
                rk = ("nrs", ti % 2)
                r_ = rs[ti % 2]
                for c in range(8):
                    k = c % 2
                    P.op("dve", lambda e: e.tensor_tensor(out=tmp[k][:, :n], in0=xT[:, c, s:e_], in1=r_[:, :n], op=ALU.mult),
                         r=xk([c], [tt]) + [rk], w=[("ntmp", k)])
                    if h2f is None:
                        P.op("act", lambda e: e.activation(out=hT[:, c, s:e_], in_=tmp[k][:, :n], func=AF.Identity,
                                                           scale=gsv[:, l, nidx, c, jj:jj + 1], bias=modv[:, l, shi * 8 + c, jj:jj + 1]),
                             r=[("ntmp", k), ("gsv", nidx), ("modv", shi)], w=hk([c], [tt]))
                    else:
                        P.op("act", lambda e: e.activation(out=h2f[:, c, :n], in_=tmp[k][:, :n], func=AF.Identity,
                                                           scale=gsv[:, l, nidx, c, jj:jj + 1], bias=modv[:, l, shi * 8 + c, jj:jj + 1]),
                             r=[("ntmp", k), ("gsv", nidx), ("modv", shi)], w=[("h2f", c)])
                        P.op("dve", lambda e: e.tensor_copy(out=hT[:, c, s:e_], in_=h2f[:, c, :n]),
                             r=[("h2f", c)], w=hk([c], [tt]))
                if after_tile is not None:
                    after_tile(tt)

            nt = len(tiles)
            for i in range(nt + 1):
                if i < nt:
                    stageA(i, tiles[i])
                if i - 1 >= 0:
                    stageB(i - 1, tiles[i - 1])

        def dump_and_finish():
            pass

        for l in range(NL):
            if dbg and stage == 0:
                break
            last = (l == NL - 1)
            all_tiles = [0, 1, 2, 3, 4]
            lat_tiles = [1, 2, 3, 4]
            q_tiles = lat_tiles if last else all_tiles
            with contextlib.ExitStack() as sm:
                with contextlib.ExitStack() as sn:
                    norm_mod(l, 0, all_tiles, sn)
                    P.barrier()
                if dbg and stage == 1 and l == 0:
                    P.dma("sp", dbgb_d[:, 0:8 * T], hT[:].rearrange("p a b -> p (a b)"), "dbg", r=hk(range(8), range(5)))
                    break
                NWP = 3
                wp = [sb(sm, "wp%d_%d" % (l, i), [128, 8, 128], BF16) for i in range(NWP)]
                wpc = [0]

                def wp_load(src_list):
                    k = wpc[0] % NWP
                    wpc[0] += 1
                    for (c0, c1, src) in src_list:
                        P.dma("pool", wp[k][:, :, c0:c1], src.rearrange("(kc p) n -> p kc n", p=128), "wp%d" % k, w=[("wp", k)])
                    return k

                def proj(k, tt, b):
                    s, e_ = TT[tt]
                    n = e_ - s
                    for kc in range(8):
                        P.op("pe", lambda e: e.matmul(ps[b][:, :n], lhsT=wp[k][:, kc, :], rhs=hT[:, kc, s:e_], start=(kc == 0), stop=(kc == 7)),
                             r=[("wp", k)] + hk([kc], [tt]), w=[psk(b)], inc=(kc == 7))

                mixR = sb(sm, "mixR%d" % l, [128, 4, T], BF16)
                mixA_box = [None]

                def MX(c_, p0=0, p1=128, c0=None, c1=None):
                    t_ = mixA_box[0] if c_ < 4 else mixR
                    return t_[p0:p1, c_ % 4, c0:c1]

                def mk(cs, tts):
                    return [("mixT", c_, t_) for c_ in cs for t_ in tts]

                with contextlib.ExitStack() as sl:
                    lw = sb(sl, "lw%d" % l, [128, 16, 128], BF16)
                    P.dma("pool", lw[:], lru_d[l].rearrange("p (a m) -> p a m", m=128), "c_lw", w=["lw"])
                    uh = sb(sl, "uh%d" % l, [128, T], F32)
                    cu = sb(sl, "cu%d" % l, [128, T], F32)
                    cub = sb(sl, "cub%d" % l, [128, T], BF16)
                    TA = sb(sl, "TA%d" % l, [128, T], F32)
                    TX = sb(sl, "TX%d" % l, [128, T], F32)
                    NM = sb(sl, "NM%d" % l, [128, T], F32)
                    gl = [sb(sl, "gl%d_%d" % (l, i), [128, 512], F32) for i in range(2)]
                    if l == 0:
                        adawL = [sb(sl, "adawS%d" % i, [128, 8, 256], BF16) for i in range(2)]
                        lp = [8]
                        lq = [8]
                        for _ in range(2):
                            mods_dma(0, lp[0], adawL)
                            lp[0] += 1
                    for c in range(4):
                        ku = wp_load([(0, 128, win_d[l][:, 768 + c * 128:768 + (c + 1) * 128])])
                        kg_ = wp_load([(0, 128, win_d[l][:, 1280 + c * 128:1280 + (c + 1) * 128])])
                        for tt in all_tiles:
                            s, e_ = TT[tt]
                            b = P.nb()
                            proj(ku, tt, b)
                            P.op("act", lambda e: e.copy(out=NM[:, s:e_], in_=ps[b][:, :e_ - s]), r=[psk(b)], w=["NM"])
                        if l == 0:
                            for _ in range(4):
                                if lq[0] < NPIECE:
                                    mods_piece(0, lq[0], adawL, dma=False)
                                    lq[0] += 1
                                    if lp[0] < NPIECE:
                                        mods_dma(0, lp[0], adawL)
                                        lp[0] += 1
                        P.op("dve", lambda e: e.tensor_scalar(out=cu[:, 0:T], in0=NM[:, 0:T], scalar1=pvi("cw", l, 2, c), scalar2=pvi("cb", l, c),
                                                              op0=ALU.mult, op1=ALU.add), r=["NM", "pv"], w=["cu"])
                        for (ga, gz) in ((0, C), (C, T)):
                            P.op("dve", lambda e: e.scalar_tensor_tensor(out=cu[:, ga + 2:gz], in0=NM[:, ga:gz - 2], scalar=pvi("cw", l, 0, c),
                                                                         in1=cu[:, ga + 2:gz], op0=ALU.mult, op1=ALU.add), r=["NM", "cu", "pv"], w=["cu"])
                            P.op("dve", lambda e: e.scalar_tensor_tensor(out=cu[:, ga + 1:gz], in0=NM[:, ga:gz - 1], scalar=pvi("cw", l, 1, c),
                                                                         in1=cu[:, ga + 1:gz], op0=ALU.mult, op1=ALU.add), r=["NM", "cu", "pv"], w=["cu"])
                            P.op("dve", lambda e: e.scalar_tensor_tensor(out=cu[:, ga:gz - 1], in0=NM[:, ga + 1:gz], scalar=pvi("cw", l, 3, c),
                                                                         in1=cu[:, ga:gz - 1], op0=ALU.mult, op1=ALU.add), r=["NM", "cu", "pv"], w=["cu"])
                        P.op("act", lambda e: e.copy(out=cub[:, :], in_=cu[:, :]), r=["cu"], w=["cub"])
                        for d in range(2):
                            li = (l * 2 + d) * 4 + c
                            for tt in all_tiles:
                                s, e_ = TT[tt]
                                n = e_ - s
                                ba_ = P.nb()
                                P.op("pe", lambda e: e.matmul(ps[ba_][:, :n], lhsT=lw[:, (d * 2 + 0) * 4 + c, :], rhs=cub[:, s:e_], start=True, stop=True),
                                     r=["lw", "cub"], w=[psk(ba_)])
                                P.op("act", lambda e: e.activation(out=TA[:, s:e_], in_=ps[ba_][:, :n], func=AF.Tanh, scale=0.5, bias=hba[:, li:li + 1]),
                                     r=[psk(ba_), "hba"], w=["TA"])
                            P.op("act", lambda e: e.activation(out=TA[:, :], in_=TA[:, :], func=AF.Exp, scale=hsp[:, li:li + 1], bias=hsp[:, li:li + 1]),
                                 r=["TA", "hsp"], w=["TA"])
                            P.op("dve", lambda e: e.scalar_tensor_tensor(out=NM[:, :], in0=TA[:, :], scalar=-1.0, in1=TA[:, :], op0=ALU.mult, op1=ALU.mult),
                                 r=["TA"], w=["NM"])
                            for tt in all_tiles:
                                s, e_ = TT[tt]
                                n = e_ - s
                                bx_ = P.nb()
                                P.op("pe", lambda e: e.matmul(ps[bx_][:, :n], lhsT=lw[:, (d * 2 + 1) * 4 + c, :], rhs=cub[:, s:e_], start=True, stop=True),
                                     r=["lw", "cub"], w=[psk(bx_)])
                                P.op("act", lambda e: e.activation(out=TX[:, s:e_], in_=ps[bx_][:, :n], func=AF.Tanh, scale=0.5, bias=hbx[:, li:li + 1]),
                                     r=[psk(bx_), "hbx"], w=["TX"])
                            P.op("act", lambda e: e.activation(out=NM[:, :], in_=NM[:, :], func=AF.Sqrt, scale=0.25, bias=0.25), r=["NM"], w=["NM"])
                            P.op("dve", lambda e: e.scalar_tensor_tensor(out=TX[:, :], in0=TX[:, :], scalar=1.0, in1=cu[:, :], op0=ALU.add, op1=ALU.mult),
                                 r=["TX", "cu"], w=["TX"])
                            P.op("dve", lambda e: e.tensor_tensor(out=TX[:, :], in0=TX[:, :], in1=NM[:, :], op=ALU.mult), r=["TX", "NM"], w=["TX"])
                            if d == 0:
                                P.op("dve", lambda e: e.tensor_tensor_scan(out=uh[:, 0:T], data0=TA[:, 0:T], data1=TX[:, 0:T], initial=0.0,
                                                                           op0=ALU.mult, op1=ALU.add), r=["TA", "TX", "cu"], w=["uh"])
                            else:
                                P.op("dve", lambda e: e.tensor_tensor_scan(out=NM[:, 0:C][:, ::-1], data0=TA[:, 0:C][:, ::-1], data1=TX[:, 0:C][:, ::-1],
                                                                           initial=0.0, op0=ALU.mult, op1=ALU.add), r=["TA", "TX"], w=["NM"])
                                P.op("dve", lambda e: e.tensor_tensor_scan(out=NM[:, C:T][:, ::-1], data0=TA[:, C:T][:, ::-1], data1=TX[:, C:T][:, ::-1],
                                                                           initial=NM[:, 0:1], op0=ALU.mult, op1=ALU.add), r=["TA", "TX", "NM"], w=["NM"])
                                P.op("dve", lambda e: e.tensor_tensor(out=uh[:, :], in0=uh[:, :], in1=NM[:, :], op=ALU.add), r=["uh", "NM"], w=["uh"])
                        for ti, tt in enumerate(q_tiles):
                            s, e_ = TT[tt]
                            n = e_ - s
                            b = P.nb()
                            proj(kg_, tt, b)
                            g_ = gl[ti % 2]
                            P.op("act", lambda e: e.activation(out=g_[:, :n], in_=ps[b][:, :n], func=AF.Gelu_apprx_tanh), r=[psk(b)], w=[("gl", ti % 2)])
                            P.op("dve", lambda e: e.tensor_tensor(out=MX(4 + c, c0=s, c1=e_), in0=uh[:, s:e_], in1=g_[:, :n], op=ALU.mult),
                                 r=["uh", ("gl", ti % 2)], w=mk([4 + c], [tt]))
                    if l == 0:
                        assert lq[0] == NPIECE
                        mods_finish(0, 2, 6)
                    P.barrier()

                if dbg and stage == 2 and l == 0:
                    (mixA_box[0] is not None and P.dma("sp", dbgb_d[:, 0:4 * T], mixA_box[0][:].rearrange("p a b -> p (a b)"), "dbg", r=mk(range(4), range(5)))); P.dma("sp", dbgb_d[:, 4 * T:8 * T], mixR[:].rearrange("p a b -> p (a b)"), "dbg", r=mk(range(4, 8), range(5)))
                    break

                mixA_box[0] = sb(sm, "mixA%d" % l, [128, 4, T], BF16)
                with contextlib.ExitStack() as sa:
                    ropeT = sb(sa, "rope%d" % l, [128, 2, S], BF16)
                    for a_ in range(2):
                        P.dma("pool", ropeT[:, a_, :], rope_d[a_], "c_rope", w=["rope"], max_dma_last_dim=4096)
                    ATT_STOP = int(os.environ.get("ATT_STOP", "9"))
                    kT2 = sb(sa, "kT2_%d" % l, [128, 2, T], BF16)
                    vaug = sb(sa, "vaug%d" % l, [128, NCH, 2, 128], BF16)
                    qZ = [sb(sa, "qZ%d_%d" % (l, i), [128, T], BF16) for i in range(2)]
                    NPT = 3
                    pt = [sb(sa, "pt%d_%d" % (l, i), [128, 512], BF16) for i in range(NPT)]
                    rc = [sb(sa, "rc%d_%d" % (l, i), [64, 512], F32) for i in range(2)]
                    sqh = sb(sa, "sqh%d" % l, [128, 512], BF16)
                    rsh = sb(sa, "rsh%d" % l, [128, 512], F32)
                    qn = sb(sa, "qn%d" % l, [128, 512], F32)
                    P.op("dve", lambda e: e.memset(vaug[:, :, :, 64:128], 1.0), w=["vones"])
                    P.op("dve", lambda e: e.memset(qZ[0][64:128, :], 0.0), w=["qz0"])
                    P.op("dve", lambda e: e.memset(qZ[1][0:64, :], 0.0), w=["qz1"])

                    sqh2 = [sqh, sb(sa, "sqhB%d" % l, [128, 512], BF16)]
                    rsh2 = [rsh, sb(sa, "rshB%d" % l, [128, 512], F32)]
                    qn2 = [qn, sb(sa, "qnB%d" % l, [128, 512], F32)]
                    hnc = [0]

                    def pnr_pipeline(items):
                        st = {}
                        n_it = len(items)
                        for i in range(n_it + 2):
                            if i < n_it:
                                k_, tt, gname, dst, dkeys = items[i]
                                b = P.nb()
                                proj(k_, tt, b)
                                st[i] = dict(b=b)
                            if 0 <= i - 1 < n_it:
                                k_, tt, gname, dst, dkeys = items[i - 1]
                                s, e_ = TT[tt]
                                n = e_ - s
                                b = st[i - 1]["b"]
                                u_ = hnc[0] % 2
                                hnc[0] += 1
                                st[i - 1]["u"] = u_
                                sq_, rs_, q_ = sqh2[u_], rsh2[u_], qn2[u_]
                                P.op("act", lambda e: e.activation(out=sq_[:, :n], in_=ps[b][:, :n], func=AF.Square), r=[psk(b)], w=[("sqh", u_)])
                                b2 = P.nb()
                                P.op("pe", lambda e: e.matmul(ps[b2][:, :n], lhsT=bones_b, rhs=sq_[:, :n], start=True, stop=True), r=[("sqh", u_), "cmb"], w=[psk(b2)])
                                P.op("act", lambda e: e.activation(out=rs_[:, :n], in_=ps[b2][:, :n], func=AF.Ln, scale=1.0 / 64, bias=EPS), r=[psk(b2)], w=[("rsh", u_)])
                                P.op("act", lambda e: e.activation(out=rs_[:, :n], in_=rs_[:, :n], func=AF.Exp, scale=-0.5), r=[("rsh", u_)], w=[("rsh", u_)])
                                P.op("dve", lambda e: e.scalar_tensor_tensor(out=q_[:, :n], in0=ps[b][:, :n], scalar=pvi(gname, l), in1=rs_[:, :n],
                                                                             op0=ALU.mult, op1=ALU.mult), r=[psk(b), ("rsh", u_), "pv"], w=[("qn", u_)])
                                if tt > 0:
                                    b3 = P.nb()
                                    P.op("pe", lambda e: e.matmul(ps[b3][:, :n], lhsT=perm, rhs=q_[:, :n], start=True, stop=True), r=[("qn", u_), "cm"], w=[psk(b3)])
                                    st[i - 1]["b3"] = b3
                            if 0 <= i - 2 < n_it:
                                k_, tt, gname, dst, dkeys = items[i - 2]
                                s, e_ = TT[tt]
                                n = e_ - s
                                u_ = st[i - 2]["u"]
                                sq_, rs_, q_ = sqh2[u_], rsh2[u_], qn2[u_]
                                if tt > 0:
                                    b3 = st[i - 2]["b3"]
                                    P.op("dve", lambda e: e.tensor_tensor(out=q_[:, :n], in0=q_[:, :n], in1=ropeT[:, 0, s - C:e_ - C], op=ALU.mult),
                                         r=[("qn", u_), "rope"], w=[("qn", u_)])
                                    P.op("dve", lambda e: e.tensor_tensor(out=rs_[:, :n], in0=ps[b3][:, :n], in1=ropeT[:, 1, s - C:e_ - C], op=ALU.mult),
                                         r=[psk(b3), "rope"], w=[("rsh", u_)])
                                    for (p0, p1, d_) in dst:
                                        P.op("dve", lambda e: e.tensor_tensor(out=d_, in0=q_[p0:p1, :n], in1=rs_[p0:p1, :n], op=ALU.add),
                                             r=[("qn", u_), ("rsh", u_)], w=dkeys)
                                else:
                                    for (p0, p1, d_) in dst:
                                        P.op("act", lambda e: e.copy(out=d_, in_=q_[p0:p1, :n]), r=[("qn", u_)], w=dkeys)

                    k_items = []
                    for kvh in range(2):
                        c0 = 512 + kvh * 64
                        kk = wp_load([(0, 64, win_d[l][:, c0:c0 + 64]), (64, 128, win_d[l][:, c0:c0 + 64])])
                        for tt in all_tiles:
                            s, e_ = TT[tt]
                            k_items.append((kk, tt, "kg", [(0, 128, kT2[:, kvh, s:e_])], [("kT2", kvh, tt)]))
                    pnr_pipeline(k_items)
                    kv_ = wp_load([(0, 128, win_d[l][:, 640:768])])
                    for g4 in range(5):
                        chunks = list(range(g4 * 4, min(g4 * 4 + 4, NCH)))
                        b = P.nb()
                        for i, tc in enumerate(chunks):
                            tt_ = 0 if tc < 2 else 1 + (tc - 2) // 4
                            for kc in range(8):
                                P.op("pe", lambda e: e.matmul(ps[b][:, i * 128:(i + 1) * 128], lhsT=hT[:, kc, tc * 128:(tc + 1) * 128], rhs=wp[kv_][:, kc, :],
                                                              start=(kc == 0), stop=(kc == 7)),
                                     r=[("wp", kv_)] + hk([kc], [tt_]), w=[psk(b)], inc=(kc == 7 and i == len(chunks) - 1))
                        nch = len(chunks)
                        src = ps[b][:, 0:nch * 128].rearrange("p (i k d) -> p i k d", i=nch, k=2)
                        tc0 = chunks[0]
                        P.op("act", lambda e: e.copy(out=vaug[:, tc0:tc0 + nch, :, 0:64], in_=src), r=[psk(b)], w=[("vaug", g4, 0)])
                    vkeys = [("vaug", g4, 0) for g4 in range(5)] + ["vones"]

                    if ATT_STOP <= 2:
                        P.barrier(); break
                    SB = [0, 1, 2, 3]
                    sbc = [0]
                    ptc = [0]
                    qtc = [0]
                    for c in range(4):
                        kvh = c // 2
                        kq = wp_load([(0, 128, win_d[l][:, c * 128:(c + 1) * 128])])
                        pnr_pipeline([(kq, tt, "qg", [(0, 64, qZ[0][0:64, TT[tt][0]:TT[tt][1]]), (64, 128, qZ[1][64:128, TT[tt][0]:TT[tt][1]])], [("qT", tt)])
                                      for tt in q_tiles])
                        if ATT_STOP <= 3:
                            P.barrier(); break
                        gsteps = []
                        for tt in q_tiles:
                            kchunks = [0, 1] if tt == 0 else list(range(NCH))
                            po = (4, 5) if qtc[0] % 2 == 0 else (6, 7)
                            qtc[0] += 1
                            for kc in kchunks:
                                for j in range(2):
                                    gsteps.append((tt, po, j, kc, kc == kchunks[0], kc == kchunks[-1]))

                        def emit_qk(tt, j, kc):
                            s, e_ = TT[tt]
                            n = e_ - s
                            sbk = SB[sbc[0] % 4]
                            sbc[0] += 1
                            ktt = 0 if kc < 2 else 1 + (kc - 2) // 4
                            P.op("pe", lambda e: e.matmul(ps[sbk][:, :n], lhsT=kT2[:, kvh, kc * 128:(kc + 1) * 128],
                                                          rhs=qZ[j][:, s:e_], start=True, stop=True),
                                 r=[("kT2", kvh, ktt), ("qT", tt), "qz0", "qz1"], w=[psk(sbk)])
                            return sbk

                        def emit_pv(tt, po, j, kc, first, lastk, sbk):
                            s, e_ = TT[tt]
                            n = e_ - s
                            pk = ptc[0] % NPT
                            ptc[0] += 1
                            P.op("act", lambda e: e.activation(out=pt[pk][:, :n], in_=ps[sbk][:, :n], func=AF.Exp, scale=0.125),
                                 r=[psk(sbk)], w=[("pt", pk)])
                            P.op("pe", lambda e: e.matmul(ps[po[j]][:, :n], lhsT=vaug[:, kc, kvh, :], rhs=pt[pk][:, :n],
                                                          start=first, stop=lastk),
                                 r=[("pt", pk)] + vkeys, w=[psk(po[j])])
                            if lastk:
                                P.op("dve", lambda e: e.reciprocal(out=rc[j][0:64, :n], in_=ps[po[j]][64:128, :n]), r=[psk(po[j])], w=[("rc", j)])
                                P.op("dve", lambda e: e.tensor_tensor(out=MX(c, j * 64, j * 64 + 64, s, e_), in0=ps[po[j]][0:64, :n], in1=rc[j][0:64, :n], op=ALU.mult),
                                     r=[psk(po[j]), ("rc", j)], w=mk([c], [tt]))

                        LOOK = 3
                        pend = []
                        for st_ in gsteps:
                            pend.append(st_ + (emit_qk(st_[0], st_[2], st_[3]),))
                            if len(pend) > LOOK:
                                emit_pv(*pend.pop(0))
                        while pend:
                            emit_pv(*pend.pop(0))
                    P.barrier()

                if dbg and stage == 3 and l == 0:
                    (mixA_box[0] is not None and P.dma("sp", dbgb_d[:, 0:4 * T], mixA_box[0][:].rearrange("p a b -> p (a b)"), "dbg", r=mk(range(4), range(5)))); P.dma("sp", dbgb_d[:, 4 * T:8 * T], mixR[:].rearrange("p a b -> p (a b)"), "dbg", r=mk(range(4, 8), range(5)))
                    break

                with contextlib.ExitStack() as so:
                    osq = [sb(so, "osq%d_%d" % (l, i), [128, 512], BF16) for i in range(2)]
                    ors = [sb(so, "ors%d_%d" % (l, i), [128, 512], F32) for i in range(2)]
                    oi = 0
                    for half, gname in ((0, "aog"), (1, "log")):
                        for tt in q_tiles:
                            s, e_ = TT[tt]
                            n = e_ - s
                            b = P.nb()
                            for c4 in range(4):
                                cc_ = half * 4 + c4
                                k = c4 % 2
                                P.op("act", lambda e: e.activation(out=osq[k][:, :n], in_=MX(cc_, c0=s, c1=e_), func=AF.Square), r=mk([cc_], [tt]), w=[("osq", k)])
                                P.op("pe", lambda e: e.matmul(ps[b][:, :n], lhsT=ones_b, rhs=osq[k][:, :n], start=(c4 == 0), stop=(c4 == 3)),
                                     r=[("osq", k), "cmb"], w=[psk(b)])
                            r_ = ors[oi % 2]
                            rk = ("ors", oi % 2)
                            oi += 1
                            P.op("act", lambda e: e.activation(out=r_[:, :n], in_=ps[b][:, :n], func=AF.Ln, scale=1.0 / 512, bias=EPS), r=[psk(b)], w=[rk])
                            P.op("act", lambda e: e.activation(out=r_[:, :n], in_=r_[:, :n], func=AF.Exp, scale=-0.5), r=[rk], w=[rk])
                            for c4 in range(4):
                                cc_ = half * 4 + c4
                                P.op("dve", lambda e: e.scalar_tensor_tensor(out=MX(cc_, c0=s, c1=e_), in0=MX(cc_, c0=s, c1=e_), scalar=pvi(gname, l, c4),
                                                                             in1=r_[:, :n], op0=ALU.mult, op1=ALU.mult), r=mk([cc_], [tt]) + [rk, "pv"], w=mk([cc_], [tt]))
                    if dbg and stage == 4 and l == 0:
                        (mixA_box[0] is not None and P.dma("sp", dbgb_d[:, 0:4 * T], mixA_box[0][:].rearrange("p a b -> p (a b)"), "dbg", r=mk(range(4), range(5)))); P.dma("sp", dbgb_d[:, 4 * T:8 * T], mixR[:].rearrange("p a b -> p (a b)"), "dbg", r=mk(range(4, 8), range(5)))
                    for o in range(8):
                        ko = wp_load([(0, 128, wout_d[l][:, o * 128:(o + 1) * 128])])
                        for tt in q_tiles:
                            s, e_ = TT[tt]
                            n = e_ - s
                            jj = 1 if tt == 0 else 0
                            b = P.nb()
                            for kc in range(8):
                                P.op("pe", lambda e: e.matmul(ps[b][:, :n], lhsT=wp[ko][:, kc, :], rhs=MX(kc, c0=s, c1=e_), start=(kc == 0), stop=(kc == 7)),
                                     r=[("wp", ko)] + mk([kc], [tt]), w=[psk(b)], inc=(kc == 7))
                            P.op("dve", lambda e: e.scalar_tensor_tensor(out=xT[:, o, s:e_], in0=ps[b][:, :n], scalar=modv[:, l, 2 * 8 + o, jj:jj + 1],
                                                                         in1=xT[:, o, s:e_], op0=ALU.mult, op1=ALU.add),
                                 r=[psk(b), ("modv", 2)] + xk([o], [tt]), w=xk([o], [tt]))
                    P.barrier()
            if dbg and stage in (1, 2, 3) and l == 0:
                break
            if dbg and stage == 4 and l == 0:
                P.dma("sp", dbgf_d[:, 0:8 * T], xT[:].rearrange("p a b -> p (a b)"), "dbg", r=xk(range(8), range(5)))
                break

            with contextlib.ExitStack() as se:
                NEP = 6
                ep = [sb(se, "ep%d_%d" % (l, i), [128, 4096], BF16) for i in range(NEP)]
                epc = [0]
                sel = sb(se, "sel%d" % l, [128, 16, 128], BF16)
                P.dma("pool", sel[:], cmb_d[:, 256:256 + 2048].rearrange("p (a m) -> p a m", m=128), "c_sel", w=["sel"])
                combT = sb(se, "combT%d" % l, [128, T], BF16)
                cbt = sb(se, "cbt%d" % l, [128, NCH, 16], F32)

                def ep_load(e_i):
                    ks = []
                    for wi, (wd_, pat) in enumerate(((wg_d, "g"), (wu_d, "u"), (wd_d, "d"))):
                        k = epc[0] % NEP
                        epc[0] += 1
                        if pat == "d":
                            P.dma("pool", ep[k][:].rearrange("p (j n) -> p j n", j=4), wd_[l][e_i].rearrange("(j p) n -> p j n", p=128), "ep%d" % k, w=[("ep", k)])
                        else:
                            P.dma("pool", ep[k][:].rearrange("p (j n) -> p j n", j=8), wd_[l][e_i].rearrange("(j p) n -> p j n", p=128), "ep%d" % k, w=[("ep", k)])
                        ks.append(k)
                    return ks

                m_tiles = lat_tiles if last else all_tiles
                ch0 = 2 if last else 0
                nchs = NCH - ch0
                eks = {0: ep_load(0)}
                with contextlib.ExitStack() as sg:
                    sg2 = contextlib.ExitStack()
                    rl = P.nb()
                    P.reserved.add(rl)
                    rwg = sb(sg2, "rwg%d" % l, [128, 2, 8, 16], F32)
                    cst = sb(sg2, "cst%d" % l, [16, 2], F32)
                    lgt = [sb(sg2, "lgt%d_%d" % (l, i), [16, 512], F32) for i in range(2)]
                    eT = sb(sg2, "eT%d" % l, [16, T], F32)
                    for jj in range(2):
                        for kc in range(8):
                            P.op("dve", lambda e: e.tensor_scalar(out=rwg[:, jj, kc, :], in0=rw[:, kc * 16:(kc + 1) * 16], scalar1=gsv[:, l, 1, kc, jj:jj + 1],
                                                                  scalar2=None, op0=ALU.mult), r=["rw", ("gsv", 1)], w=["rwg"])
                    bc = P.nb()
                    for kc in range(8):
                        P.op("pe", lambda e: e.matmul(ps[bc][0:16, 0:2], lhsT=rw[:, kc * 16:(kc + 1) * 16], rhs=modv[:, l, 3 * 8 + kc, :], start=(kc == 0), stop=(kc == 7)),
                             r=["rw", ("modv", 3)], w=[psk(bc)], inc=(kc == 7))
                    P.op("dve", lambda e: e.tensor_scalar(out=cst[:, :], in0=ps[bc][0:16, 0:2], scalar1=-1.0, scalar2=None, op0=ALU.mult), r=[psk(bc)], w=["cst"])

                    def router_tile(ti, tt, r_, rk):
                        s, e_ = TT[tt]
                        n = e_ - s
                        jj = 1 if tt == 0 else 0
                        b = P.nb()
                        for kc in range(8):
                            P.op("pe", lambda e: e.matmul(ps[b][0:16, :n], lhsT=rwg[:, jj, kc, :], rhs=xT[:, kc, s:e_], start=(kc == 0), stop=(kc == 7)),
                                 r=["rwg"] + xk([kc], [tt]), w=[psk(b)], inc=(kc == 7))
                        lg_ = lgt[ti % 2]
                        def fin():
                            P.op("dve", lambda e: e.tensor_tensor(out=lg_[:, :n], in0=ps[b][0:16, :n], in1=r_[0:16, :n], op=ALU.mult), r=[psk(b), rk], w=[("lgt", ti % 2)])
                            P.op("act", lambda e: e.activation(out=eT[:, s:e_], in_=lg_[:, :n], func=AF.Exp, scale=-1.0, bias=cst[:, jj:jj + 1]),
                                 r=[("lgt", ti % 2), "cst"], w=[("eT", tt)])
                        rdefer.append(fin)

                    rdefer = []

                    def flush_router(tt):
                        while rdefer:
                            rdefer.pop(0)()

                    norm_mod(l, 1, m_tiles, sg2, post_rstd=router_tile, after_tile=flush_router)
                    flush_router(None)
                    for ch in range(ch0, NCH):
                        tt_ = 0 if ch < 2 else 1 + (ch - 2) // 4
                        P.op("pe", lambda e: e.transpose(out=ps[rl][:, ch * 16:(ch + 1) * 16], in_=eT[0:16, ch * 128:(ch + 1) * 128], identity=ident[0:16, 0:16]),
                             r=[("eT", tt_), "cm"], w=[psk(rl)], inc=(ch == NCH - 1))
                    P.barrier()
                    sg2.close()

                    def rt(name):
                        return sb(sg, "%s_%d" % (name, l), [128, NCH * 16], F32)

                    sc = rt("r_sc"); bi = rt("r_bi"); selm = rt("r_sel")
                    m1 = rt("r_m1"); m2 = rt("r_m2"); mn = rt("r_mn"); gsum = rt("r_gs"); gmax = rt("r_gm"); ing = rt("r_in"); den = rt("r_den")
                    lo, hi = ch0 * 16, NCH * 16
                    nq = nchs * 4
                    P.op("dve", lambda e: e.tensor_scalar(out=sc[:, lo:hi], in0=ps[rl][:, lo:hi], scalar1=1.0, scalar2=None, op0=ALU.add), r=[psk(rl)], w=["r_sc"])
                    P.op("dve", lambda e: e.reciprocal(out=sc[:, lo:hi], in_=sc[:, lo:hi]), r=["r_sc"], w=["r_sc"])
                    orb = PV_OFF["rb"][0]
                    P.op("dve", lambda e: e.tensor_tensor(out=bi[:, lo:hi], in0=sc[:, lo:hi], in1=pv[:, orb + lo:orb + hi], op=ALU.add), r=["r_sc", "pv"], w=["r_bi"])
                    g4v = bi[:, lo:hi].rearrange("p (q i) -> p q i", i=4)
                    P.op("dve", lambda e: e.tensor_reduce(out=m1[:, 0:nq], in_=g4v, axis=AX.X, op=ALU.max), r=["r_bi"], w=["r_m1"])
                    first = True
                    for i in range(4):
                        for j in range(i + 1, 4):
                            if first:
                                P.op("dve", lambda e: e.tensor_tensor(out=m2[:, 0:nq], in0=g4v[:, :, i], in1=g4v[:, :, j], op=ALU.min), r=["r_bi"], w=["r_m2"])
                                first = False
                            else:
                                P.op("dve", lambda e: e.tensor_tensor(out=mn[:, 0:nq], in0=g4v[:, :, i], in1=g4v[:, :, j], op=ALU.min), r=["r_bi"], w=["r_mn"])
                                P.op("dve", lambda e: e.tensor_tensor(out=m2[:, 0:nq], in0=m2[:, 0:nq], in1=mn[:, 0:nq], op=ALU.max), r=["r_m2", "r_mn"], w=["r_m2"])
                    P.op("dve", lambda e: e.tensor_tensor(out=gsum[:, 0:nq], in0=m1[:, 0:nq], in1=m2[:, 0:nq], op=ALU.add), r=["r_m1", "r_m2"], w=["r_gs"])
                    gsv3 = gsum[:, 0:nq].rearrange("p (c g) -> p c g", g=4)
                    P.op("dve", lambda e: e.tensor_reduce(out=gmax[:, 0:nchs], in_=gsv3, axis=AX.X, op=ALU.max), r=["r_gs"], w=["r_gm"])
                    ing3 = ing[:, 0:nq].rearrange("p (c g) -> p c g", g=4)
                    for g in range(4):
                        P.op("dve", lambda e: e.tensor_tensor(out=ing3[:, :, g], in0=gsv3[:, :, g], in1=gmax[:, 0:nchs], op=ALU.is_ge), r=["r_gs", "r_gm"], w=["r_in"])
                    sel3 = selm[:, lo:hi].rearrange("p (q i) -> p q i", i=4)
                    for i in range(4):
                        P.op("dve", lambda e: e.tensor_tensor(out=sel3[:, :, i], in0=g4v[:, :, i], in1=m2[:, 0:nq], op=ALU.is_ge), r=["r_bi", "r_m2"], w=["r_sel"])
                        P.op("dve", lambda e: e.tensor_tensor(out=sel3[:, :, i], in0=sel3[:, :, i], in1=ing[:, 0:nq], op=ALU.mult), r=["r_sel", "r_in"], w=["r_sel"])
                    P.op("dve", lambda e: e.tensor_tensor(out=selm[:, lo:hi], in0=selm[:, lo:hi], in1=sc[:, lo:hi], op=ALU.mult), r=["r_sel", "r_sc"], w=["r_sel"])
                    P.op("dve", lambda e: e.tensor_reduce(out=den[:, 0:nchs], in_=selm[:, lo:hi].rearrange("p (c x) -> p c x", x=16), axis=AX.X, op=ALU.add),
                         r=["r_sel"], w=["r_den"])
                    P.op("dve", lambda e: e.reciprocal(out=den[:, 0:nchs], in_=den[:, 0:nchs]), r=["r_den"], w=["r_den"])
                    for ci in range(nchs):
                        ch = ch0 + ci
                        P.op("dve", lambda e: e.tensor_scalar(out=cbt[:, ch, :], in0=selm[:, ch * 16:(ch + 1) * 16], scalar1=den[:, ci:ci + 1], scalar2=None, op0=ALU.mult),
                             r=["r_sel", "r_den"], w=[("cbt", ch)])
                    for g4 in range((nchs + 3) // 4):
                        chs = list(range(ch0 + g4 * 4, min(ch0 + g4 * 4 + 4, NCH)))
                        b = P.nb()
                        for i, ch in enumerate(chs):
                            P.op("pe", lambda e: e.transpose(out=ps[b][0:16, i * 128:(i + 1) * 128], in_=cbt[:, ch, :], identity=ident),
                                 r=[("cbt", ch), "cm"], w=[psk(b)], inc=(i == len(chs) - 1))
                        P.op("act", lambda e: e.copy(out=combT[0:16, chs[0] * 128:(chs[-1] + 1) * 128], in_=ps[b][0:16, 0:len(chs) * 128]), r=[psk(b)], w=["combT"])
                    P.reserved.discard(rl)
                    P.barrier()

                if dbg and stage == 5 and l == 0:
                    P.dma("sp", dbgb_d[:, 0:8 * T], hT[:].rearrange("p a b -> p (a b)"), "dbg", r=hk(range(8), range(5)))
                    P.dma("sp", dbgf_d[:, 0:NCH * 16], cbt[:].rearrange("p a b -> p (a b)"), "dbg", r=[("cbt", ch) for ch in range(NCH)])
                    break

                with contextlib.ExitStack() as sh:
                    cbe = [sb(sh, "cbe%d_%d" % (l, i), [128, T], BF16) for i in range(2)]
                    sgt = [sb(sh, "sgt%d_%d" % (l, i), [128, 512], BF16) for i in range(2)]
                    hid = [sb(sh, "hid%d_%d" % (l, i), [128, 4, 512], BF16) for i in range(2)]
                    sgc = [0]
                    hdc = [0]
                    if l + 1 < NL:
                        adaw2 = [sb(sh, "adawL%d_%d" % (l, i), [128, 8, 256], BF16) for i in range(2)]
                        mods_begin(l + 1)
                        mp = [0]
                        mq = [0]
                    for ei in range(NE):
                        if ei + 1 < NE:
                            eks[ei + 1] = ep_load(ei + 1)
                        if l + 1 < NL:
                            while mq[0] < mp[0]:
                                mods_piece(l + 1, mq[0], adaw2, dma=False)
                                mq[0] += 1
                            for _ in range(2):
                                if mp[0] < NPIECE:
                                    mods_dma(l + 1, mp[0], adaw2)
                                    mp[0] += 1
                        kg_, ku_, kd_ = eks[ei]
                        wgv = ep[kg_][:].rearrange("p (j n) -> p j n", j=8)
                        wuv = ep[ku_][:].rearrange("p (j n) -> p j n", j=8)
                        wdv = ep[kd_][:].rearrange("p (j n) -> p j n", j=4)
                        cb_ = cbe[ei % 2]
                        ck = ("cbe", ei % 2)
                        for tt in m_tiles:
                            s, e_ = TT[tt]
                            n = e_ - s
                            b = P.nb()
                            P.op("pe", lambda e: e.matmul(ps[b][:, :n], lhsT=sel[0:16, ei, :], rhs=combT[0:16, s:e_], start=True, stop=True),
                                 r=["sel", "combT"], w=[psk(b)])
                            P.op("act", lambda e: e.copy(out=cb_[:, s:e_], in_=ps[b][:, :n]), r=[psk(b)], w=[ck])
                        for tt in m_tiles:
                            s, e_ = TT[tt]
                            n = e_ - s
                            jj = 1 if tt == 0 else 0
                            hd = hid[hdc[0] % 2]
                            hkey = ("hid", hdc[0] % 2)
                            hdc[0] += 1
                            for f in range(4):
                                bg = P.nb()
                                for kc in range(8):
                                    P.op("pe", lambda e: e.matmul(ps[bg][:, :n], lhsT=wgv[:, kc, f * 128:(f + 1) * 128], rhs=hT[:, kc, s:e_], start=(kc == 0), stop=(kc == 7)),
                                         r=[("ep", kg_)] + hk([kc], [tt]), w=[psk(bg)], inc=(kc == 7))
                                bu = P.nb()
                                for kc in range(8):
                                    P.op("pe", lambda e: e.matmul(ps[bu][:, :n], lhsT=wuv[:, kc, f * 128:(f + 1) * 128], rhs=hT[:, kc, s:e_], start=(kc == 0), stop=(kc == 7)),
                                         r=[("ep", ku_)] + hk([kc], [tt]), w=[psk(bu)], inc=(kc == 7))
                                sk = sgc[0] % 2
                                sgc[0] += 1
                                P.op("act", lambda e: e.activation(out=sgt[sk][:, :n], in_=ps[bg][:, :n], func=AF.Silu), r=[psk(bg)], w=[("sgt", sk)])
                                P.op("dve", lambda e: e.tensor_tensor(out=sgt[sk][:, :n], in0=sgt[sk][:, :n], in1=cb_[:, s:e_], op=ALU.mult), r=[("sgt", sk), ck], w=[("sgt", sk)])
                                P.op("dve", lambda e: e.tensor_tensor(out=hd[:, f, :n], in0=ps[bu][:, :n], in1=sgt[sk][:, :n], op=ALU.mult), r=[psk(bu), ("sgt", sk)], w=[hkey])
                            for o in range(8):
                                bo = P.nb()
                                for jc in range(4):
                                    P.op("pe", lambda e: e.matmul(ps[bo][:, :n], lhsT=wdv[:, jc, o * 128:(o + 1) * 128], rhs=hd[:, jc, :n], start=(jc == 0), stop=(jc == 3)),
                                         r=[("ep", kd_), hkey], w=[psk(bo)], inc=(jc == 3))
                                P.op("dve", lambda e: e.scalar_tensor_tensor(out=xT[:, o, s:e_], in0=ps[bo][:, :n], scalar=modv[:, l, 5 * 8 + o, jj:jj + 1],
                                                                             in1=xT[:, o, s:e_], op0=ALU.mult, op1=ALU.add),
                                     r=[psk(bo), ("modv", 5)] + xk([o], [tt]), w=xk([o], [tt]))
                    if l + 1 < NL:
                        assert mp[0] == NPIECE
                        while mq[0] < mp[0]:
                            mods_piece(l + 1, mq[0], adaw2, dma=False)
                            mq[0] += 1
                        mods_finish(l + 1)
                    P.barrier()
            if dbg and stage == 5 and l == 0:
                break
            if dbg and stage == 6 and l == 0:
                P.dma("sp", dbgf_d[:, 0:8 * T], xT[:].rearrange("p a b -> p (a b)"), "dbg", r=xk(range(8), range(5)))
                break

        if not dbg or stage >= 99:
            with contextlib.ExitStack() as sf:
                fsq = [sb(sf, "fsq%d" % i, [128, 512], BF16) for i in range(2)]
                frs = [sb(sf, "frs%d" % i, [128, 512], F32) for i in range(2)]
                yT = [sb(sf, "yT%d" % i, [128, 8, 512], F32) for i in range(2)]
                ob = [sb(sf, "ob%d" % i, [128, D], F32) for i in range(2)]
                obc = 0
                ofg = PV_OFF["fg"][0]
                obc_box = [0]

                def finA(ti, tt):
                    s, e_ = TT[tt]
                    n = e_ - s
                    b = P.nb()
                    for c in range(8):
                        k = c % 2
                        P.op("act", lambda e: e.activation(out=fsq[k][:, :n], in_=xT[:, c, s:e_], func=AF.Square), r=xk([c], [tt]), w=[("fsq", k)])
                        P.op("pe", lambda e: e.matmul(ps[b][:, :n], lhsT=ones_b, rhs=fsq[k][:, :n], start=(c == 0), stop=(c == 7)),
                             r=[("fsq", k), "cmb"], w=[psk(b)])
                    r_ = frs[ti % 2]
                    rk = ("frs", ti % 2)
                    P.op("act", lambda e: e.activation(out=r_[:, :n], in_=ps[b][:, :n], func=AF.Ln, scale=1.0 / D, bias=EPS), r=[psk(b)], w=[rk])
                    P.op("act", lambda e: e.activation(out=r_[:, :n], in_=r_[:, :n], func=AF.Exp, scale=-0.5), r=[rk], w=[rk])

                def finB(ti, tt):
                    s, e_ = TT[tt]
                    n = e_ - s
                    r_ = frs[ti % 2]
                    rk = ("frs", ti % 2)
                    y_ = yT[ti % 2]
                    for c in range(8):
                        P.op("dve", lambda e: e.scalar_tensor_tensor(out=y_[:, c, :n], in0=xT[:, c, s:e_], scalar=pv[:, ofg + c:ofg + c + 1], in1=r_[:, :n],
                                                                     op0=ALU.mult, op1=ALU.mult), r=xk([c], [tt]) + [rk, "pv"], w=[("yT", ti % 2, c)])
                    for i in range(4):
                        k = obc_box[0] % 2
                        obc_box[0] += 1
                        for half in range(2):
                            b2 = P.nb()
                            for q in range(4):
                                c = half * 4 + q
                                P.op("pe", lambda e: e.transpose(out=ps[b2][:, q * 128:(q + 1) * 128], in_=y_[:, c, i * 128:(i + 1) * 128], identity=ident),
                                     r=[("yT", ti % 2, c), "cm"], w=[psk(b2)], inc=(q == 3))
                            if half == 0:
                                P.op("act", lambda e: e.copy(out=ob[k][:, 0:512], in_=ps[b2][:, :]), r=[psk(b2)], w=[("ob", k, 0)])
                            else:
                                P.op("dve", lambda e: e.tensor_copy(out=ob[k][:, 512:1024], in_=ps[b2][:, :]), r=[psk(b2)], w=[("ob", k, 1)])
                        tok = (s - C) + i * 128
                        P.dma("sp", out_d[tok:tok + 128, :], ob[k][:], "o%d" % k, r=[("ob", k, 0), ("ob", k, 1)])

                ftiles = [1, 2, 3, 4]
                for i in range(len(ftiles) + 1):
                    if i < len(ftiles):
                        finA(i, ftiles[i])
                    if i >= 1:
                        finB(i - 1, ftiles[i - 1])
                P.barrier()
        for k, v in P.cnt.items():
            if k not in P.E and v > 0:
                P.E["sp"].wait_ge(P.sem[k], v)
                P._pw["sp"].append((k, v))
        P.check_deadlock()
    return nc


def _chunked(v):
    v = np.asarray(v, np.float32)
    k = v.shape[-1] // 128
    v = v.reshape(v.shape[:-1] + (k, 128))
    return np.moveaxis(v, -1, 0)


def _rope_tables():
    t = np.arange(S)
    r = (t // 64).astype(np.float32)
    col = (t % 64).astype(np.float32)
    inv = (np.float32(10000.0) ** (-np.arange(0, 32, 2, dtype=np.float32) / np.float32(32))).astype(np.float32)
    cosT = np.zeros((128, S), np.float32)
    sinT = np.zeros((128, S), np.float32)
    for p in range(128):
        d = p % 64
        axis, half, f = d // 32, (d % 32) // 16, d % 16
        pos = r if axis == 0 else col
        ang = (pos * inv[f]).astype(np.float32)
        cosT[p] = np.cos(ang)
        sinT[p] = np.sin(ang) * (-1.0 if half == 0 else 1.0)
    return np.stack([cosT, sinT]).astype(np.float32)


def _consts():
    ident = np.eye(128, dtype=np.float32)
    perm = np.zeros((128, 128), np.float32)
    for m in range(128):
        half = (m % 32) // 16
        k = m + 16 if half == 0 else m - 16
        perm[k, m] = 1.0
    cmat = np.concatenate([ident, perm], axis=1)
    ones = np.ones((128, 128), np.float32)
    bones = np.zeros((128, 128), np.float32)
    bones[0:64, 0:64] = 1.0
    bones[64:128, 64:128] = 1.0
    sel = np.zeros((128, 16, 128), np.float32)
    for e in range(16):
        sel[e, e, :] = 1.0
    cmatb = np.concatenate([ones, bones, sel.reshape(128, 2048)], axis=1)
    return cmat, cmatb


def _pvec(inp, b):
    pvv = np.zeros((128, NPV), np.float32)

    def put(name, arr):
        o, s = PV_OFF[name]
        n = int(np.prod(s))
        pvv[:, o:o + n] = np.asarray(arr, np.float32).reshape(128, n)

    cc = np.stack([inp["c"][b], inp["c_ctx"]], axis=0)
    put("cc", np.moveaxis(_chunked(cc), 1, 2))
    put("adab", _chunked(inp["ada_b"]))
    put("n1g", _chunked(inp["norm1_g"]))
    put("n2g", _chunked(inp["norm2_g"]))
    put("fg", _chunked(inp["final_g"]))
    put("qg", np.tile(inp["q_norm_g"].T, (2, 1)))
    put("kg", np.tile(inp["k_norm_g"].T, (2, 1)))
    put("cw", _chunked(inp["conv_w"]))
    put("cb", _chunked(inp["conv_b"]))
    put("lba", _chunked(inp["lru_ba"]))
    put("lbx", _chunked(inp["lru_bx"]))
    put("lam", _chunked(inp["lru_lambda"]))
    put("aog", _chunked(inp["attn_out_g"]))
    put("log", _chunked(inp["lru_out_g"]))
    put("rb", np.broadcast_to(inp["router_b"].reshape(1, 1, 16), (128, NCH, 16)))
    return pvv


def _lru_blockdiag(inp):
    out = np.zeros((NL, 128, 2, 2, 4, 128), np.float32)
    for gi, name in enumerate(("lru_wa", "lru_wx")):
        w = np.asarray(inp[name], np.float32)
        for c in range(4):
            out[:, 0:64, :, gi, c, 0:64] = np.transpose(w[:, :, 2 * c], (0, 2, 1, 3))
            out[:, 64:128, :, gi, c, 64:128] = np.transpose(w[:, :, 2 * c + 1], (0, 2, 1, 3))
    return out.reshape(NL, 128, 2048)


_CACHE = {}


def _get_nc():
    if "nc" not in _CACHE:
        _CACHE["nc"] = build()
    return _CACHE["nc"]


def make_in_maps(inp):
    inp = {k: np.asarray(v) for k, v in inp.items()}
    cmat, cmatb = _consts()
    rope = _rope_tables()
    lrubd = _lru_blockdiag(inp)
    rwl = np.ascontiguousarray(_chunked(inp["router_w"].T).transpose(0, 2, 1)).reshape(128, 128)
    shared = dict(cmat=cmat, cmatb=cmatb, rope=rope, ada_w=np.ascontiguousarray(inp["ada_w"], np.float32),
                  w_in=np.ascontiguousarray(inp["w_in"], np.float32), lrubd=lrubd,
                  w_out=np.ascontiguousarray(inp["w_out"], np.float32), rw=rwl,
                  wg=np.ascontiguousarray(inp["exp_w_gate"], np.float32), wu=np.ascontiguousarray(inp["exp_w_up"], np.float32),
                  wd=np.ascontiguousarray(inp["exp_w_down"], np.float32))
    maps = []
    for b in range(8):
        m = dict(shared)
        m["x"] = np.ascontiguousarray(inp["x"][b], np.float32)
        m["ctx"] = np.ascontiguousarray(inp["ctx"][b], np.float32)
        m["pvec"] = _pvec(inp, b)
        maps.append(m)
    return maps


def kernel(**inputs):
    nc = _get_nc()
    maps = make_in_maps(inputs)
    res = run_bass_kernel_spmd(nc, maps, core_ids=list(range(8)))
    return np.stack([np.asarray(r["out"], np.float32) for r in res.results], axis=0)
```
